# Optimizing a Trainium2 kernel written in Bass

```python
import math
import jax, jax.numpy as jnp
from jax import lax
import numpy as np

D_MODEL = 2048
BATCH = 8
SEQ = 2048
DEPTH = 1

MIX_WIDTH = D_MODEL
SSD_WIDTH = MIX_WIDTH // 2
ATTN_WIDTH = MIX_WIDTH - SSD_WIDTH
SSD_HEAD_DIM = 64
SSD_HEADS = SSD_WIDTH // SSD_HEAD_DIM
SSD_GROUPS = 2
SSD_STATE = 128
SSD_CONV = 4
SSD_CHUNK = 128
SSD_CONV_DIM = SSD_WIDTH + 2 * SSD_GROUPS * SSD_STATE
ATTN_V_DIM = 128
ATTN_HEADS = ATTN_WIDTH // ATTN_V_DIM
ATTN_QK_DIM = ATTN_V_DIM // 2
ROPE_THETA = 500000.0
ROPE_DIM = ATTN_QK_DIM // 4
Q_BLOCK = 128
IN_COLS = SSD_WIDTH + SSD_CONV_DIM + SSD_HEADS + 3 * ATTN_WIDTH
N_EXPERT_GROUPS = 4
EXPERTS_PER_GROUP = 8
N_EXPERTS = N_EXPERT_GROUPS * EXPERTS_PER_GROUP
TOP_K = 2
EXPERT_HIDDEN = D_MODEL // 2
EPS = 1e-6

kernel_name = "hybrid_ssd_diffattn_hmoe_layer"


def rms_norm(x, w, eps=EPS):
    xf = x.astype(jnp.float32)
    y = xf * lax.rsqrt(jnp.mean(xf * xf, axis=-1, keepdims=True) + eps)
    return (y * w.astype(jnp.float32)).astype(x.dtype)


def lambda_init_fn(layer_idx):
    return 0.8 - 0.6 * math.exp(-0.3 * layer_idx)


def partial_rotary(t, positions):
    inv_freq = jnp.power(ROPE_THETA, -jnp.arange(0, ROPE_DIM, 2, dtype=jnp.float32) / ROPE_DIM)
    ang = positions.astype(jnp.float32)[..., None] * inv_freq
    cos = jnp.cos(ang)[:, :, None, None, :]
    sin = jnp.sin(ang)[:, :, None, None, :]
    half = ROPE_DIM // 2
    t1 = t[..., :half].astype(jnp.float32)
    t2 = t[..., half:ROPE_DIM].astype(jnp.float32)
    rot = jnp.concatenate([t1 * cos - t2 * sin, t2 * cos + t1 * sin], axis=-1).astype(t.dtype)
    return jnp.concatenate([rot, t[..., ROPE_DIM:]], axis=-1)


def causal_depthwise_conv(u, w, b):
    c = u.shape[-1]
    kern = w.reshape(SSD_CONV, 1, c).astype(u.dtype)
    y = lax.conv_general_dilated(u, kern, window_strides=(1,), padding=[(SSD_CONV - 1, 0)],
                                 dimension_numbers=('NWC', 'WIO', 'NWC'), feature_group_count=c)
    return y + b.astype(u.dtype)


def ssd_chunked(x, dt, A, Bm, Cm):
    b, s, h, p = x.shape
    g, n = Bm.shape[2], Bm.shape[3]
    hg = h // g
    c = s // SSD_CHUNK
    L = SSD_CHUNK
    xdt = (x * dt[..., None]).reshape(b, c, L, g, hg, p)
    a_cs = jnp.cumsum((dt * A).reshape(b, c, L, g, hg), axis=2)
    Bc = Bm.reshape(b, c, L, g, n)
    Cc = Cm.reshape(b, c, L, g, n)
    causal = jnp.tril(jnp.ones((L, L), dtype=bool))[None, None, :, :, None, None]
    seg = a_cs[:, :, :, None] - a_cs[:, :, None, :]
    decay = jnp.exp(jnp.where(causal, seg, -jnp.inf))
    cb = jnp.einsum('bclgn,bcsgn->bclsg', Cc, Bc)
    y_diag = jnp.einsum('bclsg,bclsgh,bcsghp->bclghp', cb, decay, xdt)
    decay_states = jnp.exp(a_cs[:, :, -1:] - a_cs)
    states = jnp.einsum('bclgn,bclgh,bclghp->bcghpn', Bc, decay_states, xdt)
    chunk_decay = jnp.exp(a_cs[:, :, -1])

    def step(carry, inp):
        st, dec = inp
        return carry * dec[..., None, None] + st, carry

    init = jnp.zeros((b, g, hg, p, n), dtype=x.dtype)
    _, prev = lax.scan(step, init, (jnp.moveaxis(states, 1, 0), jnp.moveaxis(chunk_decay, 1, 0)))
    prev = jnp.moveaxis(prev, 0, 1)
    y_off = jnp.einsum('bclgn,bcghpn,bclgh->bclghp', Cc, prev, jnp.exp(a_cs))
    return (y_diag + y_off).reshape(b, s, h, p)


def ssd_branch(z, xbc, dt_raw, conv_w, conv_b, dt_bias, a_log, d_skip, norm_w):
    bsz, s, _ = z.shape
    xbc = jax.nn.silu(causal_depthwise_conv(xbc, conv_w, conv_b))
    xs, Bm, Cm = jnp.split(xbc, [SSD_WIDTH, SSD_WIDTH + SSD_GROUPS * SSD_STATE], axis=-1)
    xs = xs.reshape(bsz, s, SSD_HEADS, SSD_HEAD_DIM).astype(jnp.float32)
    Bm = Bm.reshape(bsz, s, SSD_GROUPS, SSD_STATE).astype(jnp.float32)
    Cm = Cm.reshape(bsz, s, SSD_GROUPS, SSD_STATE).astype(jnp.float32)
    dt = jax.nn.softplus(dt_raw.astype(jnp.float32) + dt_bias.astype(jnp.float32))
    A = -jnp.exp(a_log.astype(jnp.float32))
    y = ssd_chunked(xs, dt, A, Bm, Cm) + xs * d_skip.astype(jnp.float32)[:, None]
    y = y.reshape(bsz, s, SSD_WIDTH) * jax.nn.silu(z.astype(jnp.float32))
    y = y.reshape(bsz, s, SSD_GROUPS, SSD_WIDTH // SSD_GROUPS)
    y = y * lax.rsqrt(jnp.mean(y * y, axis=-1, keepdims=True) + EPS)
    y = y.reshape(bsz, s, SSD_WIDTH) * norm_w.astype(jnp.float32)
    return y.astype(z.dtype)


def diff_attention_branch(q, k, v, positions, q_norm_w, k_norm_w, lq1, lk1, lq2, lk2, subln_w, lam_init):
    bsz, s, _ = q.shape
    q = q.reshape(bsz, s, ATTN_HEADS, 2, ATTN_QK_DIM)
    k = k.reshape(bsz, s, ATTN_HEADS, 2, ATTN_QK_DIM)
    v = v.reshape(bsz, s, ATTN_HEADS, ATTN_V_DIM)
    q = partial_rotary(rms_norm(q, q_norm_w), positions)
    k = partial_rotary(rms_norm(k, k_norm_w), positions)
    f32 = jnp.float32
    lam = (jnp.exp(jnp.sum(lq1.astype(f32) * lk1.astype(f32)))
           - jnp.exp(jnp.sum(lq2.astype(f32) * lk2.astype(f32))) + lam_init)
    scale = 1.0 / math.sqrt(ATTN_QK_DIM)
    outs = []
    for start in range(0, s, Q_BLOCK):
        end = start + Q_BLOCK
        sc = jnp.einsum('bqhcd,bkhcd->bhcqk', q[:, start:end], k[:, :end]).astype(f32) * scale
        mask = (start + jnp.arange(Q_BLOCK))[:, None] >= jnp.arange(end)[None, :]
        pr = jax.nn.softmax(jnp.where(mask, sc, -jnp.inf), axis=-1)
        att = pr[:, :, 0] - lam * pr[:, :, 1]
        outs.append(jnp.einsum('bhqk,bkhd->bqhd', att.astype(v.dtype), v[:, :end]))
    o = jnp.concatenate(outs, axis=1)
    o = rms_norm(o, subln_w) * (1.0 - lam_init)
    return o.reshape(bsz, s, ATTN_WIDTH)


def hierarchical_moe(x, w_rg, b_rg, w_re, b_re, w_gate, w_up, w_down):
    bsz, s, d = x.shape
    xt = x.reshape(-1, d)
    n = xt.shape[0]
    g_logits = (xt @ w_rg).astype(jnp.float32) + b_rg.astype(jnp.float32)
    g_prob = jax.nn.softmax(g_logits, axis=-1)
    g_sel = jnp.argmax(g_logits, axis=-1)
    g_w = jnp.take_along_axis(g_prob, g_sel[:, None], axis=-1)
    e_logits = ((xt @ w_re).astype(jnp.float32) + b_re.astype(jnp.float32)).reshape(n, N_EXPERT_GROUPS, EXPERTS_PER_GROUP)
    e_in = jnp.take_along_axis(e_logits, g_sel[:, None, None], axis=1)[:, 0]
    top_p, top_i = lax.top_k(jax.nn.softmax(e_in, axis=-1), TOP_K)
    weights = top_p / jnp.sum(top_p, axis=-1, keepdims=True) * g_w
    expert = (g_sel[:, None] * EXPERTS_PER_GROUP + top_i).reshape(-1)
    order = jnp.argsort(expert)
    tok = order // TOP_K
    sizes = jnp.bincount(expert, length=N_EXPERTS).astype(jnp.int32)
    xs = xt[tok]
    hid = jax.nn.silu(lax.ragged_dot(xs, w_gate, sizes)) * lax.ragged_dot(xs, w_up, sizes)
    ys = lax.ragged_dot(hid, w_down, sizes) * weights.reshape(-1)[order][:, None].astype(x.dtype)
    out = jnp.zeros_like(xt).at[tok].add(ys)
    return out.reshape(bsz, s, d)


def setup_inputs(seed: int = 0) -> dict:
    key = jax.random.key(seed)
    ks = jax.random.split(key, 32)
    f32 = jnp.float32
    nrm = lambda k, shape, sc: jax.random.normal(k, shape, f32) * sc
    x = jax.random.normal(ks[0], (BATCH, SEQ, D_MODEL), f32)
    offs = jax.random.randint(ks[1], (BATCH, 1), 0, 4096, dtype=jnp.int32)
    positions = (offs + jnp.arange(SEQ, dtype=jnp.int32)[None, :]).astype(jnp.int32)
    dt0 = jnp.exp(jax.random.uniform(ks[6], (DEPTH, SSD_HEADS), f32, math.log(1e-3), math.log(1e-1)))
    return {
        'x': x,
        'positions': positions,
        'ln1_w': 1.0 + nrm(ks[2], (DEPTH, D_MODEL), 0.02),
        'w_in': nrm(ks[3], (DEPTH, D_MODEL, IN_COLS), D_MODEL ** -0.5),
        'conv_w': nrm(ks[4], (DEPTH, SSD_CONV, SSD_CONV_DIM), SSD_CONV ** -0.5),
        'conv_b': nrm(ks[5], (DEPTH, SSD_CONV_DIM), 0.02),
        'dt_bias': dt0 + jnp.log(-jnp.expm1(-dt0)),
        'a_log': jnp.log(jax.random.uniform(ks[7], (DEPTH, SSD_HEADS), f32, 1.0, 16.0)),
        'd_skip': 1.0 + nrm(ks[8], (DEPTH, SSD_HEADS), 0.02),
        'ssd_norm_w': 1.0 + nrm(ks[9], (DEPTH, SSD_WIDTH), 0.02),
        'q_norm_w': 1.0 + nrm(ks[10], (DEPTH, ATTN_QK_DIM), 0.02),
        'k_norm_w': 1.0 + nrm(ks[11], (DEPTH, ATTN_QK_DIM), 0.02),
        'lambda_q1': nrm(ks[12], (DEPTH, ATTN_QK_DIM), 0.1),
        'lambda_k1': nrm(ks[13], (DEPTH, ATTN_QK_DIM), 0.1),
        'lambda_q2': nrm(ks[14], (DEPTH, ATTN_QK_DIM), 0.1),
        'lambda_k2': nrm(ks[15], (DEPTH, ATTN_QK_DIM), 0.1),
        'subln_w': 1.0 + nrm(ks[16], (DEPTH, ATTN_V_DIM), 0.02),
        'w_out': nrm(ks[17], (DEPTH, MIX_WIDTH, D_MODEL), MIX_WIDTH ** -0.5),
        'ln2_w': 1.0 + nrm(ks[18], (DEPTH, D_MODEL), 0.02),
        'w_router_group': nrm(ks[19], (DEPTH, D_MODEL, N_EXPERT_GROUPS), D_MODEL ** -0.5),
        'b_router_group': nrm(ks[20], (DEPTH, N_EXPERT_GROUPS), 0.01),
        'w_router_expert': nrm(ks[21], (DEPTH, D_MODEL, N_EXPERTS), D_MODEL ** -0.5),
        'b_router_expert': nrm(ks[22], (DEPTH, N_EXPERTS), 0.01),
        'w_gate': nrm(ks[23], (DEPTH, N_EXPERTS, D_MODEL, EXPERT_HIDDEN), D_MODEL ** -0.5),
        'w_up': nrm(ks[24], (DEPTH, N_EXPERTS, D_MODEL, EXPERT_HIDDEN), D_MODEL ** -0.5),
        'w_down': nrm(ks[25], (DEPTH, N_EXPERTS, EXPERT_HIDDEN, D_MODEL), EXPERT_HIDDEN ** -0.5),
    }


def reference(x, positions, ln1_w, w_in, conv_w, conv_b, dt_bias, a_log, d_skip, ssd_norm_w,
              q_norm_w, k_norm_w, lambda_q1, lambda_k1, lambda_q2, lambda_k2, subln_w, w_out,
              ln2_w, w_router_group, b_router_group, w_router_expert, b_router_expert,
              w_gate, w_up, w_down):
    splits = np.cumsum([SSD_WIDTH, SSD_CONV_DIM, SSD_HEADS, ATTN_WIDTH, ATTN_WIDTH]).tolist()
    for l in range(DEPTH):
        u = rms_norm(x, ln1_w[l]) @ w_in[l]
        z, xbc, dt_raw, q, k, v = jnp.split(u, splits, axis=-1)
        y_ssd = ssd_branch(z, xbc, dt_raw, conv_w[l], conv_b[l], dt_bias[l], a_log[l], d_skip[l], ssd_norm_w[l])
        y_att = diff_attention_branch(q, k, v, positions, q_norm_w[l], k_norm_w[l], lambda_q1[l], lambda_k1[l],
                                      lambda_q2[l], lambda_k2[l], subln_w[l], lambda_init_fn(l))
        h = x + jnp.concatenate([y_ssd, y_att], axis=-1) @ w_out[l]
        x = h + hierarchical_moe(rms_norm(h, ln2_w[l]), w_router_group[l], b_router_group[l],
                                 w_router_expert[l], b_router_expert[l], w_gate[l], w_up[l], w_down[l])
    return x
```

```python
import contextlib
import math
import numpy as np
import concourse.bass as bass
import concourse.mybir as mybir
from concourse.bass_utils import run_bass_kernel_spmd

F32 = mybir.dt.float32
BF16 = mybir.dt.bfloat16
I32 = mybir.dt.int32
ALU = mybir.AluOpType
AF = mybir.ActivationFunctionType
AX = mybir.AxisListType

S = 2048
D = 2048
NT = 16
INC = 5648
NE = 32
CAP = 384
NST = CAP // 128
HID = 1024
EPS = 1e-6
LAM_INIT = 0.8 - 0.6 * math.exp(-0.3 * 0)
PI = math.pi


class KB:
    def __init__(self, nc, es):
        self.nc = nc
        self.es = es
        self.eng = {'pe': nc.tensor, 'act': nc.scalar, 'dve': nc.vector, 'pool': nc.gpsimd, 'sp': nc.sync}
        self.sems = {}
        self.cnt = {}
        self.waited = {e: {} for e in self.eng}
        self.lw = {}
        self.rd = {}
        self.ninst = 0

    def sem(self, key):
        if key not in self.sems:
            self.sems[key] = self.es.enter_context(self.nc.semaphore(str(key)))
            self.cnt[key] = 0
        return self.sems[key]

    def _deps(self, e, reads, writes):
        need = {}

        def add(k, v):
            if e == 'pe' and k == 'Epe':
                return
            if need.get(k, 0) < v:
                need[k] = v
        for r in reads:
            d = self.lw.get(r)
            if d:
                add(*d)
        for w in writes:
            d = self.lw.get(w)
            if d:
                add(*d)
            for k, v in self.rd.get(w, {}).items():
                add(k, v)
        return need

    def _emit_waits(self, e, need):
        for k, v in need.items():
            if self.waited[e].get(k, 0) < v:
                self.eng[e].wait_ge(self.sems[k], v)
                self.waited[e][k] = v
                self.ninst += 1

    def _record(self, reads, writes, tok):
        k, v = tok
        for r in reads:
            d = self.rd.setdefault(r, {})
            if d.get(k, 0) < v:
                d[k] = v
        for w in writes:
            self.lw[w] = tok
            self.rd[w] = {}

    def op(self, e, fn, reads=(), writes=(), inc=True):
        self._emit_waits(e, self._deps(e, reads, writes))
        inst = fn(self.eng[e])
        self.ninst += 1
        key = 'E' + e
        s = self.sem(key)
        if inc:
            self.cnt[key] += 1
            inst.then_inc(s, 1)
            tok = (key, self.cnt[key])
        else:
            tok = (key, self.cnt[key] + 1)
        self._record(reads, writes, tok)
        return inst

    def dma(self, e, out=None, in_=None, reads=(), writes=(), semkey=None, fn=None):
        self._emit_waits(e, self._deps(e, reads, writes))
        if fn is not None:
            inst = fn(self.eng[e])
        else:
            inst = self.eng[e].dma_start(out=out, in_=in_)
        self.ninst += 1
        s = self.sem(semkey)
        self.cnt[semkey] += 16
        inst.then_inc(s, 16)
        self._record(reads, writes, (semkey, self.cnt[semkey]))
        return inst

    def regroup(self, bufs, semkey):
        for b in bufs:
            self.lw[b] = (semkey, self.cnt[semkey])

    def barrier(self):
        for e in self.eng:
            need = {k: v for k, v in self.cnt.items() if v > 0}
            self._emit_waits(e, need)

    def finish(self, e, bufs):
        need = {}
        for b in bufs:
            d = self.lw.get(b)
            if d and need.get(d[0], 0) < d[1]:
                need[d[0]] = d[1]
        self._emit_waits(e, need)


def build(stage=99, dbg=()):
    nc = bass.Bass("TRN2", target_bir_lowering=False)

    def din(name, shape, dt=F32):
        return nc.dram_tensor(name, list(shape), dt, kind="ExternalInput").ap()

    def dscr(name, shape, dt):
        return nc.dram_tensor(name, list(shape), dt).ap()

    x_tm = din("x_tm", [S, D])
    xT = din("xT", [D, S])
    w_in = din("w_in", [D, INC])
    w_out = din("w_out", [D, D])
    w_gate = din("w_gate", [NE, D, HID])
    w_up = din("w_up", [NE, D, HID])
    w_down = din("w_down", [NE, HID, D])
    pos_tm = din("pos_tm", [128, NT], I32)
    ln1_t = din("ln1_t", [128, 16])
    conv_wt = din("conv_wt", [128, 12, 4])
    conv_bt = din("conv_bt", [128, 12])
    dt_bias = din("dt_bias", [16])
    a_log = din("a_log", [16])
    d_skip = din("d_skip", [16])
    ssd_norm_w = din("ssd_norm_w", [1024])
    q_norm_w = din("q_norm_w", [64])
    k_norm_w = din("k_norm_w", [64])
    lq1 = din("lq1", [64]); lk1 = din("lk1", [64]); lq2 = din("lq2", [64]); lk2 = din("lk2", [64])
    subln_w = din("subln_w", [128])
    ln2_w = din("ln2_w", [D])
    w_r = din("w_r", [D, 36])
    b_r = din("b_r", [36])
    c_ident = din("c_ident", [128, 128])
    c_utri = din("c_utri", [128, 128])
    c_ustrict = din("c_ustrict", [128, 128])
    c_negmask4 = din("c_negmask4", [128, 512])
    c_ehsel = din("c_ehsel", [16, 16, 128])
    c_invf = din("c_invf", [128, 8])
    c_ebase = din("c_ebase", [128, NE])

    out = nc.dram_tensor("out", [S, D], F32, kind="ExternalOutput").ap()
    dbg_out = {}
    for name, shape in dbg:
        dbg_out[name] = nc.dram_tensor("d_" + name, list(shape), F32, kind="ExternalOutput").ap()

    qT_scr = dscr("qT_scr", [128, 8, S], BF16)
    kT_scr = dscr("kT_scr", [128, 8, S], BF16)
    v_scr = dscr("v_scr", [S, 1024], BF16)
    zs_scr = dscr("zs_scr", [S, 1024], F32)
    ycat_scr = dscr("ycat_scr", [128, 16, S], BF16)
    h_scr = dscr("h_scr", [S, D], F32)
    xg_scr = dscr("xg_scr", [NE * CAP + 128, D], BF16)
    ys_scr = dscr("ys_scr", [NE * CAP + 128, D], F32)

    with contextlib.ExitStack() as es:
        kb = KB(nc, es)

        def sbt(st, name, shape, dt):
            return st.enter_context(nc.sbuf_tensor(name, list(shape), dt))

        PB = [es.enter_context(nc.psum_tensor(f"pb{k}", [128, 512], F32)) for k in range(8)]

        def pbb(k):
            return PB[k][:, :].bitcast(BF16)

        def P(k):
            return ('ps', k)

        def mm(outap, lhsT, rhs, start, stop, reads, writes, inc=True, sgc=False):
            return kb.op('pe', lambda e: e.matmul(outap, lhsT=lhsT, rhs=rhs, start=start, stop=stop, skip_group_check=sgc),
                         reads=reads, writes=writes, inc=inc)

        def tr(outap, in_, ident, reads, writes, inc=True):
            return kb.op('pe', lambda e: e.transpose(out=outap, in_=in_, identity=ident),
                         reads=reads, writes=writes, inc=inc)

        def act(outap, in_, func, reads, writes, bias=None, scale=None, accum=None):
            kw = {}
            if bias is not None:
                kw['bias'] = bias
            if scale is not None:
                kw['scale'] = scale
            if accum is not None:
                kw['accum_out'] = accum
            return kb.op('act', lambda e: e.activation(out=outap, in_=in_, func=func, **kw), reads=reads, writes=writes)

        def tt(outap, in0, in1, op, reads, writes, e='dve'):
            return kb.op(e, lambda g: g.tensor_tensor(out=outap, in0=in0, in1=in1, op=op), reads=reads, writes=writes)

        def ts(outap, in0, s1, s2, op0, op1, reads, writes, e='dve'):
            if op1 is None:
                return kb.op(e, lambda g: g.tensor_scalar(out=outap, in0=in0, scalar1=s1, scalar2=None, op0=op0),
                             reads=reads, writes=writes)
            return kb.op(e, lambda g: g.tensor_scalar(out=outap, in0=in0, scalar1=s1, scalar2=s2, op0=op0, op1=op1),
                         reads=reads, writes=writes)

        def stt(outap, in0, scalar, in1, op0, op1, reads, writes, e='dve'):
            return kb.op(e, lambda g: g.scalar_tensor_tensor(out=outap, in0=in0, scalar=scalar, in1=in1, op0=op0, op1=op1),
                         reads=reads, writes=writes)

        def red(outap, in_, op, reads, writes):
            return kb.op('dve', lambda g: g.tensor_reduce(out=outap, in_=in_, axis=AX.X, op=op), reads=reads, writes=writes)

        def cp(outap, in_, reads, writes, e='dve'):
            if e == 'act':
                return kb.op('act', lambda g: g.copy(out=outap, in_=in_), reads=reads, writes=writes)
            return kb.op(e, lambda g: g.tensor_copy(out=outap, in_=in_), reads=reads, writes=writes)

        eps_t = sbt(es, "eps_t", [128, 1], F32)
        kb.op('dve', lambda g: g.memset(eps_t[:], EPS), writes=["eps_t"])

        def rsqrt(outap, in_, mean_scale, reads, writes):
            act(outap, in_, AF.Ln, list(reads) + ["eps_t"], writes, bias=eps_t[:], scale=mean_scale)
            act(outap, outap, AF.Exp, writes, writes, scale=-0.5)

        cst = es
        names = []

        def cload(name, shape, src, dt=F32, cast=False):
            t = sbt(cst, name, shape, dt)
            kb.dma('pool' if cast else 'sp', out=t[:], in_=src, writes=[name], semkey='ldc_p' if cast else 'ldc_s')
            names.append((name, cast))
            return t

        ident_f = cload("ident_f", [128, 128], c_ident)
        ident_b = cload("ident_b", [128, 128], c_ident, BF16, True)
        utri_f = cload("utri_f", [128, 128], c_utri)
        utri_b = cload("utri_b", [128, 128], c_utri, BF16, True)
        ustrict_b = cload("ustrict_b", [128, 128], c_ustrict, BF16, True)
        negmask4_b = cload("negmask4_b", [128, 512], c_negmask4, BF16, True)
        invf = cload("invf", [128, 8], c_invf)
        ebase = cload("ebase", [128, NE], c_ebase)
        pos_i = cload("pos_i", [128, NT], pos_tm, I32)
        ln1 = cload("ln1", [128, 16], ln1_t)
        cw = cload("cw", [128, 12, 4], conv_wt)
        cbias = cload("cbias", [128, 12], conv_bt)
        dtb_bc = cload("dtb_bc", [128, 16], dt_bias.partition_broadcast(128))
        alog_bc = cload("alog_bc", [128, 16], a_log.partition_broadcast(128))
        dsk_bc = cload("dsk_bc", [128, 16], d_skip.partition_broadcast(128))
        wq_bc = cload("wq_bc", [128, 64], q_norm_w.partition_broadcast(128))
        wk_bc = cload("wk_bc", [128, 64], k_norm_w.partition_broadcast(128))
        l4 = sbt(cst, "l4", [128, 4, 64], F32)
        for n_, src in enumerate((lq1, lk1, lq2, lk2)):
            kb.dma('sp', out=l4[:, n_, :], in_=src.partition_broadcast(128), writes=["l4"], semkey='ldc_s')
        names.append(("l4", False))
        subw_bc = cload("subw_bc", [128, 128], subln_w.partition_broadcast(128))
        br_bc = cload("br_bc", [128, 36], b_r.partition_broadcast(128))
        kb.regroup([n for n, c in names if not c], 'ldc_s')
        kb.regroup([n for n, c in names if c], 'ldc_p')

        ones_f = sbt(cst, "ones_f", [128, 128], F32)
        kb.op('dve', lambda g: g.memset(ones_f[:], 1.0), writes=["ones_f"])
        ones_b = sbt(cst, "ones_b", [128, 128], BF16)
        kb.op('dve', lambda g: g.memset(ones_b[:], 1.0), writes=["ones_b"])

        A_bc = sbt(cst, "A_bc", [128, 16], F32)
        act(A_bc[:], alog_bc[:], AF.Exp, ["alog_bc"], ["A_bc"])
        ts(A_bc[:], A_bc[:], -1.0, None, ALU.mult, None, ["A_bc"], ["A_bc"])
        lam = sbt(cst, "lam", [128, 4], F32)
        lprod = sbt(cst, "lprod", [128, 2, 64], F32)
        tt(lprod[:, 0, :], l4[:, 0, :], l4[:, 1, :], ALU.mult, ["l4"], ["lprod"])
        tt(lprod[:, 1, :], l4[:, 2, :], l4[:, 3, :], ALU.mult, ["l4", "lprod"], ["lprod"])
        lsum = sbt(cst, "lsum", [128, 2], F32)
        red(lsum[:], lprod[:], ALU.add, ["lprod"], ["lsum"])
        act(lsum[:], lsum[:], AF.Exp, ["lsum"], ["lsum"])
        tt(lam[:, 0:1], lsum[:, 0:1], lsum[:, 1:2], ALU.subtract, ["lsum"], ["lam"])
        ts(lam[:, 0:1], lam[:, 0:1], LAM_INIT, None, ALU.add, None, ["lam"], ["lam"])
        ts(lam[:, 1:2], lam[:, 0:1], -1.0, None, ALU.mult, None, ["lam"], ["lam"])
        ts(wq_bc[:], wq_bc[:], 0.125, None, ALU.mult, None, ["wq_bc"], ["wq_bc"])
        ts(subw_bc[:], subw_bc[:], 1.0 - LAM_INIT, None, ALU.mult, None, ["subw_bc"], ["subw_bc"])
        pos_f = sbt(cst, "pos_f", [128, NT], F32)
        cp(pos_f[:], pos_i[:], ["pos_i"], ["pos_f"])
        ang = sbt(cst, "ang", [128, NT, 8], F32)
        tt(ang[:], pos_f[:].unsqueeze(2).to_broadcast([128, NT, 8]), invf[:].unsqueeze(1).to_broadcast([128, NT, 8]),
           ALU.mult, ["pos_f", "invf"], ["ang"])
        sin_t = sbt(cst, "sin_t", [128, NT, 8], F32)
        cos_t = sbt(cst, "cos_t", [128, NT, 8], F32)
        angi = sbt(cst, "angi", [128, NT, 8], I32)
        angk = sbt(cst, "angk", [128, NT, 8], F32)
        for (tab, nm, off) in ((sin_t, "sin_t", 0.0), (cos_t, "cos_t", 0.5 * PI)):
            ts(tab[:], ang[:], off, None, ALU.add, None, ["ang"], [nm])
            ts(angk[:], tab[:], 1.0 / (2 * PI), None, ALU.mult, None, [nm], ["angk"])
            cp(angi[:], angk[:], ["angk"], ["angi"])
            cp(angk[:], angi[:], ["angi"], ["angk"])
            stt(tab[:], angk[:], -2 * PI, tab[:], ALU.mult, ALU.add, ["angk", nm], [nm])
            ts(tab[:], tab[:], -PI, PI, ALU.max, ALU.min, [nm], [nm])
            act(tab[:], tab[:], AF.Sin, [nm], [nm])

        rstd1 = sbt(cst, "rstd1", [128, NT], F32)

        with contextlib.ExitStack() as sx:
            xTb = sbt(sx, "xTb", [128, 16, S], BF16)
            for c in range(16):
                kb.dma('pool', out=xTb[:, c, :], in_=xT[c * 128:(c + 1) * 128, :], writes=[("xTb", c)], semkey='ld_xT')
            kb.regroup([("xTb", c) for c in range(16)], 'ld_xT')
            for c in range(16):
                ts(xTb[:, c, :], xTb[:, c, :], ln1[:, c:c + 1], None, ALU.mult, None, [("xTb", c), "ln1"], [("xTb", c)])
            XT = [("xTb", c) for c in range(16)]

            with contextlib.ExitStack() as s1:
                xf = [sbt(s1, f"xf{k}", [128, D], F32) for k in range(2)]
                junk = sbt(s1, "junk", [128, D], BF16)
                ss1 = sbt(s1, "ss1", [128, NT], F32)
                kb.op('dve', lambda g: g.memset(ss1[:], 0.0), writes=["ss1"])
                for i in range(NT):
                    kb.dma('sp', out=xf[i % 2][:], in_=x_tm[i * 128:(i + 1) * 128, :], writes=[f"xf{i%2}"], semkey=f'ld_xf{i%2}')
                    act(junk[:], xf[i % 2][:], AF.Square, [f"xf{i%2}", "ss1"], ["junk", "ss1"], accum=ss1[:, i:i + 1])
                rsqrt(rstd1[:], ss1[:], 1.0 / D, ["ss1"], ["rstd1"])
                kb.barrier()

            if "rstd1" in dbg_out:
                kb.dma('sp', out=dbg_out["rstd1"], in_=rstd1[:], reads=["rstd1"], writes=["dbg_rstd1"], semkey='dbg')

            def proj_pass(col0, epilogue, tagp):
                with contextlib.ExitStack() as sp_:
                    W = sbt(sp_, "Wp" + tagp, [128, 16, 1024], BF16)
                    for half in range(2):
                        kb.dma('pool', out=W[:, :, half * 512:(half + 1) * 512],
                               in_=w_in[:, col0 + half * 512: col0 + (half + 1) * 512].rearrange("(c p) n -> p c n", p=128),
                               writes=[("Wp", half)], semkey=f'ld_Wp{half}')
                    for i in range(NT):
                        banks = []
                        for half in range(2):
                            b = half + 2 * (i % 2)
                            banks.append(b)
                            for dc in range(16):
                                mm(PB[b][:, :], xTb[:, dc, i * 128:(i + 1) * 128], W[:, dc, half * 512:(half + 1) * 512],
                                   dc == 0, dc == 15, [("xTb", dc), ("Wp", half)], [P(b)], inc=(dc == 15))
                        epilogue(i, banks, sp_)
                    kb.barrier()

            if stage >= 2:
                def qk_epilogue_factory(wbc, wbc_name, scr, tg):
                    state = {}

                    def ep(i, banks, st):
                        if not state:
                            state['qf'] = sbt(st, "qf" + tg, [128, 1024], F32)
                            state['sq'] = sbt(st, "sq" + tg, [128, 1024], F32)
                            state['ss'] = sbt(st, "ss" + tg, [128, 16], F32)
                            state['rot'] = sbt(st, "rot" + tg, [128, 4, 16, 8], F32)
                            state['qb'] = sbt(st, "qb" + tg, [128, 1024], BF16)
                            state['qt'] = sbt(st, "qt" + tg, [128, 8, 128], BF16)
                        qf, sq, ssq, rot, qb, qt = (state[k] for k in ('qf', 'sq', 'ss', 'rot', 'qb', 'qt'))
                        for half in range(2):
                            act(qf[:, half * 512:(half + 1) * 512], PB[banks[half]][:, :], AF.Copy,
                                [P(banks[half]), "rstd1"], ["qf"], scale=rstd1[:, i:i + 1])
                        tt(sq[:], qf[:], qf[:], ALU.mult, ["qf"], ["sq"])
                        red(ssq[:], sq[:].rearrange("p (b d) -> p b d", d=64), ALU.add, ["sq"], ["ssq"])
                        rsqrt(ssq[:], ssq[:], 1.0 / 64, ["ssq"], ["ssq"])
                        qf3 = qf[:].rearrange("p (b d) -> p b d", d=64)
                        tt(qf3, qf3, ssq[:].unsqueeze(2).to_broadcast([128, 16, 64]), ALU.mult, ["qf", "ssq"], ["qf"])
                        tt(qf3, qf3, wbc[:].unsqueeze(1).to_broadcast([128, 16, 64]), ALU.mult, ["qf", wbc_name], ["qf"])
                        cosb = cos_t[:, i, :].unsqueeze(1).to_broadcast([128, 16, 8])
                        sinb = sin_t[:, i, :].unsqueeze(1).to_broadcast([128, 16, 8])
                        t1 = qf3[:, :, 0:8]
                        t2 = qf3[:, :, 8:16]
                        tt(rot[:, 0], t1, cosb, ALU.mult, ["qf", "cos_t"], ["rot"])
                        tt(rot[:, 1], t2, sinb, ALU.mult, ["qf", "sin_t", "rot"], ["rot"])
                        tt(rot[:, 2], t2, cosb, ALU.mult, ["qf", "cos_t", "rot"], ["rot"])
                        tt(rot[:, 3], t1, sinb, ALU.mult, ["qf", "sin_t", "rot"], ["rot"])
                        qb3 = qb[:].rearrange("p (b d) -> p b d", d=64)
                        cp(qb3[:, :, 16:64], qf3[:, :, 16:64], ["qf"], ["qb"], e='pool')
                        tt(qb3[:, :, 0:8], rot[:, 0], rot[:, 1], ALU.subtract, ["rot", "qb"], ["qb"])
                        tt(qb3[:, :, 8:16], rot[:, 2], rot[:, 3], ALU.add, ["rot", "qb"], ["qb"])
                        pb = 4 + (i % 2)
                        for hd in range(8):
                            tr(pbb(pb)[:, hd * 128:(hd + 1) * 128], qb[:, hd * 128:(hd + 1) * 128], ident_b[:],
                               ["qb", "ident_b"], [P(pb)], inc=(hd == 7))
                        cp(qt[:].rearrange("p h t -> p (h t)"), pbb(pb), [P(pb)], ["qt"], e='act')
                        kb.dma('sp', out=scr[:, :, i * 128:(i + 1) * 128], in_=qt[:], reads=["qt"], writes=[("scr" + tg, i)],
                               semkey='st_' + tg)
                    return ep

                proj_pass(2576, qk_epilogue_factory(wq_bc, "wq_bc", qT_scr, "q"), "q")
                proj_pass(3600, qk_epilogue_factory(wk_bc, "wk_bc", kT_scr, "k"), "k")

                vstate = {}

                def v_ep(i, banks, st):
                    if not vstate:
                        vstate['vb'] = [sbt(st, f"vb{k}", [128, 1024], BF16) for k in range(2)]
                    vb = vstate['vb'][i % 2]
                    for half in range(2):
                        act(vb[:, half * 512:(half + 1) * 512], PB[banks[half]][:, :], AF.Copy,
                            [P(banks[half]), "rstd1"], [f"vb{i%2}"], scale=rstd1[:, i:i + 1])
                    kb.dma('sp', out=v_scr[i * 128:(i + 1) * 128, :], in_=vb[:], reads=[f"vb{i%2}"], writes=[("scrv", i)],
                           semkey=f'st_v{i%2}')
                proj_pass(4624, v_ep, "v")

            if stage >= 3:
                with contextlib.ExitStack() as s3:
                    xbcT = sbt(s3, "xbcT", [128, 12, S], BF16)
                    rstd_bc = sbt(s3, "rstd_bc", [128, S], F32)
                    with contextlib.ExitStack() as s31:
                        dg = [sbt(s31, f"dg{k}", [128, 128], F32) for k in range(2)]
                        for i in range(NT):
                            ts(dg[i % 2][:], ident_f[:], rstd1[:, i:i + 1], None, ALU.mult, None, ["ident_f", "rstd1"], [f"dg{i%2}"])
                            b = i // 4
                            mm(PB[b][:, (i % 4) * 128:(i % 4 + 1) * 128], ones_f[:], dg[i % 2][:], True, True,
                               ["ones_f", f"dg{i%2}"], [P(b)])
                        for b in range(4):
                            cp(rstd_bc[:, b * 512:(b + 1) * 512], PB[b][:, :], [P(b)], ["rstd_bc"])
                        Wx = [sbt(s31, f"Wx{k}", [128, 16, 512], BF16) for k in range(2)]
                        ub = [sbt(s31, f"ub{k}", [128, S + 3], F32) for k in range(1)]
                        acc = [sbt(s31, f"acc{k}", [128, S], F32) for k in range(1)]
                        for k in range(1):
                            kb.op('dve', lambda g: g.memset(ub[k][:, 0:3], 0.0), writes=[f"ub{k}"])
                        for blk in range(3):
                            kb.dma('pool', out=Wx[blk % 2][:],
                                   in_=w_in[:, 1024 + blk * 512: 1024 + (blk + 1) * 512].rearrange("(c p) n -> p c n", p=128),
                                   writes=[f"Wx{blk%2}"], semkey=f'ld_Wx{blk%2}')
                            for jj in range(4):
                                j = blk * 4 + jj
                                u = ub[0]
                                a = acc[0]
                                for tb in range(4):
                                    b = 4 + tb
                                    for dc in range(16):
                                        mm(PB[b][:, :], Wx[blk % 2][:, dc, jj * 128:(jj + 1) * 128], xTb[:, dc, tb * 512:(tb + 1) * 512],
                                           dc == 0, dc == 15, [f"Wx{blk%2}", ("xTb", dc)], [P(b)], inc=(dc == 15))
                                    tt(u[:, 3 + tb * 512: 3 + (tb + 1) * 512], PB[b][:, :], rstd_bc[:, tb * 512:(tb + 1) * 512], ALU.mult,
                                       [P(b), "rstd_bc"], ["ub0"])
                                act(a[:], u[:, 3:3 + S], AF.Identity, ["ub0", "cw", "cbias"], ["acc0"],
                                    bias=cbias[:, j:j + 1], scale=cw[:, j, 3:4])
                                for k in range(3):
                                    stt(a[:], u[:, k:k + S], cw[:, j, k:k + 1], a[:], ALU.mult, ALU.add,
                                        ["ub0", "cw", "acc0"], ["acc0"], e='dve')
                                act(xbcT[:, j, :], a[:], AF.Silu, ["acc0"], [("xbcT", j)])
                        kb.barrier()
                    if "xbcT" in dbg_out:
                        with contextlib.ExitStack() as sd:
                            tmpf = sbt(sd, "tmpf", [128, S], F32)
                            for j in range(12):
                                cp(tmpf[:], xbcT[:, j, :], [("xbcT", j)], ["tmpf"])
                                kb.dma('sp', out=dbg_out["xbcT"][j * 128:(j + 1) * 128, :], in_=tmpf[:], reads=["tmpf"], writes=["dbgx"], semkey='dbg')
                            kb.barrier()

                    dtv = sbt(s3, "dtv", [128, 256], F32)
                    sd_t = sbt(s3, "sd_t", [128, 256], F32)
                    E_t = sbt(s3, "E_t", [128, 256], F32)
                    nacs = sbt(s3, "nacs", [128, 256], F32)
                    cd_bc = sbt(s3, "cd_bc", [128, 256], F32)
                    acsT = sbt(s3, "acsT", [16, S], F32)
                    with contextlib.ExitStack() as s32:
                        Wdt = sbt(s32, "Wdt", [128, 16, 16], BF16)
                        kb.dma('pool', out=Wdt[:], in_=w_in[:, 2560:2576].rearrange("(c p) n -> p c n", p=128), writes=["Wdt"], semkey='ld_Wdt')
                        for i in range(NT):
                            for dc in range(16):
                                mm(PB[0][:, i * 16:(i + 1) * 16], xTb[:, dc, i * 128:(i + 1) * 128], Wdt[:, dc, :], dc == 0, dc == 15,
                                   [("xTb", dc), "Wdt"], [P(0)], inc=(dc == 15))
                        t1 = sbt(s32, "t1", [128, 256], F32)
                        t2 = sbt(s32, "t2", [128, 256], F32)
                        a_tok = sbt(s32, "a_tok", [128, 256], F32)
                        t13 = t1[:].rearrange("p (i h) -> p i h", h=16)
                        tt(t13, PB[0][:, 0:256].rearrange("p (i h) -> p i h", h=16), rstd1[:].unsqueeze(2).to_broadcast([128, NT, 16]),
                           ALU.mult, [P(0), "rstd1"], ["t1"])
                        tt(t13, t13, dtb_bc[:].unsqueeze(1).to_broadcast([128, NT, 16]), ALU.add, ["t1", "dtb_bc"], ["t1"])
                        stt(t2[:], t1[:], -1.0, t1[:], ALU.mult, ALU.max, ["t1"], ["t2"])
                        act(t2[:], t2[:], AF.Exp, ["t2"], ["t2"], scale=-1.0)
                        ts(t2[:], t2[:], 1.0, None, ALU.add, None, ["t2"], ["t2"])
                        act(t2[:], t2[:], AF.Ln, ["t2"], ["t2"])
                        ts(t1[:], t1[:], 0.0, None, ALU.max, None, ["t1"], ["t1"])
                        tt(dtv[:], t1[:], t2[:], ALU.add, ["t1", "t2"], ["dtv"])
                        tt(a_tok[:].rearrange("p (i h) -> p i h", h=16), dtv[:].rearrange("p (i h) -> p i h", h=16),
                           A_bc[:].unsqueeze(1).to_broadcast([128, NT, 16]), ALU.mult, ["dtv", "A_bc"], ["a_tok"])
                        mm(PB[1][:, 0:256], utri_f[:], a_tok[:], True, True, ["utri_f", "a_tok"], [P(1)])
                        mm(PB[2][:, 0:256], ones_f[:], a_tok[:], True, True, ["ones_f", "a_tok"], [P(2)])
                        for i in range(NT):
                            b = 4 + i // 4
                            mm(PB[b][0:16, (i % 4) * 128:(i % 4 + 1) * 128], a_tok[:, i * 16:(i + 1) * 16], utri_f[:], True, True,
                               ["a_tok", "utri_f"], [P(b)])
                        for b in range(4):
                            cp(acsT[:, b * 512:(b + 1) * 512], PB[4 + b][0:16, :], [P(4 + b)], ["acsT"])
                        act(E_t[:], PB[1][:, 0:256], AF.Exp, [P(1)], ["E_t"])
                        ts(nacs[:], PB[1][:, 0:256], -1.0, None, ALU.mult, None, [P(1)], ["nacs"])
                        act(cd_bc[:], PB[2][:, 0:256], AF.Exp, [P(2)], ["cd_bc"])
                        tt(t1[:], PB[2][:, 0:256], nacs[:], ALU.add, [P(2), "nacs"], ["t1"])
                        act(t1[:], t1[:], AF.Exp, ["t1"], ["t1"])
                        tt(sd_t[:], t1[:], dtv[:], ALU.mult, ["t1", "dtv"], ["sd_t"])
                        kb.barrier()

                    zst = {}

                    def z_ep(i, banks, st):
                        if not zst:
                            zst['z'] = [sbt(st, f"zsb{k}", [128, 1024], F32) for k in range(2)]
                        zb = zst['z'][i % 2]
                        for half in range(2):
                            act(zb[:, half * 512:(half + 1) * 512], PB[banks[half]][:, :], AF.Silu,
                                [P(banks[half]), "rstd1"], [f"zsb{i%2}"], scale=rstd1[:, i:i + 1])
                        kb.dma('sp', out=zs_scr[i * 128:(i + 1) * 128, :], in_=zb[:], reads=[f"zsb{i%2}"], writes=[("zs_scr", i)],
                               semkey=f'st_z{i%2}')
                    proj_pass(0, z_ep, "z")

                    with contextlib.ExitStack() as s34:
                        ehsel = sbt(s34, "ehsel", [16, 16, 128], F32)
                        kb.dma('sp', out=ehsel[:], in_=c_ehsel, writes=["ehsel"], semkey='ld_c3')
                        normw_bc = sbt(s34, "normw_bc", [128, 1024], F32)
                        kb.dma('sp', out=normw_bc[:], in_=ssd_norm_w.partition_broadcast(128), writes=["normw_bc"], semkey='ld_c3')
                        kb.regroup(["ehsel", "normw_bc"], 'ld_c3')
                        xdt = sbt(s34, "xdt", [128, 1024], BF16)
                        xdts = sbt(s34, "xdts", [128, 1024], BF16)
                        xsD = sbt(s34, "xsD", [128, 1024], F32)
                        B_tm = sbt(s34, "B_tm", [128, 256], BF16)
                        cbT = sbt(s34, "cbT", [128, 256], BF16)
                        decT = [sbt(s34, f"decT{k}", [128, 512], BF16) for k in range(2)]
                        MT = [sbt(s34, f"MT{k}", [128, 512], BF16) for k in range(2)]
                        ytmp = sbt(s34, "ytmp", [128, 512], F32)
                        y = sbt(s34, "y", [128, 1024], F32)
                        prev_f = sbt(s34, "prev_f", [128, 1024], F32)
                        prev_b = sbt(s34, "prev_b", [128, 1024], BF16)
                        zsb = sbt(s34, "zsb", [128, 1024], F32)
                        sq = sbt(s34, "sqy", [128, 1024], F32)
                        ss2 = sbt(s34, "ss2", [128, 2], F32)
                        ynb = sbt(s34, "ynb", [128, 1024], BF16)
                        ycT = sbt(s34, "ycT", [128, 8, 128], BF16)
                        for i in range(NT):
                            tsl = slice(i * 128, (i + 1) * 128)
                            kb.dma('sp', out=zsb[:], in_=zs_scr[tsl, :], reads=[("zs_scr", i)], writes=["zsb"], semkey='ld_zs')
                            for j in range(8):
                                tr(pbb(0)[:, j * 128:(j + 1) * 128], xbcT[:, j, tsl], ident_b[:], [("xbcT", j), "ident_b"], [P(0)], inc=(j == 7))
                            ps3 = pbb(0).rearrange("p (h d) -> p h d", d=64)
                            tt(xdt[:].rearrange("p (h d) -> p h d", d=64), ps3, dtv[:, i * 16:(i + 1) * 16].unsqueeze(2).to_broadcast([128, 16, 64]),
                               ALU.mult, [P(0), "dtv"], ["xdt"])
                            tt(xdts[:].rearrange("p (h d) -> p h d", d=64), ps3, sd_t[:, i * 16:(i + 1) * 16].unsqueeze(2).to_broadcast([128, 16, 64]),
                               ALU.mult, [P(0), "sd_t"], ["xdts"])
                            tt(xsD[:].rearrange("p (h d) -> p h d", d=64), ps3, dsk_bc[:].unsqueeze(2).to_broadcast([128, 16, 64]),
                               ALU.mult, [P(0), "dsk_bc"], ["xsD"])
                            for g in range(2):
                                tr(pbb(1)[:, g * 128:(g + 1) * 128], xbcT[:, 8 + g, tsl], ident_b[:], [("xbcT", 8 + g), "ident_b"], [P(1)], inc=(g == 1))
                            cp(B_tm[:], pbb(1)[:, 0:256], [P(1)], ["B_tm"], e='act')
                            for g in range(2):
                                mm(PB[2][:, g * 128:(g + 1) * 128], xbcT[:, 8 + g, tsl], xbcT[:, 10 + g, tsl], True, True,
                                   [("xbcT", 8 + g), ("xbcT", 10 + g)], [P(2)], inc=(g == 1))
                            cp(cbT[:], PB[2][:, 0:256], [P(2)], ["cbT"], e='act')
                            for hq in range(4):
                                g = hq // 2
                                rb = 3 + (hq % 2)
                                for q in range(4):
                                    h = hq * 4 + q
                                    mm(PB[rb][:, q * 128:(q + 1) * 128], ehsel[:, h, :], acsT[:, tsl], q == 0, False, ["ehsel", "acsT"], [P(rb)],
                                       inc=False, sgc=True)
                                mm(PB[rb][:, :], ident_b[:], negmask4_b[:], False, True, ["ident_b", "negmask4_b"], [P(rb)], sgc=True)
                                dT = decT[hq % 2]
                                for q in range(4):
                                    h = hq * 4 + q
                                    act(dT[:, q * 128:(q + 1) * 128], PB[rb][:, q * 128:(q + 1) * 128], AF.Exp, [P(rb), "nacs"], [f"decT{hq%2}"],
                                        bias=nacs[:, i * 16 + h: i * 16 + h + 1])
                                tt(MT[hq % 2][:].rearrange("p (q l) -> p q l", q=4), dT[:].rearrange("p (q l) -> p q l", q=4),
                                   cbT[:, g * 128:(g + 1) * 128].unsqueeze(1).to_broadcast([128, 4, 128]), ALU.mult,
                                   [f"decT{hq%2}", "cbT"], [f"MT{hq%2}"])
                                for q in range(4):
                                    h = hq * 4 + q
                                    mm(PB[5 + g][:, (h % 8) * 64:(h % 8 + 1) * 64], MT[hq % 2][:, q * 128:(q + 1) * 128], xdt[:, h * 64:(h + 1) * 64],
                                       True, True, [f"MT{hq%2}", "xdt"], [P(5 + g)], inc=(q == 3))
                            for g in range(2):
                                gs = slice(g * 512, (g + 1) * 512)
                                if i > 0:
                                    mm(PB[7][:, :], xbcT[:, 10 + g, tsl], prev_b[:, gs], True, True, [("xbcT", 10 + g), "prev_b"], [P(7)])
                                    tt(ytmp[:].rearrange("p (h d) -> p h d", d=64), PB[7][:, :].rearrange("p (h d) -> p h d", d=64),
                                       E_t[:, i * 16 + g * 8: i * 16 + g * 8 + 8].unsqueeze(2).to_broadcast([128, 8, 64]), ALU.mult,
                                       [P(7), "E_t"], ["ytmp"])
                                    tt(y[:, gs], PB[5 + g][:, :], ytmp[:], ALU.add, [P(5 + g), "ytmp"], ["y"])
                                else:
                                    cp(y[:, gs], PB[5 + g][:, :], [P(5 + g)], ["y"])
                                if i < NT - 1:
                                    mm(PB[7][:, :], B_tm[:, g * 128:(g + 1) * 128], xdts[:, gs], True, True, ["B_tm", "xdts"], [P(7)])
                                    if i > 0:
                                        tt(prev_f[:, gs].rearrange("p (h d) -> p h d", d=64), prev_f[:, gs].rearrange("p (h d) -> p h d", d=64),
                                           cd_bc[:, i * 16 + g * 8: i * 16 + g * 8 + 8].unsqueeze(2).to_broadcast([128, 8, 64]), ALU.mult,
                                           ["prev_f", "cd_bc"], ["prev_f"])
                                        tt(prev_f[:, gs], prev_f[:, gs], PB[7][:, :], ALU.add, ["prev_f", P(7)], ["prev_f"])
                                    else:
                                        cp(prev_f[:, gs], PB[7][:, :], [P(7)], ["prev_f"])
                                    cp(prev_b[:, gs], prev_f[:, gs], ["prev_f"], ["prev_b"], e='act')
                            tt(y[:], y[:], xsD[:], ALU.add, ["y", "xsD"], ["y"], e='pool')
                            tt(y[:], y[:], zsb[:], ALU.mult, ["y", "zsb"], ["y"])
                            tt(sq[:], y[:], y[:], ALU.mult, ["y"], ["sqy"])
                            red(ss2[:], sq[:].rearrange("p (g c) -> p g c", g=2), ALU.add, ["sqy"], ["ss2"])
                            rsqrt(ss2[:], ss2[:], 1.0 / 512, ["ss2"], ["ss2"])
                            tt(y[:].rearrange("p (g c) -> p g c", g=2), y[:].rearrange("p (g c) -> p g c", g=2),
                               ss2[:].unsqueeze(2).to_broadcast([128, 2, 512]), ALU.mult, ["y", "ss2"], ["y"])
                            tt(ynb[:], y[:], normw_bc[:], ALU.mult, ["y", "normw_bc"], ["ynb"])
                            for j in range(8):
                                tr(pbb(0)[:, j * 128:(j + 1) * 128], ynb[:, j * 128:(j + 1) * 128], ident_b[:], ["ynb", "ident_b"], [P(0)], inc=(j == 7))
                            cp(ycT[:].rearrange("p j t -> p (j t)"), pbb(0), [P(0)], ["ycT"], e='act')
                            kb.dma('sp', out=ycat_scr[:, 0:8, tsl], in_=ycT[:], reads=["ycT"], writes=[("ycat", 0, i)], semkey='st_yc')
                        kb.barrier()
                    kb.barrier()
            kb.barrier()

        if stage >= 4:
            with contextlib.ExitStack() as s4:
                qT = sbt(s4, "qT", [128, 8, S], BF16)
                kTz = [sbt(s4, f"kTz{c}", [128, 8, S], BF16) for c in range(2)]
                V = sbt(s4, "V", [128, NT, 1024], BF16)
                subw_col = sbt(s4, "subw_col", [128, 1], F32)
                kb.dma('sp', out=subw_col[:], in_=subln_w.rearrange("(p o) -> p o", o=1), writes=["subw_col"], semkey='ld_c4')
                ts(subw_col[:], subw_col[:], 1.0 - LAM_INIT, None, ALU.mult, None, ["subw_col"], ["subw_col"])
                for c in range(2):
                    oth = slice((1 - c) * 64, (2 - c) * 64)
                    kb.op('pool', lambda g: g.memset(kTz[c][oth, :, :], 0.0), writes=[f"kTz{c}"])
                for hd in range(8):
                    kb.dma('sp', out=qT[:, hd, :], in_=qT_scr[:, hd, :], reads=[("scrq", i) for i in range(NT)], writes=["qT"], semkey='ld_q')
                    for c in range(2):
                        cs = slice(c * 64, (c + 1) * 64)
                        kb.dma('sp', out=kTz[c][cs, hd, :], in_=kT_scr[cs, hd, :], reads=[("scrk", i) for i in range(NT)], writes=[f"kTz{c}"], semkey='ld_k')
                for i in range(NT):
                    kb.dma('sp', out=V[:, i, :], in_=v_scr[i * 128:(i + 1) * 128, :], reads=[("scrv", i)], writes=["V"], semkey='ld_v')
                kb.regroup(["qT"], 'ld_q'); kb.regroup(["kTz0", "kTz1"], 'ld_k'); kb.regroup(["V"], 'ld_v')
                NPT = 4
                pT = [sbt(s4, f"pT{k}", [128, 512], BF16) for k in range(NPT)]
                cR = [sbt(s4, f"cR{c}", [128, 512], F32) for c in range(2)]
                cO = [sbt(s4, f"cO{c}", [128, 512], F32) for c in range(2)]
                o0 = sbt(s4, "o0", [128, 512], F32)
                sqo = sbt(s4, "sqo", [128, 512], F32)
                rs = sbt(s4, "rs", [128, 512], F32)
                ycA = [sbt(s4, f"ycA{k}", [128, 512], BF16) for k in range(2)]
                SB = [0, 1, 2]
                steps = []
                blk = 0
                for hd in range(8):
                    for qb in range(4):
                        nkt = 4 * qb + 4
                        for c in range(2):
                            for kt in range(nkt):
                                steps.append(dict(hd=hd, qb=qb, c=c, kt=kt, nkt=nkt, blk=blk, last=(c == 1 and kt == nkt - 1)))
                        blk += 1

                def geom(st):
                    j = st['kt'] - 4 * st['qb']
                    off = max(j, 0) * 128
                    return j, off, 4 * st['qb'] * 128 + off, 512 - off

                def emit_S(i):
                    st = steps[i]
                    j, off, q0, nq = geom(st)
                    sbk = SB[i % 3]
                    mm(PB[sbk][:, 0:nq], kTz[st['c']][:, st['hd'], st['kt'] * 128:(st['kt'] + 1) * 128], qT[:, st['hd'], q0:q0 + nq],
                       True, True, [f"kTz{st['c']}", "qT"], [P(sbk)])

                def emit_rest(i):
                    st = steps[i]
                    j, off, q0, nq = geom(st)
                    sbk = SB[i % 3]
                    pk = i % NPT
                    c, kt, hd = st['c'], st['kt'], st['hd']
                    act(pT[pk][:, 0:nq], PB[sbk][:, 0:nq], AF.Exp, [P(sbk)], [f"pT{pk}"])
                    if j >= 0:
                        tt(pT[pk][:, 0:128], pT[pk][:, 0:128], utri_b[:], ALU.mult, [f"pT{pk}", "utri_b"], [f"pT{pk}"], e='pool')
                    mm(PB[3 + c][:, off:off + nq], V[:, kt, hd * 128:(hd + 1) * 128], pT[pk][:, 0:nq], kt == 0, kt == st['nkt'] - 1,
                       ["V", f"pT{pk}"], [P(3 + c)], inc=False, sgc=True)
                    mm(PB[5 + c][:, off:off + nq], ones_b[:], pT[pk][:, 0:nq], kt == 0, kt == st['nkt'] - 1,
                       ["ones_b", f"pT{pk}"], [P(5 + c)], sgc=True)

                def epiA(st):
                    for c in range(2):
                        cp(cO[c][:], PB[3 + c][:, :], [P(3 + c)], [f"cO{c}"])
                        cp(cR[c][:], PB[5 + c][:, :], [P(5 + c)], [f"cR{c}"])
                    for c in range(2):
                        kb.op('dve', lambda g: g.reciprocal(out=cR[c][:], in_=cR[c][:]), reads=[f"cR{c}"], writes=[f"cR{c}"])
                        tt(cO[c][:], cO[c][:], cR[c][:], ALU.mult, [f"cO{c}", f"cR{c}"], [f"cO{c}"])
                    stt(o0[:], cO[1][:], lam[:, 1:2], cO[0][:], ALU.mult, ALU.add, ["cO1", "lam", "cO0"], ["o0"])
                    tt(sqo[:], o0[:], o0[:], ALU.mult, ["o0"], ["sqo"])

                def epiB(st):
                    mm(PB[7][:, :], ones_f[:], sqo[:], True, True, ["ones_f", "sqo"], [P(7)])

                def epiC(st):
                    yk = st['blk'] % 2
                    act(rs[:], PB[7][:, :], AF.Ln, [P(7), "eps_t"], ["rs"], bias=eps_t[:], scale=1.0 / 128)
                    act(rs[:], rs[:], AF.Exp, ["rs"], ["rs"], scale=-0.5)
                    stt(ycA[yk][:], o0[:], subw_col[:, 0:1], rs[:], ALU.mult, ALU.mult, ["o0", "subw_col", "rs"], [f"ycA{yk}"])
                    kb.dma('sp', out=ycat_scr[:, 8 + st['hd'], st['qb'] * 512:(st['qb'] + 1) * 512], in_=ycA[yk][:], reads=[f"ycA{yk}"],
                           writes=[("ycat", 1, st['hd'], st['qb'])], semkey=f'st_ya{yk}')

                NS = len(steps)
                emit_S(0)
                emit_S(1)
                pend = []
                for i in range(NS):
                    emit_rest(i)
                    if i + 2 < NS:
                        emit_S(i + 2)
                    while pend and (pend[0][0] <= i or steps[i]['last']):
                        _, fn, st_ = pend.pop(0)
                        fn(st_)
                    if steps[i]['last']:
                        epiA(steps[i])
                        pend = [(i + 5, epiB, steps[i]), (i + 7, epiC, steps[i])]
                for _, fn, st_ in pend:
                    fn(st_)
                kb.barrier()

        if "ycat" in dbg_out:
            with contextlib.ExitStack() as sd:
                tb16 = sbt(sd, "tb16", [128, S], BF16)
                tmpf = sbt(sd, "tmpf2", [128, S], F32)
                for j in range(16):
                    kb.dma('sp', out=tb16[:], in_=ycat_scr[:, j, :], reads=[], writes=["tb16"], semkey='dbg2')
                    cp(tmpf[:], tb16[:], ["tb16"], ["tmpf2"])
                    kb.dma('sp', out=dbg_out["ycat"][j * 128:(j + 1) * 128, :], in_=tmpf[:], reads=["tmpf2"], writes=["dbgy"], semkey='dbg')
                kb.barrier()

        slots = sbt(es, "slots", [128, NT, 2], I32)
        wts = sbt(es, "wts", [128, NT, 2], F32)
        if stage >= 5:
            with contextlib.ExitStack() as s5:
                ycT_all = sbt(s5, "ycT_all", [128, 16, S], BF16)
                Wo = sbt(s5, "Wo", [128, 16, D], BF16)
                for j in range(16):
                    kb.dma('sp', out=ycT_all[:, j, :], in_=ycat_scr[:, j, :], writes=["ycT_all"], semkey='ld_yc')
                kb.regroup(["ycT_all"], 'ld_yc')
                for cb in range(4):
                    kb.dma('pool', out=Wo[:, :, cb * 512:(cb + 1) * 512],
                           in_=w_out[:, cb * 512:(cb + 1) * 512].rearrange("(c p) n -> p c n", p=128), writes=["Wo"], semkey='ld_Wo')
                kb.regroup(["Wo"], 'ld_Wo')
                ln2_bc = sbt(s5, "ln2_bc", [128, D], F32)
                kb.dma('sp', out=ln2_bc[:], in_=ln2_w.partition_broadcast(128), writes=["ln2_bc"], semkey='ld_c5')
                wr_sb = sbt(s5, "wr_sb", [128, 16, 36], F32)
                kb.dma('sp', out=wr_sb[:], in_=w_r.rearrange("(c p) n -> p c n", p=128), writes=["wr_sb"], semkey='ld_c5')
                kb.regroup(["ln2_bc", "wr_sb"], 'ld_c5')
                xr = sbt(s5, "xr", [128, D], F32)
                hsb = sbt(s5, "hsb", [128, D], F32)
                hn = sbt(s5, "hn", [128, D], F32)
                hnb = sbt(s5, "hnb", [128, D], BF16)
                junk5 = sbt(s5, "junk5", [128, D], BF16)
                ssh = sbt(s5, "ssh", [128, 1], F32)
                hnT = sbt(s5, "hnT", [128, 16, 128], F32)
                lg = sbt(s5, "lg", [128, 36], F32)
                r8 = sbt(s5, "r8", [128, 16], F32)
                goh = sbt(s5, "goh", [128, 4], F32)
                ein4 = sbt(s5, "ein4", [128, 4, 8], F32)
                ein = sbt(s5, "ein", [128, 8], F32)
                oh1 = sbt(s5, "oh1", [128, 8], F32)
                oh2 = sbt(s5, "oh2", [128, 8], F32)
                em = sbt(s5, "em", [128, 8], F32)
                sel1 = sbt(s5, "sel1", [128, 32], F32)
                sel2 = sbt(s5, "sel2", [128, 32], F32)
                selb = sbt(s5, "selb", [128, 32], BF16)
                cnt = sbt(s5, "cnt", [128, 32], F32)
                rk = sbt(s5, "rk", [128, 32], F32)
                tmp32 = sbt(s5, "tmp32", [128, 32], F32)
                okm = sbt(s5, "okm", [128, 32], F32)
                slf = sbt(s5, "slf", [128, 2], F32)
                kb.op('dve', lambda g: g.memset(cnt[:], 0.0), writes=["cnt"])
                for i in range(NT):
                    tsl = slice(i * 128, (i + 1) * 128)
                    kb.dma('sp', out=xr[:], in_=x_tm[tsl, :], writes=["xr"], semkey='ld_xr')
                    for cb in range(4):
                        for cc in range(16):
                            mm(PB[cb][:, :], ycT_all[:, cc, tsl], Wo[:, cc, cb * 512:(cb + 1) * 512], cc == 0, cc == 15,
                               ["ycT_all", "Wo"], [P(cb)], inc=(cc == 15))
                        tt(hsb[:, cb * 512:(cb + 1) * 512], PB[cb][:, :], xr[:, cb * 512:(cb + 1) * 512], ALU.add, [P(cb), "xr"], ["hsb"])
                    kb.dma('sp', out=h_scr[tsl, :], in_=hsb[:], reads=["hsb"], writes=[("h_scr", i)], semkey='st_h')
                    kb.op('dve', lambda g: g.memset(ssh[:], 0.0), writes=["ssh"])
                    act(junk5[:], hsb[:], AF.Square, ["hsb", "ssh"], ["junk5", "ssh"], accum=ssh[:])
                    rsqrt(ssh[:], ssh[:], 1.0 / D, ["ssh"], ["ssh"])
                    stt(hn[:], hsb[:], ssh[:, 0:1], ln2_bc[:], ALU.mult, ALU.mult, ["hsb", "ssh", "ln2_bc"], ["hn"])
                    cp(hnb[:].rearrange("t (c p) -> t c p", p=128), hn[:].rearrange("t (p c) -> t c p", c=16), ["hn"], ["hnb"], e='pool')
                    for dc in range(16):
                        b = 4 + dc // 4
                        tr(PB[b][:, (dc % 4) * 128:(dc % 4 + 1) * 128], hn[:, dc * 128:(dc + 1) * 128], ident_f[:], ["hn", "ident_f"], [P(b)],
                           inc=(dc % 4 == 3))
                    for b in range(4):
                        cp(hnT[:, b * 4:(b + 1) * 4, :].rearrange("p c t -> p (c t)"), PB[4 + b][:, :], [P(4 + b)], ["hnT"], e=('act' if b % 2 else 'dve'))
                    for dc in range(16):
                        mm(PB[0][:, 0:36], hnT[:, dc, :], wr_sb[:, dc, :], dc == 0, dc == 15, ["hnT", "wr_sb"], [P(0)], inc=(dc == 15))
                    tt(lg[:], PB[0][:, 0:36], br_bc[:], ALU.add, [P(0), "br_bc"], ["lg"])
                    red(r8[:, 0:1], lg[:, 0:4], ALU.max, ["lg"], ["r8"])
                    ts(goh[:], lg[:, 0:4], r8[:, 0:1], None, ALU.is_equal, None, ["lg", "r8"], ["goh"])
                    ts(tmp32[:, 0:4], lg[:, 0:4], r8[:, 0:1], None, ALU.subtract, None, ["lg", "r8"], ["tmp32"])
                    act(tmp32[:, 0:4], tmp32[:, 0:4], AF.Exp, ["tmp32"], ["tmp32"])
                    red(r8[:, 1:2], tmp32[:, 0:4], ALU.add, ["tmp32"], ["r8"])
                    kb.op('dve', lambda g: g.reciprocal(out=r8[:, 2:3], in_=r8[:, 1:2]), reads=["r8"], writes=["r8"])
                    tt(ein4[:], lg[:, 4:36].rearrange("p (g e) -> p g e", e=8), goh[:].unsqueeze(2).to_broadcast([128, 4, 8]), ALU.mult,
                       ["lg", "goh"], ["ein4"])
                    red(ein[:], ein4[:].rearrange("p g e -> p e g"), ALU.add, ["ein4"], ["ein"])
                    red(r8[:, 3:4], ein[:], ALU.max, ["ein"], ["r8"])
                    ts(oh1[:], ein[:], r8[:, 3:4], None, ALU.is_equal, None, ["ein", "r8"], ["oh1"])
                    stt(em[:], oh1[:], -1e30, ein[:], ALU.mult, ALU.add, ["oh1", "ein"], ["em"])
                    red(r8[:, 4:5], em[:], ALU.max, ["em"], ["r8"])
                    ts(oh2[:], em[:], r8[:, 4:5], None, ALU.is_equal, None, ["em", "r8"], ["oh2"])
                    tt(r8[:, 5:6], r8[:, 4:5], r8[:, 3:4], ALU.subtract, ["r8"], ["r8"])
                    act(r8[:, 5:6], r8[:, 5:6], AF.Exp, ["r8"], ["r8"])
                    ts(r8[:, 6:7], r8[:, 5:6], 1.0, None, ALU.add, None, ["r8"], ["r8"])
                    kb.op('dve', lambda g: g.reciprocal(out=r8[:, 6:7], in_=r8[:, 6:7]), reads=["r8"], writes=["r8"])
                    tt(wts[:, i, 0:1], r8[:, 6:7], r8[:, 2:3], ALU.mult, ["r8"], ["wts"])
                    tt(wts[:, i, 1:2], wts[:, i, 0:1], r8[:, 5:6], ALU.mult, ["wts", "r8"], ["wts"])
                    tt(sel1[:].rearrange("p (g e) -> p g e", e=8), goh[:].unsqueeze(2).to_broadcast([128, 4, 8]),
                       oh1[:].unsqueeze(1).to_broadcast([128, 4, 8]), ALU.mult, ["goh", "oh1"], ["sel1"])
                    tt(sel2[:].rearrange("p (g e) -> p g e", e=8), goh[:].unsqueeze(2).to_broadcast([128, 4, 8]),
                       oh2[:].unsqueeze(1).to_broadcast([128, 4, 8]), ALU.mult, ["goh", "oh2"], ["sel2"])
                    tt(selb[:], sel1[:], sel2[:], ALU.add, ["sel1", "sel2"], ["selb"])
                    mm(PB[1][:, 0:32], ustrict_b[:], selb[:], True, True, ["ustrict_b", "selb"], [P(1)])
                    mm(PB[1][:, 32:64], ones_b[:], selb[:], True, True, ["ones_b", "selb"], [P(1)])
                    tt(rk[:], PB[1][:, 0:32], cnt[:], ALU.add, [P(1), "cnt"], ["rk"])
                    tt(cnt[:], cnt[:], PB[1][:, 32:64], ALU.add, ["cnt", P(1)], ["cnt"])
                    ts(okm[:], rk[:], float(CAP) - 0.5, None, ALU.is_lt, None, ["rk"], ["okm"])
                    tt(rk[:], rk[:], ebase[:], ALU.add, ["rk", "ebase"], ["rk"])
                    ts(rk[:], rk[:], -float(NE * CAP), None, ALU.add, None, ["rk"], ["rk"])
                    tt(rk[:], rk[:], okm[:], ALU.mult, ["rk", "okm"], ["rk"])
                    ts(rk[:], rk[:], float(NE * CAP), None, ALU.add, None, ["rk"], ["rk"])
                    tt(tmp32[:], okm[:], sel1[:], ALU.mult, ["okm", "sel1"], ["tmp32"])
                    red(r8[:, 8:9], tmp32[:], ALU.add, ["tmp32"], ["r8"])
                    tt(tmp32[:], okm[:], sel2[:], ALU.mult, ["okm", "sel2", "r8"], ["tmp32"])
                    red(r8[:, 9:10], tmp32[:], ALU.add, ["tmp32"], ["r8"])
                    tt(wts[:, i, :], wts[:, i, :], r8[:, 8:10], ALU.mult, ["wts", "r8"], ["wts"])
                    tt(tmp32[:], rk[:], sel1[:], ALU.mult, ["rk", "sel1"], ["tmp32"])
                    red(slf[:, 0:1], tmp32[:], ALU.add, ["tmp32"], ["slf"])
                    tt(tmp32[:], rk[:], sel2[:], ALU.mult, ["rk", "sel2", "slf"], ["tmp32"])
                    red(slf[:, 1:2], tmp32[:], ALU.add, ["tmp32"], ["slf"])
                    cp(slots[:, i, :], slf[:], ["slf"], ["slots"])
                    for k in range(2):
                        kb.dma('pool', fn=lambda g: g.indirect_dma_start(
                            out=xg_scr, out_offset=bass.IndirectOffsetOnAxis(ap=slots[:, i, k:k + 1], axis=0),
                            in_=hnb[:], in_offset=None), reads=["hnb", "slots"], writes=["xg_scr"], semkey='sc_xg')
                kb.regroup(["xg_scr"], 'sc_xg')
                kb.barrier()
            if "h" in dbg_out:
                kb.dma('sp', out=dbg_out["h"], in_=h_scr, reads=[("h_scr", i) for i in range(NT)], writes=["dbgh"], semkey='dbg')
            if "slots" in dbg_out:
                with contextlib.ExitStack() as sd:
                    sf = sbt(sd, "sf", [128, NT * 2], F32)
                    cp(sf[:], slots[:].rearrange("p i k -> p (i k)"), ["slots"], ["sf"])
                    kb.dma('sp', out=dbg_out["slots"], in_=sf[:], reads=["sf"], writes=["dbgs"], semkey='dbg')
                    kb.dma('sp', out=dbg_out["wts"], in_=wts[:].rearrange("p i k -> p (i k)"), reads=["wts"], writes=["dbgw"], semkey='dbg')
                    kb.barrier()

        if stage >= 6:
            with contextlib.ExitStack() as s6:
                NWB = 4
                wbuf = [sbt(s6, f"wbuf{k}", [128, 16 * 1024], BF16) for k in range(NWB)]
                xgs = [sbt(s6, f"xg{k}", [128, NST, D], BF16) for k in range(2)]
                xgT = sbt(s6, "xgT", [128, 16, CAP], BF16)
                gT = sbt(s6, "gT", [128, 8, CAP], BF16)
                hT = sbt(s6, "hT", [128, 8, CAP], BF16)
                ysb = [sbt(s6, f"ysb{k}", [128, D], F32) for k in range(2)]
                wctr = [0]
                kb.op('dve', lambda g: g.memset(ysb[0][:], 0.0), writes=["ysb0"])
                kb.dma('sp', out=ys_scr[NE * CAP:NE * CAP + 128, :], in_=ysb[0][:], reads=["ysb0"], writes=["ys_scr"], semkey='st_ys0')

                def wload(src, a, b_, flat):
                    k = wctr[0] % NWB
                    wctr[0] += 1
                    v3 = wbuf[k][:, 0:a * b_].rearrange("p (a b) -> p a b", b=b_)
                    kb.dma('pool', out=(wbuf[k][:, 0:a * b_] if flat else v3), in_=src, writes=[f"wbuf{k}"], semkey=f'ld_w{k}')
                    return v3, f"wbuf{k}"

                def xgload(e):
                    kb.dma('sp', out=xgs[e % 2][:], in_=xg_scr[e * CAP:(e + 1) * CAP, :].rearrange("(s p) d -> p s d", p=128),
                           reads=["xg_scr"], writes=[f"xg{e%2}"], semkey=f'ld_xg{e%2}')
                xgload(0)
                ytile = [0]
                for e in range(NE):
                    if e + 1 < NE:
                        xgload(e + 1)
                    xg = xgs[e % 2]
                    for st_ in range(NST):
                        for dc4 in range(4):
                            b = dc4 % 2
                            for q4 in range(4):
                                dc = dc4 * 4 + q4
                                tr(pbb(b)[:, q4 * 128:(q4 + 1) * 128], xg[:, st_, dc * 128:(dc + 1) * 128], ident_b[:], [f"xg{e%2}", "ident_b"], [P(b)],
                                   inc=(q4 == 3))
                            cp(xgT[:, dc4 * 4:(dc4 + 1) * 4, st_ * 128:(st_ + 1) * 128],
                               pbb(b)[:, 0:512].rearrange("p (c t) -> p c t", t=128), [P(b)], ["xgT"], e=('act' if dc4 % 2 else 'dve'))
                    Wg, wgn = wload(w_gate[e].rearrange("(p c) n -> p (c n)", c=16), 16, 1024, True)
                    Wu, wun = wload(w_up[e].rearrange("(p c) n -> p (c n)", c=16), 16, 1024, True)
                    for hc in range(8):
                        bg = 2 + (hc % 2)
                        for dc in range(16):
                            mm(PB[bg][:, 0:CAP], Wg[:, dc, hc * 128:(hc + 1) * 128], xgT[:, dc, :], dc == 0, dc == 15, [wgn, "xgT"], [P(bg)], inc=(dc == 15))
                        act(gT[:, hc, :], PB[bg][:, 0:CAP], AF.Silu, [P(bg)], [("gT", hc)])
                    for hc in range(8):
                        bu = 4 + (hc % 2)
                        for dc in range(16):
                            mm(PB[bu][:, 0:CAP], Wu[:, dc, hc * 128:(hc + 1) * 128], xgT[:, dc, :], dc == 0, dc == 15, [wun, "xgT"], [P(bu)], inc=(dc == 15))
                        tt(hT[:, hc, :], gT[:, hc, :], PB[bu][:, 0:CAP], ALU.mult, [("gT", hc), P(bu)], ["hT"])
                    Wdv, wdn = wload(w_down[e].rearrange("(c p) n -> p c n", p=128), 8, D, False)
                    for st_ in range(NST):
                        yk = ytile[0] % 2
                        ytile[0] += 1
                        yb = ysb[yk]
                        for cb in range(4):
                            b = 6 + (cb % 2)
                            for kc in range(8):
                                mm(PB[b][:, :], hT[:, kc, st_ * 128:(st_ + 1) * 128], Wdv[:, kc, cb * 512:(cb + 1) * 512], kc == 0, kc == 7,
                                   ["hT", wdn], [P(b)], inc=(kc == 7))
                            cp(yb[:, cb * 512:(cb + 1) * 512], PB[b][:, :], [P(b)], [f"ysb{yk}"], e=('act' if cb % 2 else 'dve'))
                        kb.dma('sp', out=ys_scr[e * CAP + st_ * 128: e * CAP + (st_ + 1) * 128, :], in_=yb[:], reads=[f"ysb{yk}"],
                               writes=["ys_scr"], semkey=f'st_ys{yk}')
                kb.barrier()
                kb.regroup(["ys_scr"], 'st_ys0')

        if stage >= 7:
            with contextlib.ExitStack() as s7:
                hb_ = [sbt(s7, f"hc{k}", [128, D], F32) for k in range(2)]
                y1 = [sbt(s7, f"y1{k}", [128, D], F32) for k in range(2)]
                y2 = [sbt(s7, f"y2{k}", [128, D], F32) for k in range(2)]
                ia = [sbt(s7, f"ia{k}", [128, 1], I32) for k in range(2)]
                ib = [sbt(s7, f"ib{k}", [128, 1], I32) for k in range(2)]
                for k in range(2):
                    kb.op('dve', lambda g: g.memset(y1[k][:], 0.0), writes=[f"y1{k}"])
                    kb.op('dve', lambda g: g.memset(y2[k][:], 0.0), writes=[f"y2{k}"])
                for i in range(NT):
                    k = i % 2
                    tsl = slice(i * 128, (i + 1) * 128)
                    kb.dma('sp', out=hb_[k][:], in_=h_scr[tsl, :], reads=[("h_scr", i)], writes=[f"hc{k}"], semkey=f'ld_hc{k}')
                    cp(ia[k][:], slots[:, i, 0:1], ["slots"], [f"ia{k}"])
                    cp(ib[k][:], slots[:, i, 1:2], ["slots"], [f"ib{k}"])
                    kb.dma('pool', fn=lambda g: g.indirect_dma_start(
                        out=y1[k][:], out_offset=None, in_=ys_scr,
                        in_offset=bass.IndirectOffsetOnAxis(ap=ia[k][:, 0:1], axis=0)),
                        reads=["ys_scr", f"ia{k}"], writes=[f"y1{k}"], semkey=f'ga_y1{k}')
                    kb.dma('pool', fn=lambda g: g.indirect_dma_start(
                        out=y2[k][:], out_offset=None, in_=ys_scr,
                        in_offset=bass.IndirectOffsetOnAxis(ap=ib[k][:, 0:1], axis=0)),
                        reads=["ys_scr", f"ib{k}"], writes=[f"y2{k}"], semkey=f'ga_y2{k}')
                    stt(hb_[k][:], y1[k][:], wts[:, i, 0:1], hb_[k][:], ALU.mult, ALU.add, [f"y1{k}", "wts", f"hc{k}"], [f"hc{k}"])
                    stt(hb_[k][:], y2[k][:], wts[:, i, 1:2], hb_[k][:], ALU.mult, ALU.add, [f"y2{k}", "wts", f"hc{k}"], [f"hc{k}"])
                    kb.dma('sp', out=out[tsl, :], in_=hb_[k][:], reads=[f"hc{k}"], writes=[("out", i)], semkey=f'st_out{k}')
                kb.barrier()
        kb.barrier()
        print("instructions (incl. waits):", kb.ninst, "sems:", len(kb.sems))
    return nc


def host_consts():
    j = np.arange(128)[:, None]
    l = np.arange(128)[None, :]
    c = {}
    c["c_ident"] = np.eye(128, dtype=np.float32)
    c["c_utri"] = (j <= l).astype(np.float32)
    c["c_ustrict"] = (j < l).astype(np.float32)
    c["c_negmask4"] = np.tile(np.where(l < j, -30000.0, 0.0).astype(np.float32), (1, 4))
    eh = np.zeros((16, 16, 128), np.float32)
    for h in range(16):
        eh[h, h, :] = 1.0
    c["c_ehsel"] = eh
    invf = (500000.0 ** (-np.arange(0, 16, 2, dtype=np.float32) / 16.0)).astype(np.float32)
    c["c_invf"] = np.tile(invf[None, :], (128, 1)).astype(np.float32)
    c["c_ebase"] = np.tile((np.arange(NE, dtype=np.float32) * CAP)[None, :], (128, 1)).astype(np.float32)
    return c


def make_in_maps(inputs, cores):
    f = lambda a: np.ascontiguousarray(a)
    shared = dict(host_consts())
    shared["w_in"] = f(inputs["w_in"][0])
    shared["w_out"] = f(inputs["w_out"][0])
    shared["w_gate"] = f(inputs["w_gate"][0])
    shared["w_up"] = f(inputs["w_up"][0])
    shared["w_down"] = f(inputs["w_down"][0])
    shared["ln1_t"] = f(inputs["ln1_w"][0].reshape(16, 128).T)
    shared["conv_wt"] = f(inputs["conv_w"][0].reshape(4, 12, 128).transpose(2, 1, 0))
    shared["conv_bt"] = f(inputs["conv_b"][0].reshape(12, 128).T)
    for k in ("dt_bias", "a_log", "d_skip", "ssd_norm_w", "q_norm_w", "k_norm_w", "subln_w", "ln2_w"):
        shared[k] = f(inputs[k][0])
    shared["lq1"] = f(inputs["lambda_q1"][0]); shared["lk1"] = f(inputs["lambda_k1"][0])
    shared["lq2"] = f(inputs["lambda_q2"][0]); shared["lk2"] = f(inputs["lambda_k2"][0])
    shared["w_r"] = f(np.concatenate([inputs["w_router_group"][0], inputs["w_router_expert"][0]], axis=1))
    shared["b_r"] = f(np.concatenate([inputs["b_router_group"][0], inputs["b_router_expert"][0]], axis=0))
    maps = []
    for b in cores:
        m = dict(shared)
        xb = inputs["x"][b]
        m["x_tm"] = f(xb)
        m["xT"] = f(xb.T)
        m["pos_tm"] = f(inputs["positions"][b].reshape(NT, 128).T.astype(np.int32))
        maps.append(m)
    return maps


def kernel(**inputs):
    inputs = {k: np.asarray(v) for k, v in inputs.items()}
    nc = build()
    maps = make_in_maps(inputs, list(range(8)))
    res = run_bass_kernel_spmd(nc, maps, core_ids=list(range(8)))
    return np.stack([r["out"] for r in res.results], axis=0).astype(np.float32)
```

```python
import contextlib
import math
import numpy as np
import concourse.bass as bass
import concourse.mybir as mybir
from concourse.bass_utils import run_bass_kernel_spmd

F32 = mybir.dt.float32
BF16 = mybir.dt.bfloat16
I32 = mybir.dt.int32
ALU = mybir.AluOpType
AF = mybir.ActivationFunctionType
AX = mybir.AxisListType

S = 2048
D = 2048
NT = 16
INC = 5648
NE = 32
CAP = 384
NST = CAP // 128
HID = 1024
EPS = 1e-6
LAM_INIT = 0.8 - 0.6 * math.exp(-0.3 * 0)
PI = math.pi


class KB:
    def __init__(self, nc, es):
        self.nc = nc
        self.es = es
        self.eng = {'pe': nc.tensor, 'act': nc.scalar, 'dve': nc.vector, 'pool': nc.gpsimd, 'sp': nc.sync}
        self.sems = {}
        self.cnt = {}
        self.waited = {e: {} for e in self.eng}
        self.lw = {}
        self.rd = {}
        self.ninst = 0

    def sem(self, key):
        if key not in self.sems:
            self.sems[key] = self.es.enter_context(self.nc.semaphore(str(key)))
            self.cnt[key] = 0
        return self.sems[key]

    def _deps(self, e, reads, writes):
        need = {}

        def add(k, v):
            if e == 'pe' and k == 'Epe':
                return
            if need.get(k, 0) < v:
                need[k] = v
        for r in reads:
            d = self.lw.get(r)
            if d:
                add(*d)
        for w in writes:
            d = self.lw.get(w)
            if d:
                add(*d)
            for k, v in self.rd.get(w, {}).items():
                add(k, v)
        return need

    def _emit_waits(self, e, need):
        for k, v in need.items():
            if self.waited[e].get(k, 0) < v:
                self.eng[e].wait_ge(self.sems[k], v)
                self.waited[e][k] = v
                self.ninst += 1

    def _record(self, reads, writes, tok):
        k, v = tok
        for r in reads:
            d = self.rd.setdefault(r, {})
            if d.get(k, 0) < v:
                d[k] = v
        for w in writes:
            self.lw[w] = tok
            self.rd[w] = {}

    def op(self, e, fn, reads=(), writes=(), inc=True):
        self._emit_waits(e, self._deps(e, reads, writes))
        inst = fn(self.eng[e])
        self.ninst += 1
        key = 'E' + e
        s = self.sem(key)
        if inc:
            self.cnt[key] += 1
            inst.then_inc(s, 1)
            tok = (key, self.cnt[key])
        else:
            tok = (key, self.cnt[key] + 1)
        self._record(reads, writes, tok)
        return inst

    def dma(self, e, out=None, in_=None, reads=(), writes=(), semkey=None, fn=None):
        self._emit_waits(e, self._deps(e, reads, writes))
        if fn is not None:
            inst = fn(self.eng[e])
        else:
            inst = self.eng[e].dma_start(out=out, in_=in_)
        self.ninst += 1
        s = self.sem(semkey)
        self.cnt[semkey] += 16
        inst.then_inc(s, 16)
        self._record(reads, writes, (semkey, self.cnt[semkey]))
        return inst

    def regroup(self, bufs, semkey):
        for b in bufs:
            self.lw[b] = (semkey, self.cnt[semkey])

    def barrier(self):
        for e in self.eng:
            need = {k: v for k, v in self.cnt.items() if v > 0}
            self._emit_waits(e, need)

    def finish(self, e, bufs):
        need = {}
        for b in bufs:
            d = self.lw.get(b)
            if d and need.get(d[0], 0) < d[1]:
                need[d[0]] = d[1]
        self._emit_waits(e, need)


def build(stage=99, dbg=()):
    nc = bass.Bass("TRN2", target_bir_lowering=False)

    def din(name, shape, dt=F32):
        return nc.dram_tensor(name, list(shape), dt, kind="ExternalInput").ap()

    def dscr(name, shape, dt):
        return nc.dram_tensor(name, list(shape), dt).ap()

    x_tm = din("x_tm", [S, D])
    xT = din("xT", [D, S])
    w_in = din("w_in", [D, INC])
    w_out = din("w_out", [D, D])
    w_gate = din("w_gate", [NE, D, HID])
    w_up = din("w_up", [NE, D, HID])
    w_down = din("w_down", [NE, HID, D])
    pos_tm = din("pos_tm", [128, NT], I32)
    ln1_t = din("ln1_t", [128, 16])
    conv_wt = din("conv_wt", [128, 12, 4])
    conv_bt = din("conv_bt", [128, 12])
    dt_bias = din("dt_bias", [16])
    a_log = din("a_log", [16])
    d_skip = din("d_skip", [16])
    ssd_norm_w = din("ssd_norm_w", [1024])
    q_norm_w = din("q_norm_w", [64])
    k_norm_w = din("k_norm_w", [64])
    lq1 = din("lq1", [64]); lk1 = din("lk1", [64]); lq2 = din("lq2", [64]); lk2 = din("lk2", [64])
    subln_w = din("subln_w", [128])
    ln2_w = din("ln2_w", [D])
    w_r = din("w_r", [D, 36])
    b_r = din("b_r", [36])
    c_ident = din("c_ident", [128, 128])
    c_utri = din("c_utri", [128, 128])
    c_ustrict = din("c_ustrict", [128, 128])
    c_negmask4 = din("c_negmask4", [128, 512])
    c_ehsel = din("c_ehsel", [16, 16, 128])
    c_invf = din("c_invf", [128, 8])
    c_ebase = din("c_ebase", [128, NE])

    out = nc.dram_tensor("out", [S, D], F32, kind="ExternalOutput").ap()
    dbg_out = {}
    for name, shape in dbg:
        dbg_out[name] = nc.dram_tensor("d_" + name, list(shape), F32, kind="ExternalOutput").ap()

    qT_scr = dscr("qT_scr", [128, 8, S], BF16)
    kT_scr = dscr("kT_scr", [128, 8, S], BF16)
    v_scr = dscr("v_scr", [S, 1024], BF16)
    zs_scr = dscr("zs_scr", [S, 1024], F32)
    ycat_scr = dscr("ycat_scr", [128, 16, S], BF16)
    h_scr = dscr("h_scr", [S, D], F32)
    xg_scr = dscr("xg_scr", [NE * CAP + 128, D], BF16)
    ys_scr = dscr("ys_scr", [NE * CAP + 128, D], F32)

    with contextlib.ExitStack() as es:
        kb = KB(nc, es)

        def sbt(st, name, shape, dt):
            return st.enter_context(nc.sbuf_tensor(name, list(shape), dt))

        PB = [es.enter_context(nc.psum_tensor(f"pb{k}", [128, 512], F32)) for k in range(8)]

        def pbb(k):
            return PB[k][:, :].bitcast(BF16)

        def P(k):
            return ('ps', k)

        def mm(outap, lhsT, rhs, start, stop, reads, writes, inc=True, sgc=False):
            return kb.op('pe', lambda e: e.matmul(outap, lhsT=lhsT, rhs=rhs, start=start, stop=stop, skip_group_check=sgc),
                         reads=reads, writes=writes, inc=inc)

        def tr(outap, in_, ident, reads, writes, inc=True):
            return kb.op('pe', lambda e: e.transpose(out=outap, in_=in_, identity=ident),
                         reads=reads, writes=writes, inc=inc)

        def act(outap, in_, func, reads, writes, bias=None, scale=None, accum=None):
            kw = {}
            if bias is not None:
                kw['bias'] = bias
            if scale is not None:
                kw['scale'] = scale
            if accum is not None:
                kw['accum_out'] = accum
            return kb.op('act', lambda e: e.activation(out=outap, in_=in_, func=func, **kw), reads=reads, writes=writes)

        def tt(outap, in0, in1, op, reads, writes, e='dve'):
            return kb.op(e, lambda g: g.tensor_tensor(out=outap, in0=in0, in1=in1, op=op), reads=reads, writes=writes)

        def ts(outap, in0, s1, s2, op0, op1, reads, writes, e='dve'):
            if op1 is None:
                return kb.op(e, lambda g: g.tensor_scalar(out=outap, in0=in0, scalar1=s1, scalar2=None, op0=op0),
                             reads=reads, writes=writes)
            return kb.op(e, lambda g: g.tensor_scalar(out=outap, in0=in0, scalar1=s1, scalar2=s2, op0=op0, op1=op1),
                         reads=reads, writes=writes)

        def stt(outap, in0, scalar, in1, op0, op1, reads, writes, e='dve'):
            return kb.op(e, lambda g: g.scalar_tensor_tensor(out=outap, in0=in0, scalar=scalar, in1=in1, op0=op0, op1=op1),
                         reads=reads, writes=writes)

        def red(outap, in_, op, reads, writes):
            return kb.op('dve', lambda g: g.tensor_reduce(out=outap, in_=in_, axis=AX.X, op=op), reads=reads, writes=writes)

        def cp(outap, in_, reads, writes, e='dve'):
            if e == 'act':
                return kb.op('act', lambda g: g.copy(out=outap, in_=in_), reads=reads, writes=writes)
            return kb.op(e, lambda g: g.tensor_copy(out=outap, in_=in_), reads=reads, writes=writes)

        eps_t = sbt(es, "eps_t", [128, 1], F32)
        kb.op('dve', lambda g: g.memset(eps_t[:], EPS), writes=["eps_t"])

        def rsqrt(outap, in_, mean_scale, reads, writes):
            act(outap, in_, AF.Ln, list(reads) + ["eps_t"], writes, bias=eps_t[:], scale=mean_scale)
            act(outap, outap, AF.Exp, writes, writes, scale=-0.5)

        cst = es
        names = []

        def cload(name, shape, src, dt=F32, cast=False):
            t = sbt(cst, name, shape, dt)
            kb.dma('pool' if cast else 'sp', out=t[:], in_=src, writes=[name], semkey='ldc_p' if cast else 'ldc_s')
            names.append((name, cast))
            return t

        ident_f = cload("ident_f", [128, 128], c_ident)
        ident_b = cload("ident_b", [128, 128], c_ident, BF16, True)
        utri_f = cload("utri_f", [128, 128], c_utri)
        utri_b = cload("utri_b", [128, 128], c_utri, BF16, True)
        ustrict_b = cload("ustrict_b", [128, 128], c_ustrict, BF16, True)
        negmask4_b = cload("negmask4_b", [128, 512], c_negmask4, BF16, True)
        invf = cload("invf", [128, 8], c_invf)
        ebase = cload("ebase", [128, NE], c_ebase)
        pos_i = cload("pos_i", [128, NT], pos_tm, I32)
        ln1 = cload("ln1", [128, 16], ln1_t)
        cw = cload("cw", [128, 12, 4], conv_wt)
        cbias = cload("cbias", [128, 12], conv_bt)
        dtb_bc = cload("dtb_bc", [128, 16], dt_bias.partition_broadcast(128))
        alog_bc = cload("alog_bc", [128, 16], a_log.partition_broadcast(128))
        dsk_bc = cload("dsk_bc", [128, 16], d_skip.partition_broadcast(128))
        wq_bc = cload("wq_bc", [128, 64], q_norm_w.partition_broadcast(128))
        wk_bc = cload("wk_bc", [128, 64], k_norm_w.partition_broadcast(128))
        l4 = sbt(cst, "l4", [128, 4, 64], F32)
        for n_, src in enumerate((lq1, lk1, lq2, lk2)):
            kb.dma('sp', out=l4[:, n_, :], in_=src.partition_broadcast(128), writes=["l4"], semkey='ldc_s')
        names.append(("l4", False))
        subw_bc = cload("subw_bc", [128, 128], subln_w.partition_broadcast(128))
        br_bc = cload("br_bc", [128, 36], b_r.partition_broadcast(128))
        kb.regroup([n for n, c in names if not c], 'ldc_s')
        kb.regroup([n for n, c in names if c], 'ldc_p')

        ones_f = sbt(cst, "ones_f", [128, 128], F32)
        kb.op('dve', lambda g: g.memset(ones_f[:], 1.0), writes=["ones_f"])
        ones_b = sbt(cst, "ones_b", [128, 128], BF16)
        kb.op('dve', lambda g: g.memset(ones_b[:], 1.0), writes=["ones_b"])

        A_bc = sbt(cst, "A_bc", [128, 16], F32)
        act(A_bc[:], alog_bc[:], AF.Exp, ["alog_bc"], ["A_bc"])
        ts(A_bc[:], A_bc[:], -1.0, None, ALU.mult, None, ["A_bc"], ["A_bc"])
        lam = sbt(cst, "lam", [128, 4], F32)
        lprod = sbt(cst, "lprod", [128, 2, 64], F32)
        tt(lprod[:, 0, :], l4[:, 0, :], l4[:, 1, :], ALU.mult, ["l4"], ["lprod"])
        tt(lprod[:, 1, :], l4[:, 2, :], l4[:, 3, :], ALU.mult, ["l4", "lprod"], ["lprod"])
        lsum = sbt(cst, "lsum", [128, 2], F32)
        red(lsum[:], lprod[:], ALU.add, ["lprod"], ["lsum"])
        act(lsum[:], lsum[:], AF.Exp, ["lsum"], ["lsum"])
        tt(lam[:, 0:1], lsum[:, 0:1], lsum[:, 1:2], ALU.subtract, ["lsum"], ["lam"])
        ts(lam[:, 0:1], lam[:, 0:1], LAM_INIT, None, ALU.add, None, ["lam"], ["lam"])
        ts(lam[:, 1:2], lam[:, 0:1], -1.0, None, ALU.mult, None, ["lam"], ["lam"])
        ts(wq_bc[:], wq_bc[:], 0.125, None, ALU.mult, None, ["wq_bc"], ["wq_bc"])
        ts(subw_bc[:], subw_bc[:], 1.0 - LAM_INIT, None, ALU.mult, None, ["subw_bc"], ["subw_bc"])
        pos_f = sbt(cst, "pos_f", [128, NT], F32)
        cp(pos_f[:], pos_i[:], ["pos_i"], ["pos_f"])
        ang = sbt(cst, "ang", [128, NT, 8], F32)
        tt(ang[:], pos_f[:].unsqueeze(2).to_broadcast([128, NT, 8]), invf[:].unsqueeze(1).to_broadcast([128, NT, 8]),
           ALU.mult, ["pos_f", "invf"], ["ang"])
        sin_t = sbt(cst, "sin_t", [128, NT, 8], F32)
        cos_t = sbt(cst, "cos_t", [128, NT, 8], F32)
        angi = sbt(cst, "angi", [128, NT, 8], I32)
        angk = sbt(cst, "angk", [128, NT, 8], F32)
        for (tab, nm, off) in ((sin_t, "sin_t", 0.0), (cos_t, "cos_t", 0.5 * PI)):
            ts(tab[:], ang[:], off, None, ALU.add, None, ["ang"], [nm])
            ts(angk[:], tab[:], 1.0 / (2 * PI), None, ALU.mult, None, [nm], ["angk"])
            cp(angi[:], angk[:], ["angk"], ["angi"])
            cp(angk[:], angi[:], ["angi"], ["angk"])
            stt(tab[:], angk[:], -2 * PI, tab[:], ALU.mult, ALU.add, ["angk", nm], [nm])
            ts(tab[:], tab[:], -PI, PI, ALU.max, ALU.min, [nm], [nm])
            act(tab[:], tab[:], AF.Sin, [nm], [nm])

        rstd1 = sbt(cst, "rstd1", [128, NT], F32)

        with contextlib.ExitStack() as sx:
            xTb = sbt(sx, "xTb", [128, 16, S], BF16)
            for c in range(16):
                kb.dma('pool', out=xTb[:, c, :], in_=xT[c * 128:(c + 1) * 128, :], writes=[("xTb", c)], semkey='ld_xT')
            kb.regroup([("xTb", c) for c in range(16)], 'ld_xT')
            for c in range(16):
                ts(xTb[:, c, :], xTb[:, c, :], ln1[:, c:c + 1], None, ALU.mult, None, [("xTb", c), "ln1"], [("xTb", c)])
            XT = [("xTb", c) for c in range(16)]

            with contextlib.ExitStack() as s1:
                xf = [sbt(s1, f"xf{k}", [128, D], F32) for k in range(2)]
                junk = sbt(s1, "junk", [128, D], BF16)
                ss1 = sbt(s1, "ss1", [128, NT], F32)
                kb.op('dve', lambda g: g.memset(ss1[:], 0.0), writes=["ss1"])
                for i in range(NT):
                    kb.dma('sp', out=xf[i % 2][:], in_=x_tm[i * 128:(i + 1) * 128, :], writes=[f"xf{i%2}"], semkey=f'ld_xf{i%2}')
                    act(junk[:], xf[i % 2][:], AF.Square, [f"xf{i%2}", "ss1"], ["junk", "ss1"], accum=ss1[:, i:i + 1])
                rsqrt(rstd1[:], ss1[:], 1.0 / D, ["ss1"], ["rstd1"])
                kb.barrier()

            if "rstd1" in dbg_out:
                kb.dma('sp', out=dbg_out["rstd1"], in_=rstd1[:], reads=["rstd1"], writes=["dbg_rstd1"], semkey='dbg')

            def proj_pass(col0, epilogue, tagp):
                with contextlib.ExitStack() as sp_:
                    W = sbt(sp_, "Wp" + tagp, [128, 16, 1024], BF16)
                    for half in range(2):
                        kb.dma('pool', out=W[:, :, half * 512:(half + 1) * 512],
                               in_=w_in[:, col0 + half * 512: col0 + (half + 1) * 512].rearrange("(c p) n -> p c n", p=128),
                               writes=[("Wp", half)], semkey=f'ld_Wp{half}')
                    for i in range(NT):
                        banks = []
                        for half in range(2):
                            b = half + 2 * (i % 2)
                            banks.append(b)
                            for dc in range(16):
                                mm(PB[b][:, :], xTb[:, dc, i * 128:(i + 1) * 128], W[:, dc, half * 512:(half + 1) * 512],
                                   dc == 0, dc == 15, [("xTb", dc), ("Wp", half)], [P(b)], inc=(dc == 15))
                        epilogue(i, banks, sp_)
                    kb.barrier()

            if stage >= 2:
                def qk_epilogue_factory(wbc, wbc_name, scr, tg):
                    state = {}

                    def ep(i, banks, st):
                        if not state:
                            for k in range(2):
                                state['qf', k] = sbt(st, f"qf{tg}{k}", [128, 1024], F32)
                                state['sq', k] = sbt(st, f"sq{tg}{k}", [128, 1024], F32)
                                state['ss', k] = sbt(st, f"ss{tg}{k}", [128, 16], F32)
                                state['rot', k] = sbt(st, f"rot{tg}{k}", [128, 4, 16, 8], F32)
                                state['qb', k] = sbt(st, f"qb{tg}{k}", [128, 1024], BF16)
                                state['qt', k] = sbt(st, f"qt{tg}{k}", [128, 8, 128], BF16)
                        k = i % 2
                        qf, sq, ssq, rot, qb, qt = (state[n, k] for n in ('qf', 'sq', 'ss', 'rot', 'qb', 'qt'))
                        QF, SQ, SS, ROT, QB, QT = (f"{n}{k}" for n in ('qf', 'sq', 'ssq', 'rot', 'qb', 'qt'))
                        for half in range(2):
                            act(qf[:, half * 512:(half + 1) * 512], PB[banks[half]][:, :], AF.Copy,
                                [P(banks[half]), "rstd1"], [QF], scale=rstd1[:, i:i + 1])
                        tt(sq[:], qf[:], qf[:], ALU.mult, [QF], [SQ], e='pool')
                        red(ssq[:], sq[:].rearrange("p (b d) -> p b d", d=64), ALU.add, [SQ], [SS])
                        rsqrt(ssq[:], ssq[:], 1.0 / 64, [SS], [SS])
                        qf3 = qf[:].rearrange("p (b d) -> p b d", d=64)
                        tt(qf3, qf3, ssq[:].unsqueeze(2).to_broadcast([128, 16, 64]), ALU.mult, [QF, SS], [QF])
                        tt(qf3, qf3, wbc[:].unsqueeze(1).to_broadcast([128, 16, 64]), ALU.mult, [QF, wbc_name], [QF])
                        cosb = cos_t[:, i, :].unsqueeze(1).to_broadcast([128, 16, 8])
                        sinb = sin_t[:, i, :].unsqueeze(1).to_broadcast([128, 16, 8])
                        t1 = qf3[:, :, 0:8]
                        t2 = qf3[:, :, 8:16]
                        tt(rot[:, 0], t1, cosb, ALU.mult, [QF, "cos_t"], [ROT])
                        tt(rot[:, 1], t2, sinb, ALU.mult, [QF, "sin_t", ROT], [ROT])
                        tt(rot[:, 2], t2, cosb, ALU.mult, [QF, "cos_t", ROT], [ROT])
                        tt(rot[:, 3], t1, sinb, ALU.mult, [QF, "sin_t", ROT], [ROT])
                        qb3 = qb[:].rearrange("p (b d) -> p b d", d=64)
                        cp(qb3[:, :, 16:64], qf3[:, :, 16:64], [QF], [QB], e='pool')
                        tt(qb3[:, :, 0:8], rot[:, 0], rot[:, 1], ALU.subtract, [ROT, QB], [QB])
                        tt(qb3[:, :, 8:16], rot[:, 2], rot[:, 3], ALU.add, [ROT, QB], [QB])
                        if i >= 1:
                            ep_b(i - 1)
                        if i == NT - 1:
                            ep_b(i)

                    def ep_b(i):
                        k = i % 2
                        qb, qt = state['qb', k], state['qt', k]
                        QB, QT = f"qb{k}", f"qt{k}"
                        pb = 4 + (i % 2)
                        for hd in range(8):
                            tr(pbb(pb)[:, hd * 128:(hd + 1) * 128], qb[:, hd * 128:(hd + 1) * 128], ident_b[:],
                               [QB, "ident_b"], [P(pb)], inc=(hd == 7))
                        cp(qt[:].rearrange("p h t -> p (h t)"), pbb(pb), [P(pb)], [QT], e='act')
                        kb.dma('sp', out=scr[:, :, i * 128:(i + 1) * 128], in_=qt[:], reads=[QT], writes=[("scr" + tg, i)],
                               semkey=f'st_{tg}{k}')
                    return ep

                proj_pass(2576, qk_epilogue_factory(wq_bc, "wq_bc", qT_scr, "q"), "q")
                proj_pass(3600, qk_epilogue_factory(wk_bc, "wk_bc", kT_scr, "k"), "k")

                vstate = {}

                def v_ep(i, banks, st):
                    if not vstate:
                        vstate['vb'] = [sbt(st, f"vb{k}", [128, 1024], BF16) for k in range(2)]
                    vb = vstate['vb'][i % 2]
                    for half in range(2):
                        act(vb[:, half * 512:(half + 1) * 512], PB[banks[half]][:, :], AF.Copy,
                            [P(banks[half]), "rstd1"], [f"vb{i%2}"], scale=rstd1[:, i:i + 1])
                    kb.dma('sp', out=v_scr[i * 128:(i + 1) * 128, :], in_=vb[:], reads=[f"vb{i%2}"], writes=[("scrv", i)],
                           semkey=f'st_v{i%2}')
                proj_pass(4624, v_ep, "v")

            if stage >= 3:
                with contextlib.ExitStack() as s3:
                    xbcT = sbt(s3, "xbcT", [128, 12, S], BF16)
                    rstd_bc = sbt(s3, "rstd_bc", [128, S], F32)
                    with contextlib.ExitStack() as s31:
                        dg = [sbt(s31, f"dg{k}", [128, 128], F32) for k in range(2)]
                        for i in range(NT):
                            ts(dg[i % 2][:], ident_f[:], rstd1[:, i:i + 1], None, ALU.mult, None, ["ident_f", "rstd1"], [f"dg{i%2}"])
                            b = i // 4
                            mm(PB[b][:, (i % 4) * 128:(i % 4 + 1) * 128], ones_f[:], dg[i % 2][:], True, True,
                               ["ones_f", f"dg{i%2}"], [P(b)])
                        for b in range(4):
                            cp(rstd_bc[:, b * 512:(b + 1) * 512], PB[b][:, :], [P(b)], ["rstd_bc"])
                        Wx = [sbt(s31, f"Wx{k}", [128, 16, 512], BF16) for k in range(2)]
                        ub = [sbt(s31, f"ub{k}", [128, S + 3], F32) for k in range(1)]
                        acc = [sbt(s31, f"acc{k}", [128, S], F32) for k in range(1)]
                        for k in range(1):
                            kb.op('dve', lambda g: g.memset(ub[k][:, 0:3], 0.0), writes=[f"ub{k}"])
                        for blk in range(3):
                            kb.dma('pool', out=Wx[blk % 2][:],
                                   in_=w_in[:, 1024 + blk * 512: 1024 + (blk + 1) * 512].rearrange("(c p) n -> p c n", p=128),
                                   writes=[f"Wx{blk%2}"], semkey=f'ld_Wx{blk%2}')
                            for jj in range(4):
                                j = blk * 4 + jj
                                u = ub[0]
                                a = acc[0]
                                for tb in range(4):
                                    b = 4 + tb
                                    for dc in range(16):
                                        mm(PB[b][:, :], Wx[blk % 2][:, dc, jj * 128:(jj + 1) * 128], xTb[:, dc, tb * 512:(tb + 1) * 512],
                                           dc == 0, dc == 15, [f"Wx{blk%2}", ("xTb", dc)], [P(b)], inc=(dc == 15))
                                    tt(u[:, 3 + tb * 512: 3 + (tb + 1) * 512], PB[b][:, :], rstd_bc[:, tb * 512:(tb + 1) * 512], ALU.mult,
                                       [P(b), "rstd_bc"], ["ub0"])
                                act(a[:], u[:, 3:3 + S], AF.Identity, ["ub0", "cw", "cbias"], ["acc0"],
                                    bias=cbias[:, j:j + 1], scale=cw[:, j, 3:4])
                                for k in range(3):
                                    stt(a[:], u[:, k:k + S], cw[:, j, k:k + 1], a[:], ALU.mult, ALU.add,
                                        ["ub0", "cw", "acc0"], ["acc0"], e='dve')
                                act(xbcT[:, j, :], a[:], AF.Silu, ["acc0"], [("xbcT", j)])
                        kb.barrier()
                    if "xbcT" in dbg_out:
                        with contextlib.ExitStack() as sd:
                            tmpf = sbt(sd, "tmpf", [128, S], F32)
                            for j in range(12):
                                cp(tmpf[:], xbcT[:, j, :], [("xbcT", j)], ["tmpf"])
                                kb.dma('sp', out=dbg_out["xbcT"][j * 128:(j + 1) * 128, :], in_=tmpf[:], reads=["tmpf"], writes=["dbgx"], semkey='dbg')
                            kb.barrier()

                    dtv = sbt(s3, "dtv", [128, 256], F32)
                    sd_t = sbt(s3, "sd_t", [128, 256], F32)
                    E_t = sbt(s3, "E_t", [128, 256], F32)
                    nacs = sbt(s3, "nacs", [128, 256], F32)
                    cd_bc = sbt(s3, "cd_bc", [128, 256], F32)
                    acsT = sbt(s3, "acsT", [16, S], F32)
                    with contextlib.ExitStack() as s32:
                        Wdt = sbt(s32, "Wdt", [128, 16, 16], BF16)
                        kb.dma('pool', out=Wdt[:], in_=w_in[:, 2560:2576].rearrange("(c p) n -> p c n", p=128), writes=["Wdt"], semkey='ld_Wdt')
                        for i in range(NT):
                            for dc in range(16):
                                mm(PB[0][:, i * 16:(i + 1) * 16], xTb[:, dc, i * 128:(i + 1) * 128], Wdt[:, dc, :], dc == 0, dc == 15,
                                   [("xTb", dc), "Wdt"], [P(0)], inc=(dc == 15))
                        t1 = sbt(s32, "t1", [128, 256], F32)
                        t2 = sbt(s32, "t2", [128, 256], F32)
                        a_tok = sbt(s32, "a_tok", [128, 256], F32)
                        t13 = t1[:].rearrange("p (i h) -> p i h", h=16)
                        tt(t13, PB[0][:, 0:256].rearrange("p (i h) -> p i h", h=16), rstd1[:].unsqueeze(2).to_broadcast([128, NT, 16]),
                           ALU.mult, [P(0), "rstd1"], ["t1"])
                        tt(t13, t13, dtb_bc[:].unsqueeze(1).to_broadcast([128, NT, 16]), ALU.add, ["t1", "dtb_bc"], ["t1"])
                        stt(t2[:], t1[:], -1.0, t1[:], ALU.mult, ALU.max, ["t1"], ["t2"])
                        act(t2[:], t2[:], AF.Exp, ["t2"], ["t2"], scale=-1.0)
                        ts(t2[:], t2[:], 1.0, None, ALU.add, None, ["t2"], ["t2"])
                        act(t2[:], t2[:], AF.Ln, ["t2"], ["t2"])
                        ts(t1[:], t1[:], 0.0, None, ALU.max, None, ["t1"], ["t1"])
                        tt(dtv[:], t1[:], t2[:], ALU.add, ["t1", "t2"], ["dtv"])
                        tt(a_tok[:].rearrange("p (i h) -> p i h", h=16), dtv[:].rearrange("p (i h) -> p i h", h=16),
                           A_bc[:].unsqueeze(1).to_broadcast([128, NT, 16]), ALU.mult, ["dtv", "A_bc"], ["a_tok"])
                        mm(PB[1][:, 0:256], utri_f[:], a_tok[:], True, True, ["utri_f", "a_tok"], [P(1)])
                        mm(PB[2][:, 0:256], ones_f[:], a_tok[:], True, True, ["ones_f", "a_tok"], [P(2)])
                        for i in range(NT):
                            b = 4 + i // 4
                            mm(PB[b][0:16, (i % 4) * 128:(i % 4 + 1) * 128], a_tok[:, i * 16:(i + 1) * 16], utri_f[:], True, True,
                               ["a_tok", "utri_f"], [P(b)])
                        for b in range(4):
                            cp(acsT[:, b * 512:(b + 1) * 512], PB[4 + b][0:16, :], [P(4 + b)], ["acsT"])
                        act(E_t[:], PB[1][:, 0:256], AF.Exp, [P(1)], ["E_t"])
                        ts(nacs[:], PB[1][:, 0:256], -1.0, None, ALU.mult, None, [P(1)], ["nacs"])
                        act(cd_bc[:], PB[2][:, 0:256], AF.Exp, [P(2)], ["cd_bc"])
                        tt(t1[:], PB[2][:, 0:256], nacs[:], ALU.add, [P(2), "nacs"], ["t1"])
                        act(t1[:], t1[:], AF.Exp, ["t1"], ["t1"])
                        tt(sd_t[:], t1[:], dtv[:], ALU.mult, ["t1", "dtv"], ["sd_t"])
                        kb.barrier()

                    zst = {}

                    def z_ep(i, banks, st):
                        if not zst:
                            zst['z'] = [sbt(st, f"zsb{k}", [128, 1024], F32) for k in range(2)]
                        zb = zst['z'][i % 2]
                        for half in range(2):
                            act(zb[:, half * 512:(half + 1) * 512], PB[banks[half]][:, :], AF.Silu,
                                [P(banks[half]), "rstd1"], [f"zsb{i%2}"], scale=rstd1[:, i:i + 1])
                        kb.dma('sp', out=zs_scr[i * 128:(i + 1) * 128, :], in_=zb[:], reads=[f"zsb{i%2}"], writes=[("zs_scr", i)],
                               semkey=f'st_z{i%2}')
                    proj_pass(0, z_ep, "z")

                    with contextlib.ExitStack() as s34:
                        ehsel = sbt(s34, "ehsel", [16, 16, 128], F32)
                        kb.dma('sp', out=ehsel[:], in_=c_ehsel, writes=["ehsel"], semkey='ld_c3')
                        normw_bc = sbt(s34, "normw_bc", [128, 1024], F32)
                        kb.dma('sp', out=normw_bc[:], in_=ssd_norm_w.partition_broadcast(128), writes=["normw_bc"], semkey='ld_c3')
                        kb.regroup(["ehsel", "normw_bc"], 'ld_c3')
                        xdt = sbt(s34, "xdt", [128, 1024], BF16)
                        xdts = sbt(s34, "xdts", [128, 1024], BF16)
                        xsD = sbt(s34, "xsD", [128, 1024], F32)
                        B_tm = sbt(s34, "B_tm", [128, 256], BF16)
                        cbT = sbt(s34, "cbT", [128, 256], BF16)
                        decT = [sbt(s34, f"decT{k}", [128, 512], BF16) for k in range(2)]
                        MT = [sbt(s34, f"MT{k}", [128, 512], BF16) for k in range(2)]
                        ytmp = sbt(s34, "ytmp", [128, 512], F32)
                        y = sbt(s34, "y", [128, 1024], F32)
                        prev_f = sbt(s34, "prev_f", [128, 1024], F32)
                        prev_b = sbt(s34, "prev_b", [128, 1024], BF16)
                        zsb = sbt(s34, "zsb", [128, 1024], F32)
                        sq = sbt(s34, "sqy", [128, 1024], F32)
                        ss2 = sbt(s34, "ss2", [128, 2], F32)
                        ynbs = [sbt(s34, f"ynb{k}", [128, 1024], BF16) for k in range(2)]
                        ycTs = [sbt(s34, f"ycT{k}", [128, 8, 128], BF16) for k in range(2)]

                        def gate_b(i):
                            k = i % 2
                            tsl_ = slice(i * 128, (i + 1) * 128)
                            for j in range(8):
                                tr(pbb(0)[:, j * 128:(j + 1) * 128], ynbs[k][:, j * 128:(j + 1) * 128], ident_b[:], [f"ynb{k}", "ident_b"], [P(0)], inc=(j == 7))
                            cp(ycTs[k][:].rearrange("p j t -> p (j t)"), pbb(0), [P(0)], [f"ycT{k}"], e='act')
                            kb.dma('sp', out=ycat_scr[:, 0:8, tsl_], in_=ycTs[k][:], reads=[f"ycT{k}"], writes=[("ycat", 0, i)], semkey=f'st_yc{k}')

                        for i in range(NT):
                            ynb = ynbs[i % 2]
                            tsl = slice(i * 128, (i + 1) * 128)
                            kb.dma('sp', out=zsb[:], in_=zs_scr[tsl, :], reads=[("zs_scr", i)], writes=["zsb"], semkey='ld_zs')
                            for j in range(8):
                                tr(pbb(0)[:, j * 128:(j + 1) * 128], xbcT[:, j, tsl], ident_b[:], [("xbcT", j), "ident_b"], [P(0)], inc=(j == 7))
                            ps3 = pbb(0).rearrange("p (h d) -> p h d", d=64)
                            tt(xdt[:].rearrange("p (h d) -> p h d", d=64), ps3, dtv[:, i * 16:(i + 1) * 16].unsqueeze(2).to_broadcast([128, 16, 64]),
                               ALU.mult, [P(0), "dtv"], ["xdt"])
                            tt(xdts[:].rearrange("p (h d) -> p h d", d=64), ps3, sd_t[:, i * 16:(i + 1) * 16].unsqueeze(2).to_broadcast([128, 16, 64]),
                               ALU.mult, [P(0), "sd_t"], ["xdts"])
                            tt(xsD[:].rearrange("p (h d) -> p h d", d=64), ps3, dsk_bc[:].unsqueeze(2).to_broadcast([128, 16, 64]),
                               ALU.mult, [P(0), "dsk_bc"], ["xsD"])
                            for g in range(2):
                                tr(pbb(1)[:, g * 128:(g + 1) * 128], xbcT[:, 8 + g, tsl], ident_b[:], [("xbcT", 8 + g), "ident_b"], [P(1)], inc=(g == 1))
                            cp(B_tm[:], pbb(1)[:, 0:256], [P(1)], ["B_tm"], e='act')
                            for g in range(2):
                                mm(PB[2][:, g * 128:(g + 1) * 128], xbcT[:, 8 + g, tsl], xbcT[:, 10 + g, tsl], True, True,
                                   [("xbcT", 8 + g), ("xbcT", 10 + g)], [P(2)], inc=(g == 1))
                            cp(cbT[:], PB[2][:, 0:256], [P(2)], ["cbT"], e='act')
                            for hq in range(4):
                                g = hq // 2
                                rb = 3 + (hq % 2)
                                for q in range(4):
                                    h = hq * 4 + q
                                    mm(PB[rb][:, q * 128:(q + 1) * 128], ehsel[:, h, :], acsT[:, tsl], q == 0, False, ["ehsel", "acsT"], [P(rb)],
                                       inc=False, sgc=True)
                                mm(PB[rb][:, :], ident_b[:], negmask4_b[:], False, True, ["ident_b", "negmask4_b"], [P(rb)], sgc=True)
                                dT = decT[hq % 2]
                                for q in range(4):
                                    h = hq * 4 + q
                                    act(dT[:, q * 128:(q + 1) * 128], PB[rb][:, q * 128:(q + 1) * 128], AF.Exp, [P(rb), "nacs"], [f"decT{hq%2}"],
                                        bias=nacs[:, i * 16 + h: i * 16 + h + 1])
                                tt(MT[hq % 2][:].rearrange("p (q l) -> p q l", q=4), dT[:].rearrange("p (q l) -> p q l", q=4),
                                   cbT[:, g * 128:(g + 1) * 128].unsqueeze(1).to_broadcast([128, 4, 128]), ALU.mult,
                                   [f"decT{hq%2}", "cbT"], [f"MT{hq%2}"])
                                for q in range(4):
                                    h = hq * 4 + q
                                    mm(PB[5 + g][:, (h % 8) * 64:(h % 8 + 1) * 64], MT[hq % 2][:, q * 128:(q + 1) * 128], xdt[:, h * 64:(h + 1) * 64],
                                       True, True, [f"MT{hq%2}", "xdt"], [P(5 + g)], inc=(q == 3))
                            for g in range(2):
                                gs = slice(g * 512, (g + 1) * 512)
                                if i > 0:
                                    mm(PB[7][:, :], xbcT[:, 10 + g, tsl], prev_b[:, gs], True, True, [("xbcT", 10 + g), "prev_b"], [P(7)])
                                    tt(ytmp[:].rearrange("p (h d) -> p h d", d=64), PB[7][:, :].rearrange("p (h d) -> p h d", d=64),
                                       E_t[:, i * 16 + g * 8: i * 16 + g * 8 + 8].unsqueeze(2).to_broadcast([128, 8, 64]), ALU.mult,
                                       [P(7), "E_t"], ["ytmp"])
                                    tt(y[:, gs], PB[5 + g][:, :], ytmp[:], ALU.add, [P(5 + g), "ytmp"], ["y"])
                                else:
                                    cp(y[:, gs], PB[5 + g][:, :], [P(5 + g)], ["y"])
                                if i < NT - 1:
                                    mm(PB[7][:, :], B_tm[:, g * 128:(g + 1) * 128], xdts[:, gs], True, True, ["B_tm", "xdts"], [P(7)])
                                    if i > 0:
                                        tt(prev_f[:, gs].rearrange("p (h d) -> p h d", d=64), prev_f[:, gs].rearrange("p (h d) -> p h d", d=64),
                                           cd_bc[:, i * 16 + g * 8: i * 16 + g * 8 + 8].unsqueeze(2).to_broadcast([128, 8, 64]), ALU.mult,
                                           ["prev_f", "cd_bc"], ["prev_f"])
                                        tt(prev_f[:, gs], prev_f[:, gs], PB[7][:, :], ALU.add, ["prev_f", P(7)], ["prev_f"])
                                    else:
                                        cp(prev_f[:, gs], PB[7][:, :], [P(7)], ["prev_f"])
                                    cp(prev_b[:, gs], prev_f[:, gs], ["prev_f"], ["prev_b"], e='act')
                            tt(y[:], y[:], xsD[:], ALU.add, ["y", "xsD"], ["y"], e='pool')
                            tt(y[:], y[:], zsb[:], ALU.mult, ["y", "zsb"], ["y"])
                            tt(sq[:], y[:], y[:], ALU.mult, ["y"], ["sqy"])
                            red(ss2[:], sq[:].rearrange("p (g c) -> p g c", g=2), ALU.add, ["sqy"], ["ss2"])
                            rsqrt(ss2[:], ss2[:], 1.0 / 512, ["ss2"], ["ss2"])
                            tt(y[:].rearrange("p (g c) -> p g c", g=2), y[:].rearrange("p (g c) -> p g c", g=2),
                               ss2[:].unsqueeze(2).to_broadcast([128, 2, 512]), ALU.mult, ["y", "ss2"], ["y"])
                            tt(ynb[:], y[:], normw_bc[:], ALU.mult, ["y", "normw_bc"], [f"ynb{i%2}"])
                            if i >= 1:
                                gate_b(i - 1)
                            if i == NT - 1:
                                gate_b(i)
                        kb.barrier()
                    kb.barrier()
            kb.barrier()

        if stage >= 4:
            with contextlib.ExitStack() as s4:
                qT = sbt(s4, "qT", [128, 8, S], BF16)
                kTz = [sbt(s4, f"kTz{c}", [128, 8, S], BF16) for c in range(2)]
                V = sbt(s4, "V", [128, NT, 1024], BF16)
                subw_col = sbt(s4, "subw_col", [128, 1], F32)
                kb.dma('sp', out=subw_col[:], in_=subln_w.rearrange("(p o) -> p o", o=1), writes=["subw_col"], semkey='ld_c4')
                ts(subw_col[:], subw_col[:], 1.0 - LAM_INIT, None, ALU.mult, None, ["subw_col"], ["subw_col"])
                for c in range(2):
                    oth = slice((1 - c) * 64, (2 - c) * 64)
                    kb.op('pool', lambda g: g.memset(kTz[c][oth, :, :], 0.0), writes=[f"kTz{c}"])
                for hd in range(8):
                    kb.dma('sp', out=qT[:, hd, :], in_=qT_scr[:, hd, :], reads=[("scrq", i) for i in range(NT)], writes=["qT"], semkey='ld_q')
                    for c in range(2):
                        cs = slice(c * 64, (c + 1) * 64)
                        kb.dma('sp', out=kTz[c][cs, hd, :], in_=kT_scr[cs, hd, :], reads=[("scrk", i) for i in range(NT)], writes=[f"kTz{c}"], semkey='ld_k')
                for i in range(NT):
                    kb.dma('sp', out=V[:, i, :], in_=v_scr[i * 128:(i + 1) * 128, :], reads=[("scrv", i)], writes=["V"], semkey='ld_v')
                kb.regroup(["qT"], 'ld_q'); kb.regroup(["kTz0", "kTz1"], 'ld_k'); kb.regroup(["V"], 'ld_v')
                NPT = 4
                pT = [sbt(s4, f"pT{k}", [128, 512], BF16) for k in range(NPT)]
                cR = [sbt(s4, f"cR{c}", [128, 512], F32) for c in range(2)]
                cO = [sbt(s4, f"cO{c}", [128, 512], F32) for c in range(2)]
                o0 = sbt(s4, "o0", [128, 512], F32)
                sqo = sbt(s4, "sqo", [128, 512], F32)
                rs = sbt(s4, "rs", [128, 512], F32)
                ycA = [sbt(s4, f"ycA{k}", [128, 512], BF16) for k in range(2)]
                SB = [0, 1, 2]
                steps = []
                blk = 0
                for hd in range(8):
                    for qb in range(4):
                        nkt = 4 * qb + 4
                        for c in range(2):
                            for kt in range(nkt):
                                steps.append(dict(hd=hd, qb=qb, c=c, kt=kt, nkt=nkt, blk=blk, last=(c == 1 and kt == nkt - 1)))
                        blk += 1

                def geom(st):
                    j = st['kt'] - 4 * st['qb']
                    off = max(j, 0) * 128
                    return j, off, 4 * st['qb'] * 128 + off, 512 - off

                def emit_S(i):
                    st = steps[i]
                    j, off, q0, nq = geom(st)
                    sbk = SB[i % 3]
                    mm(PB[sbk][:, 0:nq], kTz[st['c']][:, st['hd'], st['kt'] * 128:(st['kt'] + 1) * 128], qT[:, st['hd'], q0:q0 + nq],
                       True, True, [f"kTz{st['c']}", "qT"], [P(sbk)])

                def emit_rest(i):
                    st = steps[i]
                    j, off, q0, nq = geom(st)
                    sbk = SB[i % 3]
                    pk = i % NPT
                    c, kt, hd = st['c'], st['kt'], st['hd']
                    act(pT[pk][:, 0:nq], PB[sbk][:, 0:nq], AF.Exp, [P(sbk)], [f"pT{pk}"])
                    if j >= 0:
                        tt(pT[pk][:, 0:128], pT[pk][:, 0:128], utri_b[:], ALU.mult, [f"pT{pk}", "utri_b"], [f"pT{pk}"], e='dve')
                    mm(PB[3 + c][:, off:off + nq], V[:, kt, hd * 128:(hd + 1) * 128], pT[pk][:, 0:nq], kt == 0, kt == st['nkt'] - 1,
                       ["V", f"pT{pk}"], [P(3 + c)], inc=False, sgc=True)
                    mm(PB[5 + c][:, off:off + nq], ones_b[:], pT[pk][:, 0:nq], kt == 0, kt == st['nkt'] - 1,
                       ["ones_b", f"pT{pk}"], [P(5 + c)], sgc=True)

                def epiA(st):
                    act(cR[0][:], PB[5][:, :], AF.Ln, [P(5)], ["cR0"])
                    cp(cO[0][:], PB[3][:, :], [P(3)], ["cO0"])
                    act(cR[1][:], PB[6][:, :], AF.Ln, [P(6)], ["cR1"])
                    cp(cO[1][:], PB[4][:, :], [P(4)], ["cO1"])
                    for c in range(2):
                        act(cR[c][:], cR[c][:], AF.Exp, [f"cR{c}"], [f"cR{c}"], scale=-1.0)
                    for c in range(2):
                        tt(cO[c][:], cO[c][:], cR[c][:], ALU.mult, [f"cO{c}", f"cR{c}"], [f"cO{c}"])
                    stt(o0[:], cO[1][:], lam[:, 1:2], cO[0][:], ALU.mult, ALU.add, ["cO1", "lam", "cO0"], ["o0"])
                    tt(sqo[:], o0[:], o0[:], ALU.mult, ["o0"], ["sqo"])

                def epiB(st):
                    mm(PB[7][:, :], ones_f[:], sqo[:], True, True, ["ones_f", "sqo"], [P(7)])

                def epiC(st):
                    yk = st['blk'] % 2
                    act(rs[:], PB[7][:, :], AF.Ln, [P(7), "eps_t"], ["rs"], bias=eps_t[:], scale=1.0 / 128)
                    act(rs[:], rs[:], AF.Exp, ["rs"], ["rs"], scale=-0.5)
                    stt(ycA[yk][:], o0[:], subw_col[:, 0:1], rs[:], ALU.mult, ALU.mult, ["o0", "subw_col", "rs"], [f"ycA{yk}"])
                    kb.dma('sp', out=ycat_scr[:, 8 + st['hd'], st['qb'] * 512:(st['qb'] + 1) * 512], in_=ycA[yk][:], reads=[f"ycA{yk}"],
                           writes=[("ycat", 1, st['hd'], st['qb'])], semkey=f'st_ya{yk}')

                NS = len(steps)
                emit_S(0)
                emit_S(1)
                pend = []
                for i in range(NS):
                    emit_rest(i)
                    if i + 2 < NS:
                        emit_S(i + 2)
                    while pend and (pend[0][0] <= i or steps[i]['last']):
                        _, fn, st_ = pend.pop(0)
                        fn(st_)
                    if steps[i]['last']:
                        epiA(steps[i])
                        pend = [(i + 8, epiB, steps[i]), (i + 11, epiC, steps[i])]
                for _, fn, st_ in pend:
                    fn(st_)
                kb.barrier()

        if "ycat" in dbg_out:
            with contextlib.ExitStack() as sd:
                tb16 = sbt(sd, "tb16", [128, S], BF16)
                tmpf = sbt(sd, "tmpf2", [128, S], F32)
                for j in range(16):
                    kb.dma('sp', out=tb16[:], in_=ycat_scr[:, j, :], reads=[], writes=["tb16"], semkey='dbg2')
                    cp(tmpf[:], tb16[:], ["tb16"], ["tmpf2"])
                    kb.dma('sp', out=dbg_out["ycat"][j * 128:(j + 1) * 128, :], in_=tmpf[:], reads=["tmpf2"], writes=["dbgy"], semkey='dbg')
                kb.barrier()

        slots = sbt(es, "slots", [128, NT, 2], I32)
        wts = sbt(es, "wts", [128, NT, 2], F32)
        if stage >= 5:
            with contextlib.ExitStack() as s5:
                ycT_all = sbt(s5, "ycT_all", [128, 16, S], BF16)
                Wo = sbt(s5, "Wo", [128, 16, D], BF16)
                for j in range(16):
                    kb.dma('sp', out=ycT_all[:, j, :], in_=ycat_scr[:, j, :], writes=["ycT_all"], semkey='ld_yc')
                kb.regroup(["ycT_all"], 'ld_yc')
                for cb in range(4):
                    kb.dma('pool', out=Wo[:, :, cb * 512:(cb + 1) * 512],
                           in_=w_out[:, cb * 512:(cb + 1) * 512].rearrange("(c p) n -> p c n", p=128), writes=["Wo"], semkey='ld_Wo')
                kb.regroup(["Wo"], 'ld_Wo')
                ln2_bc = sbt(s5, "ln2_bc", [128, D], F32)
                kb.dma('sp', out=ln2_bc[:], in_=ln2_w.partition_broadcast(128), writes=["ln2_bc"], semkey='ld_c5')
                wr_sb = sbt(s5, "wr_sb", [128, 16, 36], F32)
                kb.dma('sp', out=wr_sb[:], in_=w_r.rearrange("(c p) n -> p c n", p=128), writes=["wr_sb"], semkey='ld_c5')
                kb.regroup(["ln2_bc", "wr_sb"], 'ld_c5')
                xr = sbt(s5, "xr", [128, D], F32)
                hsb = sbt(s5, "hsb", [128, D], F32)
                ssh = sbt(s5, "ssh", [128, 1], F32)
                hnT = sbt(s5, "hnT", [128, 16, 128], F32)
                r8 = sbt(s5, "r8", [128, 16], F32)
                goh = sbt(s5, "goh", [128, 4], F32)
                ein4 = sbt(s5, "ein4", [128, 4, 8], F32)
                ein = sbt(s5, "ein", [128, 8], F32)
                oh1 = sbt(s5, "oh1", [128, 8], F32)
                oh2 = sbt(s5, "oh2", [128, 8], F32)
                em = sbt(s5, "em", [128, 8], F32)
                sel1 = sbt(s5, "sel1", [128, 32], F32)
                sel2 = sbt(s5, "sel2", [128, 32], F32)
                selb = sbt(s5, "selb", [128, 32], BF16)
                cnt = sbt(s5, "cnt", [128, 32], F32)
                rk = sbt(s5, "rk", [128, 32], F32)
                tmp32 = sbt(s5, "tmp32", [128, 32], F32)
                okm = sbt(s5, "okm", [128, 32], F32)
                slf = sbt(s5, "slf", [128, 2], F32)
                kb.op('dve', lambda g: g.memset(cnt[:], 0.0), writes=["cnt"])
                hns = [sbt(s5, f"hn{k}", [128, D], F32) for k in range(2)]
                hnbs = [sbt(s5, f"hnb{k}", [128, D], BF16) for k in range(3)]
                lgs = [sbt(s5, f"lg{k}", [128, 36], F32) for k in range(2)]

                def stage_A(i):
                        tsl = slice(i * 128, (i + 1) * 128)
                        kb.dma('sp', out=xr[:], in_=x_tm[tsl, :], writes=["xr"], semkey='ld_xr')
                        for cb in range(4):
                            for cc in range(16):
                                mm(PB[cb][:, :], ycT_all[:, cc, tsl], Wo[:, cc, cb * 512:(cb + 1) * 512], cc == 0, cc == 15,
                                   ["ycT_all", "Wo"], [P(cb)], inc=(cc == 15))
                            tt(hsb[:, cb * 512:(cb + 1) * 512], PB[cb][:, :], xr[:, cb * 512:(cb + 1) * 512], ALU.add, [P(cb), "xr"], ["hsb"])
                        kb.dma('sp', out=h_scr[tsl, :], in_=hsb[:], reads=["hsb"], writes=[("h_scr", i)], semkey='st_h')
                        kb.op('dve', lambda g: g.memset(ssh[:], 0.0), writes=["ssh"])
                        act(hnbs[i % 3][:], hsb[:], AF.Square, ["hsb", "ssh"], [f"hnb{i%3}", "ssh"], accum=ssh[:])
                        rsqrt(ssh[:], ssh[:], 1.0 / D, ["ssh"], ["ssh"])
                        stt(hns[i % 2][:], hsb[:], ssh[:, 0:1], ln2_bc[:], ALU.mult, ALU.mult, ["hsb", "ssh", "ln2_bc"], [f"hn{i%2}"])
                        cp(hnbs[i % 3][:].rearrange("t (c p) -> t c p", p=128), hns[i % 2][:].rearrange("t (p c) -> t c p", c=16), [f"hn{i%2}"], [f"hnb{i%3}"], e='pool')

                def stage_B(i):
                        for half in range(2):
                            b = 4 + half
                            for d8 in range(8):
                                dc = half * 8 + d8
                                tr(PB[b][:, (d8 % 4) * 128:(d8 % 4 + 1) * 128] if d8 < 4 else PB[b][:, (d8 % 4) * 128:(d8 % 4 + 1) * 128],
                                   hns[i % 2][:, dc * 128:(dc + 1) * 128], ident_f[:], [f"hn{i%2}", "ident_f"], [P(b)], inc=(d8 % 4 == 3))
                                if d8 % 4 == 3:
                                    c0 = half * 8 + (d8 // 4) * 4
                                    cp(hnT[:, c0:c0 + 4, :].rearrange("p c t -> p (c t)"), PB[b][:, :], [P(b)], ["hnT"], e=('act' if (d8 // 4) else 'dve'))
                        for dc in range(16):
                            mm(PB[6][:, 0:36], hnT[:, dc, :], wr_sb[:, dc, :], dc == 0, dc == 15, ["hnT", "wr_sb"], [P(6)], inc=(dc == 15))
                        tt(lgs[i % 2][:], PB[6][:, 0:36], br_bc[:], ALU.add, [P(6), "br_bc"], [f"lg{i%2}"])

                def stage_C(i):
                        red(r8[:, 0:1], lgs[i % 2][:, 0:4], ALU.max, [f"lg{i%2}"], ["r8"])
                        ts(goh[:], lgs[i % 2][:, 0:4], r8[:, 0:1], None, ALU.is_equal, None, [f"lg{i%2}", "r8"], ["goh"])
                        ts(tmp32[:, 0:4], lgs[i % 2][:, 0:4], r8[:, 0:1], None, ALU.subtract, None, [f"lg{i%2}", "r8"], ["tmp32"])
                        act(tmp32[:, 0:4], tmp32[:, 0:4], AF.Exp, ["tmp32"], ["tmp32"])
                        red(r8[:, 1:2], tmp32[:, 0:4], ALU.add, ["tmp32"], ["r8"])
                        kb.op('dve', lambda g: g.reciprocal(out=r8[:, 2:3], in_=r8[:, 1:2]), reads=["r8"], writes=["r8"])
                        tt(ein4[:], lgs[i % 2][:, 4:36].rearrange("p (g e) -> p g e", e=8), goh[:].unsqueeze(2).to_broadcast([128, 4, 8]), ALU.mult,
                           [f"lg{i%2}", "goh"], ["ein4"])
                        red(ein[:], ein4[:].rearrange("p g e -> p e g"), ALU.add, ["ein4"], ["ein"])
                        red(r8[:, 3:4], ein[:], ALU.max, ["ein"], ["r8"])
                        ts(oh1[:], ein[:], r8[:, 3:4], None, ALU.is_equal, None, ["ein", "r8"], ["oh1"])
                        stt(em[:], oh1[:], -1e30, ein[:], ALU.mult, ALU.add, ["oh1", "ein"], ["em"])
                        red(r8[:, 4:5], em[:], ALU.max, ["em"], ["r8"])
                        ts(oh2[:], em[:], r8[:, 4:5], None, ALU.is_equal, None, ["em", "r8"], ["oh2"])
                        tt(r8[:, 5:6], r8[:, 4:5], r8[:, 3:4], ALU.subtract, ["r8"], ["r8"])
                        act(r8[:, 5:6], r8[:, 5:6], AF.Exp, ["r8"], ["r8"])
                        ts(r8[:, 6:7], r8[:, 5:6], 1.0, None, ALU.add, None, ["r8"], ["r8"])
                        kb.op('dve', lambda g: g.reciprocal(out=r8[:, 6:7], in_=r8[:, 6:7]), reads=["r8"], writes=["r8"])
                        tt(wts[:, i, 0:1], r8[:, 6:7], r8[:, 2:3], ALU.mult, ["r8"], ["wts"])
                        tt(wts[:, i, 1:2], wts[:, i, 0:1], r8[:, 5:6], ALU.mult, ["wts", "r8"], ["wts"])
                        tt(sel1[:].rearrange("p (g e) -> p g e", e=8), goh[:].unsqueeze(2).to_broadcast([128, 4, 8]),
                           oh1[:].unsqueeze(1).to_broadcast([128, 4, 8]), ALU.mult, ["goh", "oh1"], ["sel1"])
                        tt(sel2[:].rearrange("p (g e) -> p g e", e=8), goh[:].unsqueeze(2).to_broadcast([128, 4, 8]),
                           oh2[:].unsqueeze(1).to_broadcast([128, 4, 8]), ALU.mult, ["goh", "oh2"], ["sel2"])
                        tt(selb[:], sel1[:], sel2[:], ALU.add, ["sel1", "sel2"], ["selb"])
                        mm(PB[7][:, 0:32], ustrict_b[:], selb[:], True, True, ["ustrict_b", "selb"], [P(7)])
                        mm(PB[7][:, 32:64], ones_b[:], selb[:], True, True, ["ones_b", "selb"], [P(7)])
                        tt(rk[:], PB[7][:, 0:32], cnt[:], ALU.add, [P(7), "cnt"], ["rk"])
                        tt(cnt[:], cnt[:], PB[7][:, 32:64], ALU.add, ["cnt", P(7)], ["cnt"])
                        ts(okm[:], rk[:], float(CAP) - 0.5, None, ALU.is_lt, None, ["rk"], ["okm"])
                        tt(rk[:], rk[:], ebase[:], ALU.add, ["rk", "ebase"], ["rk"])
                        ts(rk[:], rk[:], -float(NE * CAP), None, ALU.add, None, ["rk"], ["rk"])
                        tt(rk[:], rk[:], okm[:], ALU.mult, ["rk", "okm"], ["rk"])
                        ts(rk[:], rk[:], float(NE * CAP), None, ALU.add, None, ["rk"], ["rk"])
                        tt(tmp32[:], okm[:], sel1[:], ALU.mult, ["okm", "sel1"], ["tmp32"])
                        red(r8[:, 8:9], tmp32[:], ALU.add, ["tmp32"], ["r8"])
                        tt(tmp32[:], okm[:], sel2[:], ALU.mult, ["okm", "sel2", "r8"], ["tmp32"])
                        red(r8[:, 9:10], tmp32[:], ALU.add, ["tmp32"], ["r8"])
                        tt(wts[:, i, :], wts[:, i, :], r8[:, 8:10], ALU.mult, ["wts", "r8"], ["wts"])
                        tt(tmp32[:], rk[:], sel1[:], ALU.mult, ["rk", "sel1"], ["tmp32"])
                        red(slf[:, 0:1], tmp32[:], ALU.add, ["tmp32"], ["slf"])
                        tt(tmp32[:], rk[:], sel2[:], ALU.mult, ["rk", "sel2", "slf"], ["tmp32"])
                        red(slf[:, 1:2], tmp32[:], ALU.add, ["tmp32"], ["slf"])
                        cp(slots[:, i, :], slf[:], ["slf"], ["slots"])
                        for k in range(2):
                            kb.dma('pool', fn=lambda g: g.indirect_dma_start(
                                out=xg_scr, out_offset=bass.IndirectOffsetOnAxis(ap=slots[:, i, k:k + 1], axis=0),
                                in_=hnbs[i % 3][:], in_offset=None), reads=[f"hnb{i%3}", "slots"], writes=["xg_scr"], semkey='sc_xg')

                stage_A(0)
                for i in range(NT):
                    if i + 1 < NT:
                        stage_A(i + 1)
                    stage_B(i)
                    if i >= 1:
                        stage_C(i - 1)
                stage_C(NT - 1)
                kb.regroup(["xg_scr"], 'sc_xg')
                kb.barrier()
            if "h" in dbg_out:
                kb.dma('sp', out=dbg_out["h"], in_=h_scr, reads=[("h_scr", i) for i in range(NT)], writes=["dbgh"], semkey='dbg')
            if "slots" in dbg_out:
                with contextlib.ExitStack() as sd:
                    sf = sbt(sd, "sf", [128, NT * 2], F32)
                    cp(sf[:], slots[:].rearrange("p i k -> p (i k)"), ["slots"], ["sf"])
                    kb.dma('sp', out=dbg_out["slots"], in_=sf[:], reads=["sf"], writes=["dbgs"], semkey='dbg')
                    kb.dma('sp', out=dbg_out["wts"], in_=wts[:].rearrange("p i k -> p (i k)"), reads=["wts"], writes=["dbgw"], semkey='dbg')
                    kb.barrier()

        if stage >= 6:
            with contextlib.ExitStack() as s6:
                NWB = 4
                wbuf = [sbt(s6, f"wbuf{k}", [128, 16 * 1024], BF16) for k in range(NWB)]
                xgs = [sbt(s6, f"xg{k}", [128, NST, D], BF16) for k in range(2)]
                xgT = sbt(s6, "xgT", [128, 16, CAP], BF16)
                gT = sbt(s6, "gT", [128, 8, CAP], BF16)
                hT = sbt(s6, "hT", [128, 8, CAP], BF16)
                ysb = [sbt(s6, f"ysb{k}", [128, D], F32) for k in range(2)]
                wctr = [0]
                kb.op('dve', lambda g: g.memset(ysb[0][:], 0.0), writes=["ysb0"])
                kb.dma('sp', out=ys_scr[NE * CAP:NE * CAP + 128, :], in_=ysb[0][:], reads=["ysb0"], writes=["ys_scr"], semkey='st_ys0')

                def wload(src, a, b_, flat):
                    k = wctr[0] % NWB
                    wctr[0] += 1
                    v3 = wbuf[k][:, 0:a * b_].rearrange("p (a b) -> p a b", b=b_)
                    kb.dma('pool', out=(wbuf[k][:, 0:a * b_] if flat else v3), in_=src, writes=[f"wbuf{k}"], semkey=f'ld_w{k}')
                    return v3, f"wbuf{k}"

                def xgload(e):
                    kb.dma('sp', out=xgs[e % 2][:], in_=xg_scr[e * CAP:(e + 1) * CAP, :].rearrange("(s p) d -> p s d", p=128),
                           reads=["xg_scr"], writes=[f"xg{e%2}"], semkey=f'ld_xg{e%2}')
                xgload(0)
                ytile = [0]
                for e in range(NE):
                    if e + 1 < NE:
                        xgload(e + 1)
                    xg = xgs[e % 2]
                    for st_ in range(NST):
                        for dc4 in range(4):
                            b = dc4 % 2
                            for q4 in range(4):
                                dc = dc4 * 4 + q4
                                tr(pbb(b)[:, q4 * 128:(q4 + 1) * 128], xg[:, st_, dc * 128:(dc + 1) * 128], ident_b[:], [f"xg{e%2}", "ident_b"], [P(b)],
                                   inc=(q4 == 3))
                            cp(xgT[:, dc4 * 4:(dc4 + 1) * 4, st_ * 128:(st_ + 1) * 128],
                               pbb(b)[:, 0:512].rearrange("p (c t) -> p c t", t=128), [P(b)], ["xgT"], e=('act' if dc4 % 2 else 'dve'))
                    Wg, wgn = wload(w_gate[e].rearrange("(p c) n -> p (c n)", c=16), 16, 1024, True)
                    Wu, wun = wload(w_up[e].rearrange("(p c) n -> p (c n)", c=16), 16, 1024, True)
                    for hc in range(8):
                        bg = 2 + (hc % 2)
                        for dc in range(16):
                            mm(PB[bg][:, 0:CAP], Wg[:, dc, hc * 128:(hc + 1) * 128], xgT[:, dc, :], dc == 0, dc == 15, [wgn, "xgT"], [P(bg)], inc=(dc == 15))
                        act(gT[:, hc, :], PB[bg][:, 0:CAP], AF.Silu, [P(bg)], [("gT", hc)])
                    for hc in range(8):
                        bu = 4 + (hc % 2)
                        for dc in range(16):
                            mm(PB[bu][:, 0:CAP], Wu[:, dc, hc * 128:(hc + 1) * 128], xgT[:, dc, :], dc == 0, dc == 15, [wun, "xgT"], [P(bu)], inc=(dc == 15))
                        tt(hT[:, hc, :], gT[:, hc, :], PB[bu][:, 0:CAP], ALU.mult, [("gT", hc), P(bu)], ["hT"])
                    Wdv, wdn = wload(w_down[e].rearrange("(c p) n -> p c n", p=128), 8, D, False)
                    for st_ in range(NST):
                        yk = ytile[0] % 2
                        ytile[0] += 1
                        yb = ysb[yk]
                        for cb in range(4):
                            b = 6 + (cb % 2)
                            for kc in range(8):
                                mm(PB[b][:, :], hT[:, kc, st_ * 128:(st_ + 1) * 128], Wdv[:, kc, cb * 512:(cb + 1) * 512], kc == 0, kc == 7,
                                   ["hT", wdn], [P(b)], inc=(kc == 7))
                            cp(yb[:, cb * 512:(cb + 1) * 512], PB[b][:, :], [P(b)], [f"ysb{yk}"], e=('act' if cb % 2 else 'dve'))
                        kb.dma('sp', out=ys_scr[e * CAP + st_ * 128: e * CAP + (st_ + 1) * 128, :], in_=yb[:], reads=[f"ysb{yk}"],
                               writes=["ys_scr"], semkey=f'st_ys{yk}')
                kb.barrier()
                kb.regroup(["ys_scr"], 'st_ys0')

        if stage >= 7:
            with contextlib.ExitStack() as s7:
                hb_ = [sbt(s7, f"hc{k}", [128, D], F32) for k in range(2)]
                y1 = [sbt(s7, f"y1{k}", [128, D], F32) for k in range(2)]
                y2 = [sbt(s7, f"y2{k}", [128, D], F32) for k in range(2)]
                ia = [sbt(s7, f"ia{k}", [128, 1], I32) for k in range(2)]
                ib = [sbt(s7, f"ib{k}", [128, 1], I32) for k in range(2)]
                for k in range(2):
                    kb.op('dve', lambda g: g.memset(y1[k][:], 0.0), writes=[f"y1{k}"])
                    kb.op('dve', lambda g: g.memset(y2[k][:], 0.0), writes=[f"y2{k}"])
                for i in range(NT):
                    k = i % 2
                    tsl = slice(i * 128, (i + 1) * 128)
                    kb.dma('sp', out=hb_[k][:], in_=h_scr[tsl, :], reads=[("h_scr", i)], writes=[f"hc{k}"], semkey=f'ld_hc{k}')
                    cp(ia[k][:], slots[:, i, 0:1], ["slots"], [f"ia{k}"])
                    cp(ib[k][:], slots[:, i, 1:2], ["slots"], [f"ib{k}"])
                    kb.dma('pool', fn=lambda g: g.indirect_dma_start(
                        out=y1[k][:], out_offset=None, in_=ys_scr,
                        in_offset=bass.IndirectOffsetOnAxis(ap=ia[k][:, 0:1], axis=0)),
                        reads=["ys_scr", f"ia{k}"], writes=[f"y1{k}"], semkey=f'ga_y1{k}')
                    kb.dma('pool', fn=lambda g: g.indirect_dma_start(
                        out=y2[k][:], out_offset=None, in_=ys_scr,
                        in_offset=bass.IndirectOffsetOnAxis(ap=ib[k][:, 0:1], axis=0)),
                        reads=["ys_scr", f"ib{k}"], writes=[f"y2{k}"], semkey=f'ga_y2{k}')
                    stt(hb_[k][:], y1[k][:], wts[:, i, 0:1], hb_[k][:], ALU.mult, ALU.add, [f"y1{k}", "wts", f"hc{k}"], [f"hc{k}"])
                    stt(hb_[k][:], y2[k][:], wts[:, i, 1:2], hb_[k][:], ALU.mult, ALU.add, [f"y2{k}", "wts", f"hc{k}"], [f"hc{k}"])
                    kb.dma('sp', out=out[tsl, :], in_=hb_[k][:], reads=[f"hc{k}"], writes=[("out", i)], semkey=f'st_out{k}')
                kb.barrier()
        kb.barrier()
        print("instructions (incl. waits):", kb.ninst, "sems:", len(kb.sems))
    return nc


def host_consts():
    j = np.arange(128)[:, None]
    l = np.arange(128)[None, :]
    c = {}
    c["c_ident"] = np.eye(128, dtype=np.float32)
    c["c_utri"] = (j <= l).astype(np.float32)
    c["c_ustrict"] = (j < l).astype(np.float32)
    c["c_negmask4"] = np.tile(np.where(l < j, -30000.0, 0.0).astype(np.float32), (1, 4))
    eh = np.zeros((16, 16, 128), np.float32)
    for h in range(16):
        eh[h, h, :] = 1.0
    c["c_ehsel"] = eh
    invf = (500000.0 ** (-np.arange(0, 16, 2, dtype=np.float32) / 16.0)).astype(np.float32)
    c["c_invf"] = np.tile(invf[None, :], (128, 1)).astype(np.float32)
    c["c_ebase"] = np.tile((np.arange(NE, dtype=np.float32) * CAP)[None, :], (128, 1)).astype(np.float32)
    return c


def make_in_maps(inputs, cores):
    f = lambda a: np.ascontiguousarray(a)
    shared = dict(host_consts())
    shared["w_in"] = f(inputs["w_in"][0])
    shared["w_out"] = f(inputs["w_out"][0])
    shared["w_gate"] = f(inputs["w_gate"][0])
    shared["w_up"] = f(inputs["w_up"][0])
    shared["w_down"] = f(inputs["w_down"][0])
    shared["ln1_t"] = f(inputs["ln1_w"][0].reshape(16, 128).T)
    shared["conv_wt"] = f(inputs["conv_w"][0].reshape(4, 12, 128).transpose(2, 1, 0))
    shared["conv_bt"] = f(inputs["conv_b"][0].reshape(12, 128).T)
    for k in ("dt_bias", "a_log", "d_skip", "ssd_norm_w", "q_norm_w", "k_norm_w", "subln_w", "ln2_w"):
        shared[k] = f(inputs[k][0])
    shared["lq1"] = f(inputs["lambda_q1"][0]); shared["lk1"] = f(inputs["lambda_k1"][0])
    shared["lq2"] = f(inputs["lambda_q2"][0]); shared["lk2"] = f(inputs["lambda_k2"][0])
    shared["w_r"] = f(np.concatenate([inputs["w_router_group"][0], inputs["w_router_expert"][0]], axis=1))
    shared["b_r"] = f(np.concatenate([inputs["b_router_group"][0], inputs["b_router_expert"][0]], axis=0))
    maps = []
    for b in cores:
        m = dict(shared)
        xb = inputs["x"][b]
        m["x_tm"] = f(xb)
        m["xT"] = f(xb.T)
        m["pos_tm"] = f(inputs["positions"][b].reshape(NT, 128).T.astype(np.int32))
        maps.append(m)
    return maps


def kernel(**inputs):
    inputs = {k: np.asarray(v) for k, v in inputs.items()}
    nc = build()
    maps = make_in_maps(inputs, list(range(8)))
    res = run_bass_kernel_spmd(nc, maps, core_ids=list(range(8)))
    return np.stack([r["out"] for r in res.results], axis=0).astype(np.float32)
```

```python
import contextlib
import math
import numpy as np
import concourse.bass as bass
import concourse.mybir as mybir
from concourse.bass_utils import run_bass_kernel_spmd

F32 = mybir.dt.float32
BF16 = mybir.dt.bfloat16
I32 = mybir.dt.int32
ALU = mybir.AluOpType
AF = mybir.ActivationFunctionType
AX = mybir.AxisListType

S = 2048
D = 2048
NT = 16
INC = 5648
NE = 32
TWO_EXP = False
CAP = 384
NST = CAP // 128
HID = 1024
EPS = 1e-6
LAM_INIT = 0.8 - 0.6 * math.exp(-0.3 * 0)
PI = math.pi


class KB:
    def __init__(self, nc, es):
        self.nc = nc
        self.es = es
        self.eng = {'pe': nc.tensor, 'act': nc.scalar, 'dve': nc.vector, 'pool': nc.gpsimd, 'sp': nc.sync}
        self.sems = {}
        self.cnt = {}
        self.waited = {e: {} for e in self.eng}
        self.lw = {}
        self.rd = {}
        self.ninst = 0

    def sem(self, key):
        if key not in self.sems:
            self.sems[key] = self.es.enter_context(self.nc.semaphore(str(key)))
            self.cnt[key] = 0
        return self.sems[key]

    def _deps(self, e, reads, writes):
        need = {}

        def add(k, v):
            if e == 'pe' and k == 'Epe':
                return
            if need.get(k, 0) < v:
                need[k] = v
        for r in reads:
            d = self.lw.get(r)
            if d:
                add(*d)
        for w in writes:
            d = self.lw.get(w)
            if d:
                add(*d)
            for k, v in self.rd.get(w, {}).items():
                add(k, v)
        return need

    def _emit_waits(self, e, need):
        for k, v in need.items():
            if self.waited[e].get(k, 0) < v:
                self.eng[e].wait_ge(self.sems[k], v)
                self.waited[e][k] = v
                self.ninst += 1

    def _record(self, reads, writes, tok):
        k, v = tok
        for r in reads:
            d = self.rd.setdefault(r, {})
            if d.get(k, 0) < v:
                d[k] = v
        for w in writes:
            self.lw[w] = tok
            self.rd[w] = {}

    def op(self, e, fn, reads=(), writes=(), inc=True):
        self._emit_waits(e, self._deps(e, reads, writes))
        inst = fn(self.eng[e])
        self.ninst += 1
        key = 'E' + e
        s = self.sem(key)
        if inc:
            self.cnt[key] += 1
            inst.then_inc(s, 1)
            tok = (key, self.cnt[key])
        else:
            tok = (key, self.cnt[key] + 1)
        self._record(reads, writes, tok)
        return inst

    def dma(self, e, out=None, in_=None, reads=(), writes=(), semkey=None, fn=None):
        self._emit_waits(e, self._deps(e, reads, writes))
        if fn is not None:
            inst = fn(self.eng[e])
        else:
            inst = self.eng[e].dma_start(out=out, in_=in_)
        self.ninst += 1
        s = self.sem(semkey)
        self.cnt[semkey] += 16
        inst.then_inc(s, 16)
        self._record(reads, writes, (semkey, self.cnt[semkey]))
        return inst

    def regroup(self, bufs, semkey):
        for b in bufs:
            self.lw[b] = (semkey, self.cnt[semkey])

    def barrier(self):
        for e in self.eng:
            need = {k: v for k, v in self.cnt.items() if v > 0}
            self._emit_waits(e, need)

    def finish(self, e, bufs):
        need = {}
        for b in bufs:
            d = self.lw.get(b)
            if d and need.get(d[0], 0) < d[1]:
                need[d[0]] = d[1]
        self._emit_waits(e, need)


def build(stage=99, dbg=()):
    nc = bass.Bass("TRN2", target_bir_lowering=False)

    def din(name, shape, dt=F32):
        return nc.dram_tensor(name, list(shape), dt, kind="ExternalInput").ap()

    def dscr(name, shape, dt):
        return nc.dram_tensor(name, list(shape), dt).ap()

    x_tm = din("x_tm", [S, D])
    xT = din("xT", [D, S])
    w_in = din("w_in", [D, INC])
    w_out = din("w_out", [D, D])
    w_gate = din("w_gate", [NE, D, HID])
    w_up = din("w_up", [NE, D, HID])
    w_down = din("w_down", [NE, HID, D])
    pos_tm = din("pos_tm", [128, NT], I32)
    ln1_t = din("ln1_t", [128, 16])
    conv_wt = din("conv_wt", [128, 12, 4])
    conv_bt = din("conv_bt", [128, 12])
    dt_bias = din("dt_bias", [16])
    a_log = din("a_log", [16])
    d_skip = din("d_skip", [16])
    ssd_norm_w = din("ssd_norm_w", [1024])
    q_norm_w = din("q_norm_w", [64])
    k_norm_w = din("k_norm_w", [64])
    lq1 = din("lq1", [64]); lk1 = din("lk1", [64]); lq2 = din("lq2", [64]); lk2 = din("lk2", [64])
    subln_w = din("subln_w", [128])
    ln2_w = din("ln2_w", [D])
    w_r = din("w_r", [D, 36])
    b_r = din("b_r", [36])
    c_ident = din("c_ident", [128, 128])
    c_utri = din("c_utri", [128, 128])
    c_ustrict = din("c_ustrict", [128, 128])
    c_negmask4 = din("c_negmask4", [128, 512])
    c_ehsel = din("c_ehsel", [16, 16, 128])
    c_invf = din("c_invf", [128, 8])
    c_ebase = din("c_ebase", [128, NE])

    out = nc.dram_tensor("out", [S, D], F32, kind="ExternalOutput").ap()
    dbg_out = {}
    for name, shape in dbg:
        dbg_out[name] = nc.dram_tensor("d_" + name, list(shape), F32, kind="ExternalOutput").ap()

    qT_scr = dscr("qT_scr", [128, 8, S], BF16)
    kT_scr = dscr("kT_scr", [128, 8, S], BF16)
    v_scr = dscr("v_scr", [S, 1024], BF16)
    zs_scr = dscr("zs_scr", [S, 1024], F32)
    ycat_scr = dscr("ycat_scr", [128, 16, S], BF16)
    h_scr = dscr("h_scr", [S, D], F32)
    xg_scr = dscr("xg_scr", [NE * CAP + 128, D], BF16)
    ys_scr = dscr("ys_scr", [NE * CAP + 128, D], BF16)

    with contextlib.ExitStack() as es:
        kb = KB(nc, es)

        def sbt(st, name, shape, dt):
            return st.enter_context(nc.sbuf_tensor(name, list(shape), dt))

        PD = [es.enter_context(nc.psum_tensor(f"pd{k}", [128, 1024], F32)) for k in range(4)]
        PB = [PD[k // 2][:, (k % 2) * 512:(k % 2 + 1) * 512] for k in range(8)]

        def pbb(k):
            return PB[k][:, :].bitcast(BF16)

        def P(k):
            return ('ps', k)

        def mm(outap, lhsT, rhs, start, stop, reads, writes, inc=True, sgc=False):
            return kb.op('pe', lambda e: e.matmul(outap, lhsT=lhsT, rhs=rhs, start=start, stop=stop, skip_group_check=sgc),
                         reads=reads, writes=writes, inc=inc)

        def tr(outap, in_, ident, reads, writes, inc=True):
            return kb.op('pe', lambda e: e.transpose(out=outap, in_=in_, identity=ident),
                         reads=reads, writes=writes, inc=inc)

        def act(outap, in_, func, reads, writes, bias=None, scale=None, accum=None):
            kw = {}
            if bias is not None:
                kw['bias'] = bias
            if scale is not None:
                kw['scale'] = scale
            if accum is not None:
                kw['accum_out'] = accum
            return kb.op('act', lambda e: e.activation(out=outap, in_=in_, func=func, **kw), reads=reads, writes=writes)

        def tt(outap, in0, in1, op, reads, writes, e='dve'):
            return kb.op(e, lambda g: g.tensor_tensor(out=outap, in0=in0, in1=in1, op=op), reads=reads, writes=writes)

        def ts(outap, in0, s1, s2, op0, op1, reads, writes, e='dve'):
            if op1 is None:
                return kb.op(e, lambda g: g.tensor_scalar(out=outap, in0=in0, scalar1=s1, scalar2=None, op0=op0),
                             reads=reads, writes=writes)
            return kb.op(e, lambda g: g.tensor_scalar(out=outap, in0=in0, scalar1=s1, scalar2=s2, op0=op0, op1=op1),
                         reads=reads, writes=writes)

        def stt(outap, in0, scalar, in1, op0, op1, reads, writes, e='dve'):
            return kb.op(e, lambda g: g.scalar_tensor_tensor(out=outap, in0=in0, scalar=scalar, in1=in1, op0=op0, op1=op1),
                         reads=reads, writes=writes)

        def red(outap, in_, op, reads, writes):
            return kb.op('dve', lambda g: g.tensor_reduce(out=outap, in_=in_, axis=AX.X, op=op), reads=reads, writes=writes)

        def cp(outap, in_, reads, writes, e='dve'):
            if e == 'act':
                return kb.op('act', lambda g: g.copy(out=outap, in_=in_), reads=reads, writes=writes)
            return kb.op(e, lambda g: g.tensor_copy(out=outap, in_=in_), reads=reads, writes=writes)

        eps_t = sbt(es, "eps_t", [128, 1], F32)
        kb.op('dve', lambda g: g.memset(eps_t[:], EPS), writes=["eps_t"])

        def rsqrt(outap, in_, mean_scale, reads, writes):
            act(outap, in_, AF.Ln, list(reads) + ["eps_t"], writes, bias=eps_t[:], scale=mean_scale)
            act(outap, outap, AF.Exp, writes, writes, scale=-0.5)

        cst = es
        names = []

        def cload(name, shape, src, dt=F32, cast=False):
            t = sbt(cst, name, shape, dt)
            kb.dma('pool' if cast else 'sp', out=t[:], in_=src, writes=[name], semkey='ldc_p' if cast else 'ldc_s')
            names.append((name, cast))
            return t

        ident_f = cload("ident_f", [128, 128], c_ident)
        ident_b = cload("ident_b", [128, 128], c_ident, BF16, True)
        utri_f = cload("utri_f", [128, 128], c_utri)
        utri_b = cload("utri_b", [128, 128], c_utri, BF16, True)
        ustrict_b = cload("ustrict_b", [128, 128], c_ustrict, BF16, True)
        negmask4_b = cload("negmask4_b", [128, 512], c_negmask4, BF16, True)
        invf = cload("invf", [128, 8], c_invf)
        ebase = cload("ebase", [128, NE], c_ebase)
        pos_i = cload("pos_i", [128, NT], pos_tm, I32)
        ln1 = cload("ln1", [128, 16], ln1_t)
        cw = cload("cw", [128, 12, 4], conv_wt)
        cbias = cload("cbias", [128, 12], conv_bt)
        dtb_bc = cload("dtb_bc", [128, 16], dt_bias.partition_broadcast(128))
        alog_bc = cload("alog_bc", [128, 16], a_log.partition_broadcast(128))
        dsk_bc = cload("dsk_bc", [128, 16], d_skip.partition_broadcast(128))
        wq_bc = cload("wq_bc", [128, 64], q_norm_w.partition_broadcast(128))
        wk_bc = cload("wk_bc", [128, 64], k_norm_w.partition_broadcast(128))
        l4 = sbt(cst, "l4", [128, 4, 64], F32)
        for n_, src in enumerate((lq1, lk1, lq2, lk2)):
            kb.dma('sp', out=l4[:, n_, :], in_=src.partition_broadcast(128), writes=["l4"], semkey='ldc_s')
        names.append(("l4", False))
        subw_bc = cload("subw_bc", [128, 128], subln_w.partition_broadcast(128))
        br_bc = cload("br_bc", [128, 36], b_r.partition_broadcast(128))
        kb.regroup([n for n, c in names if not c], 'ldc_s')
        kb.regroup([n for n, c in names if c], 'ldc_p')

        ones_f = sbt(cst, "ones_f", [128, 128], F32)
        kb.op('dve', lambda g: g.memset(ones_f[:], 1.0), writes=["ones_f"])
        ones_b = sbt(cst, "ones_b", [128, 128], BF16)
        kb.op('dve', lambda g: g.memset(ones_b[:], 1.0), writes=["ones_b"])

        A_bc = sbt(cst, "A_bc", [128, 16], F32)
        act(A_bc[:], alog_bc[:], AF.Exp, ["alog_bc"], ["A_bc"])
        ts(A_bc[:], A_bc[:], -1.0, None, ALU.mult, None, ["A_bc"], ["A_bc"])
        lam = sbt(cst, "lam", [128, 4], F32)
        lprod = sbt(cst, "lprod", [128, 2, 64], F32)
        tt(lprod[:, 0, :], l4[:, 0, :], l4[:, 1, :], ALU.mult, ["l4"], ["lprod"])
        tt(lprod[:, 1, :], l4[:, 2, :], l4[:, 3, :], ALU.mult, ["l4", "lprod"], ["lprod"])
        lsum = sbt(cst, "lsum", [128, 2], F32)
        red(lsum[:], lprod[:], ALU.add, ["lprod"], ["lsum"])
        act(lsum[:], lsum[:], AF.Exp, ["lsum"], ["lsum"])
        tt(lam[:, 0:1], lsum[:, 0:1], lsum[:, 1:2], ALU.subtract, ["lsum"], ["lam"])
        ts(lam[:, 0:1], lam[:, 0:1], LAM_INIT, None, ALU.add, None, ["lam"], ["lam"])
        ts(lam[:, 1:2], lam[:, 0:1], -1.0, None, ALU.mult, None, ["lam"], ["lam"])
        ts(wq_bc[:], wq_bc[:], 0.125, None, ALU.mult, None, ["wq_bc"], ["wq_bc"])
        ts(subw_bc[:], subw_bc[:], 1.0 - LAM_INIT, None, ALU.mult, None, ["subw_bc"], ["subw_bc"])
        pos_f = sbt(cst, "pos_f", [128, NT], F32)
        cp(pos_f[:], pos_i[:], ["pos_i"], ["pos_f"])
        ang = sbt(cst, "ang", [128, NT, 8], F32)
        tt(ang[:], pos_f[:].unsqueeze(2).to_broadcast([128, NT, 8]), invf[:].unsqueeze(1).to_broadcast([128, NT, 8]),
           ALU.mult, ["pos_f", "invf"], ["ang"])
        sin_t = sbt(cst, "sin_t", [128, NT, 8], F32)
        cos_t = sbt(cst, "cos_t", [128, NT, 8], F32)
        angi = sbt(cst, "angi", [128, NT, 8], I32)
        angk = sbt(cst, "angk", [128, NT, 8], F32)
        for (tab, nm, off) in ((sin_t, "sin_t", 0.0), (cos_t, "cos_t", 0.5 * PI)):
            ts(tab[:], ang[:], off, None, ALU.add, None, ["ang"], [nm])
            ts(angk[:], tab[:], 1.0 / (2 * PI), None, ALU.mult, None, [nm], ["angk"])
            cp(angi[:], angk[:], ["angk"], ["angi"])
            cp(angk[:], angi[:], ["angi"], ["angk"])
            stt(tab[:], angk[:], -2 * PI, tab[:], ALU.mult, ALU.add, ["angk", nm], [nm])
            ts(tab[:], tab[:], -PI, PI, ALU.max, ALU.min, [nm], [nm])
            act(tab[:], tab[:], AF.Sin, [nm], [nm])

        rstd1 = sbt(cst, "rstd1", [128, NT], F32)

        with contextlib.ExitStack() as sx:
            xTb = sbt(sx, "xTb", [128, 16, S], BF16)
            for c in range(16):
                kb.dma('pool', out=xTb[:, c, :], in_=xT[c * 128:(c + 1) * 128, :], writes=[("xTb", c)], semkey='ld_xT')
            kb.regroup([("xTb", c) for c in range(16)], 'ld_xT')
            for c in range(16):
                ts(xTb[:, c, :], xTb[:, c, :], ln1[:, c:c + 1], None, ALU.mult, None, [("xTb", c), "ln1"], [("xTb", c)])
            XT = [("xTb", c) for c in range(16)]

            with contextlib.ExitStack() as s1:
                xf = [sbt(s1, f"xf{k}", [128, D], F32) for k in range(2)]
                junk = sbt(s1, "junk", [128, D], BF16)
                ss1 = sbt(s1, "ss1", [128, NT], F32)
                kb.op('dve', lambda g: g.memset(ss1[:], 0.0), writes=["ss1"])
                for i in range(NT):
                    kb.dma('sp', out=xf[i % 2][:], in_=x_tm[i * 128:(i + 1) * 128, :], writes=[f"xf{i%2}"], semkey=f'ld_xf{i%2}')
                    act(junk[:], xf[i % 2][:], AF.Square, [f"xf{i%2}", "ss1"], ["junk", "ss1"], accum=ss1[:, i:i + 1])
                rsqrt(rstd1[:], ss1[:], 1.0 / D, ["ss1"], ["rstd1"])
                kb.barrier()

            if "rstd1" in dbg_out:
                kb.dma('sp', out=dbg_out["rstd1"], in_=rstd1[:], reads=["rstd1"], writes=["dbg_rstd1"], semkey='dbg')

            def proj_pass(col0, epilogue, tagp):
                with contextlib.ExitStack() as sp_:
                    W = sbt(sp_, "Wp" + tagp, [128, 16, 1024], BF16)
                    for half in range(2):
                        kb.dma('pool', out=W[:, :, half * 512:(half + 1) * 512],
                               in_=w_in[:, col0 + half * 512: col0 + (half + 1) * 512].rearrange("(c p) n -> p c n", p=128),
                               writes=[("Wp", half)], semkey=f'ld_Wp{half}')
                    for i in range(NT):
                        banks = []
                        for half in range(2):
                            b = half + 2 * (i % 2)
                            banks.append(b)
                            for dc in range(16):
                                mm(PB[b][:, :], xTb[:, dc, i * 128:(i + 1) * 128], W[:, dc, half * 512:(half + 1) * 512],
                                   dc == 0, dc == 15, [("xTb", dc), ("Wp", half)], [P(b)], inc=(dc == 15))
                        epilogue(i, banks, sp_)
                    kb.barrier()

            if stage >= 2:
                def qk_epilogue_factory(wbc, wbc_name, scr, tg):
                    state = {}

                    def ep(i, banks, st):
                        if not state:
                            for k in range(2):
                                state['qf', k] = sbt(st, f"qf{tg}{k}", [128, 1024], F32)
                                state['sq', k] = sbt(st, f"sq{tg}{k}", [128, 1024], F32)
                                state['ss', k] = sbt(st, f"ss{tg}{k}", [128, 16], F32)
                                state['rot', k] = sbt(st, f"rot{tg}{k}", [128, 4, 16, 8], F32)
                                state['qb', k] = sbt(st, f"qb{tg}{k}", [128, 1024], BF16)
                                state['qt', k] = sbt(st, f"qt{tg}{k}", [128, 8, 128], BF16)
                        k = i % 2
                        qf, sq, ssq, rot, qb, qt = (state[n, k] for n in ('qf', 'sq', 'ss', 'rot', 'qb', 'qt'))
                        QF, SQ, SS, ROT, QB, QT = (f"{n}{k}" for n in ('qf', 'sq', 'ssq', 'rot', 'qb', 'qt'))
                        for half in range(2):
                            act(qf[:, half * 512:(half + 1) * 512], PB[banks[half]][:, :], AF.Copy,
                                [P(banks[half]), "rstd1"], [QF], scale=rstd1[:, i:i + 1])
                        tt(sq[:], qf[:], qf[:], ALU.mult, [QF], [SQ], e='pool')
                        red(ssq[:], sq[:].rearrange("p (b d) -> p b d", d=64), ALU.add, [SQ], [SS])
                        rsqrt(ssq[:], ssq[:], 1.0 / 64, [SS], [SS])
                        qf3 = qf[:].rearrange("p (b d) -> p b d", d=64)
                        tt(qf3, qf3, ssq[:].unsqueeze(2).to_broadcast([128, 16, 64]), ALU.mult, [QF, SS], [QF])
                        tt(qf3, qf3, wbc[:].unsqueeze(1).to_broadcast([128, 16, 64]), ALU.mult, [QF, wbc_name], [QF])
                        cosb = cos_t[:, i, :].unsqueeze(1).to_broadcast([128, 16, 8])
                        sinb = sin_t[:, i, :].unsqueeze(1).to_broadcast([128, 16, 8])
                        t1 = qf3[:, :, 0:8]
                        t2 = qf3[:, :, 8:16]
                        tt(rot[:, 0], t1, cosb, ALU.mult, [QF, "cos_t"], [ROT])
                        tt(rot[:, 1], t2, sinb, ALU.mult, [QF, "sin_t", ROT], [ROT])
                        tt(rot[:, 2], t2, cosb, ALU.mult, [QF, "cos_t", ROT], [ROT])
                        tt(rot[:, 3], t1, sinb, ALU.mult, [QF, "sin_t", ROT], [ROT])
                        qb3 = qb[:].rearrange("p (b d) -> p b d", d=64)
                        cp(qb3[:, :, 16:64], qf3[:, :, 16:64], [QF], [QB], e='pool')
                        tt(qb3[:, :, 0:8], rot[:, 0], rot[:, 1], ALU.subtract, [ROT, QB], [QB])
                        tt(qb3[:, :, 8:16], rot[:, 2], rot[:, 3], ALU.add, [ROT, QB], [QB])
                        if i >= 1:
                            ep_b(i - 1)
                        if i == NT - 1:
                            ep_b(i)

                    def ep_b(i):
                        k = i % 2
                        qb, qt = state['qb', k], state['qt', k]
                        QB, QT = f"qb{k}", f"qt{k}"
                        pb = 4 + (i % 2)
                        for hd in range(8):
                            tr(pbb(pb)[:, hd * 128:(hd + 1) * 128], qb[:, hd * 128:(hd + 1) * 128], ident_b[:],
                               [QB, "ident_b"], [P(pb)], inc=(hd == 7))
                        cp(qt[:].rearrange("p h t -> p (h t)"), pbb(pb), [P(pb)], [QT], e='act')
                        kb.dma('sp', out=scr[:, :, i * 128:(i + 1) * 128], in_=qt[:], reads=[QT], writes=[("scr" + tg, i)],
                               semkey=f'st_{tg}{k}')
                    return ep

                proj_pass(2576, qk_epilogue_factory(wq_bc, "wq_bc", qT_scr, "q"), "q")
                proj_pass(3600, qk_epilogue_factory(wk_bc, "wk_bc", kT_scr, "k"), "k")

                vstate = {}

                def v_ep(i, banks, st):
                    if not vstate:
                        vstate['vb'] = [sbt(st, f"vb{k}", [128, 1024], BF16) for k in range(2)]
                    vb = vstate['vb'][i % 2]
                    for half in range(2):
                        act(vb[:, half * 512:(half + 1) * 512], PB[banks[half]][:, :], AF.Copy,
                            [P(banks[half]), "rstd1"], [f"vb{i%2}"], scale=rstd1[:, i:i + 1])
                    kb.dma('sp', out=v_scr[i * 128:(i + 1) * 128, :], in_=vb[:], reads=[f"vb{i%2}"], writes=[("scrv", i)],
                           semkey=f'st_v{i%2}')
                proj_pass(4624, v_ep, "v")

            if stage >= 3:
                with contextlib.ExitStack() as s3:
                    xbcT = sbt(s3, "xbcT", [128, 12, S], BF16)
                    rstd_bc = sbt(s3, "rstd_bc", [128, S], F32)
                    with contextlib.ExitStack() as s31:
                        dg = [sbt(s31, f"dg{k}", [128, 128], F32) for k in range(2)]
                        for i in range(NT):
                            ts(dg[i % 2][:], ident_f[:], rstd1[:, i:i + 1], None, ALU.mult, None, ["ident_f", "rstd1"], [f"dg{i%2}"])
                            b = i // 4
                            mm(PB[b][:, (i % 4) * 128:(i % 4 + 1) * 128], ones_f[:], dg[i % 2][:], True, True,
                               ["ones_f", f"dg{i%2}"], [P(b)])
                        for b in range(4):
                            cp(rstd_bc[:, b * 512:(b + 1) * 512], PB[b][:, :], [P(b)], ["rstd_bc"])
                        Wx = [sbt(s31, f"Wx{k}", [128, 16, 512], BF16) for k in range(2)]
                        ub = [sbt(s31, f"ub{k}", [128, S + 3], F32) for k in range(1)]
                        acc = [sbt(s31, f"acc{k}", [128, S], F32) for k in range(1)]
                        for k in range(1):
                            kb.op('dve', lambda g: g.memset(ub[k][:, 0:3], 0.0), writes=[f"ub{k}"])
                        for blk in range(3):
                            kb.dma('pool', out=Wx[blk % 2][:],
                                   in_=w_in[:, 1024 + blk * 512: 1024 + (blk + 1) * 512].rearrange("(c p) n -> p c n", p=128),
                                   writes=[f"Wx{blk%2}"], semkey=f'ld_Wx{blk%2}')
                            for jj in range(4):
                                j = blk * 4 + jj
                                u = ub[0]
                                a = acc[0]
                                for tb in range(4):
                                    b = 4 + tb
                                    for dc in range(16):
                                        mm(PB[b][:, :], Wx[blk % 2][:, dc, jj * 128:(jj + 1) * 128], xTb[:, dc, tb * 512:(tb + 1) * 512],
                                           dc == 0, dc == 15, [f"Wx{blk%2}", ("xTb", dc)], [P(b)], inc=(dc == 15))
                                    tt(u[:, 3 + tb * 512: 3 + (tb + 1) * 512], PB[b][:, :], rstd_bc[:, tb * 512:(tb + 1) * 512], ALU.mult,
                                       [P(b), "rstd_bc"], ["ub0"])
                                act(a[:], u[:, 3:3 + S], AF.Identity, ["ub0", "cw", "cbias"], ["acc0"],
                                    bias=cbias[:, j:j + 1], scale=cw[:, j, 3:4])
                                for k in range(3):
                                    stt(a[:], u[:, k:k + S], cw[:, j, k:k + 1], a[:], ALU.mult, ALU.add,
                                        ["ub0", "cw", "acc0"], ["acc0"], e='dve')
                                act(xbcT[:, j, :], a[:], AF.Silu, ["acc0"], [("xbcT", j)])
                        kb.barrier()
                    if "xbcT" in dbg_out:
                        with contextlib.ExitStack() as sd:
                            tmpf = sbt(sd, "tmpf", [128, S], F32)
                            for j in range(12):
                                cp(tmpf[:], xbcT[:, j, :], [("xbcT", j)], ["tmpf"])
                                kb.dma('sp', out=dbg_out["xbcT"][j * 128:(j + 1) * 128, :], in_=tmpf[:], reads=["tmpf"], writes=["dbgx"], semkey='dbg')
                            kb.barrier()

                    dtv = sbt(s3, "dtv", [128, 256], F32)
                    sd_t = sbt(s3, "sd_t", [128, 256], F32)
                    E_t = sbt(s3, "E_t", [128, 256], F32)
                    nacs = sbt(s3, "nacs", [128, 256], F32)
                    cd_bc = sbt(s3, "cd_bc", [128, 256], F32)
                    acsT = sbt(s3, "acsT", [16, S], F32)
                    with contextlib.ExitStack() as s32:
                        Wdt = sbt(s32, "Wdt", [128, 16, 16], BF16)
                        kb.dma('pool', out=Wdt[:], in_=w_in[:, 2560:2576].rearrange("(c p) n -> p c n", p=128), writes=["Wdt"], semkey='ld_Wdt')
                        for i in range(NT):
                            for dc in range(16):
                                mm(PB[0][:, i * 16:(i + 1) * 16], xTb[:, dc, i * 128:(i + 1) * 128], Wdt[:, dc, :], dc == 0, dc == 15,
                                   [("xTb", dc), "Wdt"], [P(0)], inc=(dc == 15))
                        t1 = sbt(s32, "t1", [128, 256], F32)
                        t2 = sbt(s32, "t2", [128, 256], F32)
                        a_tok = sbt(s32, "a_tok", [128, 256], F32)
                        t13 = t1[:].rearrange("p (i h) -> p i h", h=16)
                        tt(t13, PB[0][:, 0:256].rearrange("p (i h) -> p i h", h=16), rstd1[:].unsqueeze(2).to_broadcast([128, NT, 16]),
                           ALU.mult, [P(0), "rstd1"], ["t1"])
                        tt(t13, t13, dtb_bc[:].unsqueeze(1).to_broadcast([128, NT, 16]), ALU.add, ["t1", "dtb_bc"], ["t1"])
                        stt(t2[:], t1[:], -1.0, t1[:], ALU.mult, ALU.max, ["t1"], ["t2"])
                        act(t2[:], t2[:], AF.Exp, ["t2"], ["t2"], scale=-1.0)
                        ts(t2[:], t2[:], 1.0, None, ALU.add, None, ["t2"], ["t2"])
                        act(t2[:], t2[:], AF.Ln, ["t2"], ["t2"])
                        ts(t1[:], t1[:], 0.0, None, ALU.max, None, ["t1"], ["t1"])
                        tt(dtv[:], t1[:], t2[:], ALU.add, ["t1", "t2"], ["dtv"])
                        tt(a_tok[:].rearrange("p (i h) -> p i h", h=16), dtv[:].rearrange("p (i h) -> p i h", h=16),
                           A_bc[:].unsqueeze(1).to_broadcast([128, NT, 16]), ALU.mult, ["dtv", "A_bc"], ["a_tok"])
                        mm(PB[1][:, 0:256], utri_f[:], a_tok[:], True, True, ["utri_f", "a_tok"], [P(1)])
                        mm(PB[2][:, 0:256], ones_f[:], a_tok[:], True, True, ["ones_f", "a_tok"], [P(2)])
                        for i in range(NT):
                            b = 4 + i // 4
                            mm(PB[b][0:16, (i % 4) * 128:(i % 4 + 1) * 128], a_tok[:, i * 16:(i + 1) * 16], utri_f[:], True, True,
                               ["a_tok", "utri_f"], [P(b)])
                        for b in range(4):
                            cp(acsT[:, b * 512:(b + 1) * 512], PB[4 + b][0:16, :], [P(4 + b)], ["acsT"])
                        act(E_t[:], PB[1][:, 0:256], AF.Exp, [P(1)], ["E_t"])
                        ts(nacs[:], PB[1][:, 0:256], -1.0, None, ALU.mult, None, [P(1)], ["nacs"])
                        act(cd_bc[:], PB[2][:, 0:256], AF.Exp, [P(2)], ["cd_bc"])
                        tt(t1[:], PB[2][:, 0:256], nacs[:], ALU.add, [P(2), "nacs"], ["t1"])
                        act(t1[:], t1[:], AF.Exp, ["t1"], ["t1"])
                        tt(sd_t[:], t1[:], dtv[:], ALU.mult, ["t1", "dtv"], ["sd_t"])
                        kb.barrier()

                    zst = {}

                    def z_ep(i, banks, st):
                        if not zst:
                            zst['z'] = [sbt(st, f"zsb{k}", [128, 1024], F32) for k in range(2)]
                        zb = zst['z'][i % 2]
                        for half in range(2):
                            act(zb[:, half * 512:(half + 1) * 512], PB[banks[half]][:, :], AF.Silu,
                                [P(banks[half]), "rstd1"], [f"zsb{i%2}"], scale=rstd1[:, i:i + 1])
                        kb.dma('sp', out=zs_scr[i * 128:(i + 1) * 128, :], in_=zb[:], reads=[f"zsb{i%2}"], writes=[("zs_scr", i)],
                               semkey=f'st_z{i%2}')
                    proj_pass(0, z_ep, "z")

                    with contextlib.ExitStack() as s34:
                        ehsel = sbt(s34, "ehsel", [16, 16, 128], F32)
                        kb.dma('sp', out=ehsel[:], in_=c_ehsel, writes=["ehsel"], semkey='ld_c3')
                        normw_bc = sbt(s34, "normw_bc", [128, 1024], F32)
                        kb.dma('sp', out=normw_bc[:], in_=ssd_norm_w.partition_broadcast(128), writes=["normw_bc"], semkey='ld_c3')
                        kb.regroup(["ehsel", "normw_bc"], 'ld_c3')
                        xdt = sbt(s34, "xdt", [128, 1024], BF16)
                        xdts = sbt(s34, "xdts", [128, 1024], BF16)
                        xsD = sbt(s34, "xsD", [128, 1024], BF16)
                        B_tm = sbt(s34, "B_tm", [128, 256], BF16)
                        cbT = sbt(s34, "cbT", [128, 256], BF16)
                        decT = [sbt(s34, f"decT{k}", [128, 512], BF16) for k in range(2)]
                        MT = [sbt(s34, f"MT{k}", [128, 512], BF16) for k in range(2)]
                        ytmp = [sbt(s34, f"ytmp{g}", [128, 512], F32) for g in range(2)]
                        y = sbt(s34, "y", [128, 1024], F32)
                        prev_f = sbt(s34, "prev_f", [128, 1024], F32)
                        prev_b = sbt(s34, "prev_b", [128, 1024], BF16)
                        zsb = sbt(s34, "zsb", [128, 1024], F32)
                        sq = sbt(s34, "sqy", [128, 1024], BF16)
                        ss2 = sbt(s34, "ss2", [128, 2], F32)
                        ynbs = [sbt(s34, f"ynb{k}", [128, 1024], BF16) for k in range(2)]
                        ycTs = [sbt(s34, f"ycT{k}", [128, 8, 128], BF16) for k in range(2)]

                        def gate_b(i):
                            k = i % 2
                            tsl_ = slice(i * 128, (i + 1) * 128)
                            for j in range(8):
                                tr(pbb(0)[:, j * 128:(j + 1) * 128], ynbs[k][:, j * 128:(j + 1) * 128], ident_b[:], [f"ynb{k}", "ident_b"], [P(0)], inc=(j == 7))
                            cp(ycTs[k][:].rearrange("p j t -> p (j t)"), pbb(0), [P(0)], [f"ycT{k}"], e='act')
                            kb.dma('sp', out=ycat_scr[:, 0:8, tsl_], in_=ycTs[k][:], reads=[f"ycT{k}"], writes=[("ycat", 0, i)], semkey=f'st_yc{k}')

                        YOB = [7, 2]
                        STB = [1, 0]
                        for i in range(NT):
                            ynb = ynbs[i % 2]
                            tsl = slice(i * 128, (i + 1) * 128)
                            kb.dma('sp', out=zsb[:], in_=zs_scr[tsl, :], reads=[("zs_scr", i)], writes=["zsb"], semkey='ld_zs')
                            for j in range(8):
                                tr(pbb(0)[:, j * 128:(j + 1) * 128], xbcT[:, j, tsl], ident_b[:], [("xbcT", j), "ident_b"], [P(0)], inc=(j == 7))
                            ps3 = pbb(0).rearrange("p (h d) -> p h d", d=64)
                            tt(xdt[:].rearrange("p (h d) -> p h d", d=64), ps3, dtv[:, i * 16:(i + 1) * 16].unsqueeze(2).to_broadcast([128, 16, 64]),
                               ALU.mult, [P(0), "dtv"], ["xdt"])
                            tt(xsD[:].rearrange("p (h d) -> p h d", d=64), ps3, dsk_bc[:].unsqueeze(2).to_broadcast([128, 16, 64]),
                               ALU.mult, [P(0), "dsk_bc"], ["xsD"])
                            tt(xdts[:].rearrange("p (h d) -> p h d", d=64), ps3, sd_t[:, i * 16:(i + 1) * 16].unsqueeze(2).to_broadcast([128, 16, 64]),
                               ALU.mult, [P(0), "sd_t"], ["xdts"])
                            for g in range(2):
                                tr(pbb(1)[:, g * 128:(g + 1) * 128], xbcT[:, 8 + g, tsl], ident_b[:], [("xbcT", 8 + g), "ident_b"], [P(1)], inc=(g == 1))
                            cp(B_tm[:], pbb(1)[:, 0:256], [P(1)], ["B_tm"], e='act')
                            for g in range(2):
                                mm(PB[2][:, g * 128:(g + 1) * 128], xbcT[:, 8 + g, tsl], xbcT[:, 10 + g, tsl], True, True,
                                   [("xbcT", 8 + g), ("xbcT", 10 + g)], [P(2)], inc=(g == 1))
                            cp(cbT[:], PB[2][:, 0:256], [P(2)], ["cbT"], e='act')

                            def emit_R(hq):
                                rb = 3 + (hq % 2)
                                for q in range(4):
                                    h = hq * 4 + q
                                    mm(PB[rb][:, q * 128:(q + 1) * 128], ehsel[:, h, :], acsT[:, tsl], q == 0, False, ["ehsel", "acsT"], [P(rb)],
                                       inc=False, sgc=True)
                                mm(PB[rb][:, :], ident_b[:], negmask4_b[:], False, True, ["ident_b", "negmask4_b"], [P(rb)], sgc=True)

                            def emit_Y(hq):
                                g = hq // 2
                                rb = 3 + (hq % 2)
                                dT = decT[hq % 2]
                                for q in range(4):
                                    h = hq * 4 + q
                                    act(dT[:, q * 128:(q + 1) * 128], PB[rb][:, q * 128:(q + 1) * 128], AF.Exp, [P(rb), "nacs"], [f"decT{hq%2}"],
                                        bias=nacs[:, i * 16 + h: i * 16 + h + 1])
                                tt(MT[hq % 2][:].rearrange("p (q l) -> p q l", q=4), dT[:].rearrange("p (q l) -> p q l", q=4),
                                   cbT[:, g * 128:(g + 1) * 128].unsqueeze(1).to_broadcast([128, 4, 128]), ALU.mult,
                                   [f"decT{hq%2}", "cbT"], [f"MT{hq%2}"])
                                if hq % 2 == 0:
                                    mm(PB[5 + g][:, :], ident_b[:], xsD[:, g * 512:(g + 1) * 512], True, False, ["ident_b", "xsD"], [P(5 + g)],
                                       inc=False, sgc=True)
                                for q in range(4):
                                    h = hq * 4 + q
                                    mm(PB[5 + g][:, (h % 8) * 64:(h % 8 + 1) * 64], MT[hq % 2][:, q * 128:(q + 1) * 128], xdt[:, h * 64:(h + 1) * 64],
                                       False, (hq % 2 == 1 and q == 3), [f"MT{hq%2}", "xdt"], [P(5 + g)], inc=(q == 3), sgc=True)

                            emit_R(0)
                            for hq in range(4):
                                if hq + 1 < 4:
                                    emit_R(hq + 1)
                                emit_Y(hq)
                            for g in range(2):
                                gs = slice(g * 512, (g + 1) * 512)
                                yb_, sb_ = YOB[g], STB[g]
                                if i > 0:
                                    mm(PB[yb_][:, :], xbcT[:, 10 + g, tsl], prev_b[:, gs], True, True, [("xbcT", 10 + g), "prev_b"], [P(yb_)])
                                if i < NT - 1:
                                    mm(PB[sb_][:, :], B_tm[:, g * 128:(g + 1) * 128], xdts[:, gs], True, True, ["B_tm", "xdts"], [P(sb_)])
                            for g in range(2):
                                gs = slice(g * 512, (g + 1) * 512)
                                yb_, sb_ = YOB[g], STB[g]
                                if i > 0:
                                    tt(ytmp[g][:].rearrange("p (h d) -> p h d", d=64), PB[yb_][:, :].rearrange("p (h d) -> p h d", d=64),
                                       E_t[:, i * 16 + g * 8: i * 16 + g * 8 + 8].unsqueeze(2).to_broadcast([128, 8, 64]), ALU.mult,
                                       [P(yb_), "E_t"], [f"ytmp{g}"])
                                    tt(y[:, gs], PB[5 + g][:, :], ytmp[g][:], ALU.add, [P(5 + g), f"ytmp{g}"], ["y"])
                                else:
                                    cp(y[:, gs], PB[5 + g][:, :], [P(5 + g)], ["y"])
                                if i < NT - 1:
                                    if i > 0:
                                        tt(prev_f[:, gs].rearrange("p (h d) -> p h d", d=64), prev_f[:, gs].rearrange("p (h d) -> p h d", d=64),
                                           cd_bc[:, i * 16 + g * 8: i * 16 + g * 8 + 8].unsqueeze(2).to_broadcast([128, 8, 64]), ALU.mult,
                                           ["prev_f", "cd_bc"], ["prev_f"])
                                        tt(prev_f[:, gs], prev_f[:, gs], PB[sb_][:, :], ALU.add, ["prev_f", P(sb_)], ["prev_f"])
                                    else:
                                        cp(prev_f[:, gs], PB[sb_][:, :], [P(sb_)], ["prev_f"])
                                    cp(prev_b[:, gs], prev_f[:, gs], ["prev_f"], ["prev_b"], e='act')
                            tt(y[:], y[:], zsb[:], ALU.mult, ["y", "zsb"], ["y"])
                            kb.op('dve', lambda g_: g_.memset(ss2[:], 0.0), writes=["ss2"])
                            for g in range(2):
                                gs = slice(g * 512, (g + 1) * 512)
                                act(sq[:, gs], y[:, gs], AF.Square, ["y", "ss2"], ["sqy", "ss2"], accum=ss2[:, g:g + 1])
                            rsqrt(ss2[:], ss2[:], 1.0 / 512, ["ss2"], ["ss2"])
                            for g in range(2):
                                gs = slice(g * 512, (g + 1) * 512)
                                stt(ynb[:, gs], y[:, gs], ss2[:, g:g + 1], normw_bc[:, gs], ALU.mult, ALU.mult, ["y", "ss2", "normw_bc"], [f"ynb{i%2}"])
                            if i >= 1:
                                gate_b(i - 1)
                            if i == NT - 1:
                                gate_b(i)
                        kb.barrier()
                    kb.barrier()
            kb.barrier()

        if stage >= 4:
            with contextlib.ExitStack() as s4:
                qT = sbt(s4, "qT", [128, 8, S], BF16)
                kTz = [sbt(s4, f"kTz{c}", [128, 8, S], BF16) for c in range(2)]
                V = sbt(s4, "V", [128, NT, 1024], BF16)
                subw_col = sbt(s4, "subw_col", [128, 1], F32)
                kb.dma('sp', out=subw_col[:], in_=subln_w.rearrange("(p o) -> p o", o=1), writes=["subw_col"], semkey='ld_c4')
                ts(subw_col[:], subw_col[:], 1.0 - LAM_INIT, None, ALU.mult, None, ["subw_col"], ["subw_col"])
                for c in range(2):
                    oth = slice((1 - c) * 64, (2 - c) * 64)
                    kb.op('pool', lambda g: g.memset(kTz[c][oth, :, :], 0.0), writes=[f"kTz{c}"])
                for hd in range(8):
                    kb.dma('sp', out=qT[:, hd, :], in_=qT_scr[:, hd, :], reads=[("scrq", i) for i in range(NT)], writes=["qT"], semkey='ld_q')
                    for c in range(2):
                        cs = slice(c * 64, (c + 1) * 64)
                        kb.dma('sp', out=kTz[c][cs, hd, :], in_=kT_scr[cs, hd, :], reads=[("scrk", i) for i in range(NT)], writes=[f"kTz{c}"], semkey='ld_k')
                for i in range(NT):
                    kb.dma('sp', out=V[:, i, :], in_=v_scr[i * 128:(i + 1) * 128, :], reads=[("scrv", i)], writes=["V"], semkey='ld_v')
                kb.regroup(["qT"], 'ld_q'); kb.regroup(["kTz0", "kTz1"], 'ld_k'); kb.regroup(["V"], 'ld_v')
                NPT = 3
                pT = [sbt(s4, f"pT{k}", [128, 2, 512], BF16) for k in range(NPT)]
                cR = [sbt(s4, f"cR{c}", [128, 512], F32) for c in range(2)]
                cO = [sbt(s4, f"cO{c}", [128, 512], F32) for c in range(2)]
                o0 = sbt(s4, "o0", [128, 512], F32)
                sqo = sbt(s4, "sqo", [128, 512], F32)
                rs = sbt(s4, "rs", [128, 512], F32)
                ssc = sbt(s4, "ssc", [128, 512], F32)
                ycA = [sbt(s4, f"ycA{k}", [128, 512], BF16) for k in range(2)]
                OB, RB, SSB = 4, 6, 3
                steps = []
                blk = 0
                for hd in range(8):
                    for qb in range(4):
                        nkt = 4 * qb + 4
                        for c in range(2):
                            for kp in range(nkt // 2):
                                steps.append(dict(hd=hd, qb=qb, c=c, kp=kp, nkt=nkt, blk=blk, last=(c == 1 and kp == nkt // 2 - 1)))
                        blk += 1

                def geom(st, h):
                    kt = 2 * st['kp'] + h
                    j = kt - 4 * st['qb']
                    off = max(j, 0) * 128
                    return kt, j, off, 4 * st['qb'] * 128 + off, 512 - off

                def emit_S(i):
                    st = steps[i]
                    d = i % 2
                    for h in range(2):
                        kt, j, off, q0, nq = geom(st, h)
                        mm(PB[2 * d + h][:, 0:nq], kTz[st['c']][:, st['hd'], kt * 128:(kt + 1) * 128], qT[:, st['hd'], q0:q0 + nq],
                           True, True, [f"kTz{st['c']}", "qT"], [("psd", d)], inc=(h == 1))

                def emit_rest(i):
                    st = steps[i]
                    d = i % 2
                    pk = i % NPT
                    c, hd = st['c'], st['hd']
                    W = geom(st, 0)[4]
                    if TWO_EXP:
                        for h in range(2):
                            nq_h = geom(st, h)[4]
                            act(pT[pk][:, h, 0:nq_h], PB[2 * d + h][:, 0:nq_h], AF.Exp, [("psd", d)], [f"pT{pk}"])
                    else:
                        act(pT[pk][:, :, 0:W], PD[d][:, :].rearrange("p (h w) -> p h w", h=2)[:, :, 0:W], AF.Exp, [("psd", d)], [f"pT{pk}"])
                    for h in range(2):
                        kt, j, off, q0, nq = geom(st, h)
                        if j >= 0:
                            tt(pT[pk][:, h, 0:128], pT[pk][:, h, 0:128], utri_b[:], ALU.mult, [f"pT{pk}", "utri_b"], [f"pT{pk}"], e='dve')
                    for h in range(2):
                        kt, j, off, q0, nq = geom(st, h)
                        mm(PB[OB + c][:, off:off + nq], V[:, kt, hd * 128:(hd + 1) * 128], pT[pk][:, h, 0:nq], kt == 0, kt == st['nkt'] - 1,
                           ["V", f"pT{pk}"], [P(OB + c)], inc=False, sgc=True)
                        mm(PB[RB + c][:, off:off + nq], ones_b[:], pT[pk][:, h, 0:nq], kt == 0, kt == st['nkt'] - 1,
                           ["ones_b", f"pT{pk}"], [P(RB + c)], sgc=True)

                def epiA(st):
                    act(cR[0][:], PB[RB][:, :], AF.Ln, [P(RB)], ["cR0"])
                    cp(cO[0][:], PB[OB][:, :], [P(OB)], ["cO0"])
                    act(cR[1][:], PB[RB + 1][:, :], AF.Ln, [P(RB + 1)], ["cR1"])
                    cp(cO[1][:], PB[OB + 1][:, :], [P(OB + 1)], ["cO1"])
                    for c in range(2):
                        act(cR[c][:], cR[c][:], AF.Exp, [f"cR{c}"], [f"cR{c}"], scale=-1.0)
                    for c in range(2):
                        tt(cO[c][:], cO[c][:], cR[c][:], ALU.mult, [f"cO{c}", f"cR{c}"], [f"cO{c}"])
                    stt(o0[:], cO[1][:], lam[:, 1:2], cO[0][:], ALU.mult, ALU.add, ["cO1", "lam", "cO0"], ["o0"])
                    tt(sqo[:], o0[:], o0[:], ALU.mult, ["o0"], ["sqo"])

                def epiB(st):
                    mm(PB[SSB][:, :], ones_f[:], sqo[:], True, True, ["ones_f", "sqo"], [("psd", 1)])
                    cp(ssc[:], PB[SSB][:, :], [("psd", 1)], ["ssc"])

                def epiC(st):
                    yk = st['blk'] % 2
                    act(rs[:], ssc[:], AF.Ln, ["ssc", "eps_t"], ["rs"], bias=eps_t[:], scale=1.0 / 128)
                    act(rs[:], rs[:], AF.Exp, ["rs"], ["rs"], scale=-0.5)
                    stt(ycA[yk][:], o0[:], subw_col[:, 0:1], rs[:], ALU.mult, ALU.mult, ["o0", "subw_col", "rs"], [f"ycA{yk}"])
                    kb.dma('sp', out=ycat_scr[:, 8 + st['hd'], st['qb'] * 512:(st['qb'] + 1) * 512], in_=ycA[yk][:], reads=[f"ycA{yk}"],
                           writes=[("ycat", 1, st['hd'], st['qb'])], semkey=f'st_ya{yk}')

                NS = len(steps)
                emit_S(0)
                pend = []
                for i in range(NS):
                    if i + 1 < NS:
                        emit_S(i + 1)
                    emit_rest(i)
                    while pend and (pend[0][0] <= i or steps[i]['last']):
                        _, fn, st_ = pend.pop(0)
                        fn(st_)
                    if steps[i]['last']:
                        epiA(steps[i])
                        pend = [(i + 4, epiB, steps[i]), (i + 6, epiC, steps[i])]
                for _, fn, st_ in pend:
                    fn(st_)
                kb.barrier()

        if "ycat" in dbg_out:
            with contextlib.ExitStack() as sd:
                tb16 = sbt(sd, "tb16", [128, S], BF16)
                tmpf = sbt(sd, "tmpf2", [128, S], F32)
                for j in range(16):
                    kb.dma('sp', out=tb16[:], in_=ycat_scr[:, j, :], reads=[], writes=["tb16"], semkey='dbg2')
                    cp(tmpf[:], tb16[:], ["tb16"], ["tmpf2"])
                    kb.dma('sp', out=dbg_out["ycat"][j * 128:(j + 1) * 128, :], in_=tmpf[:], reads=["tmpf2"], writes=["dbgy"], semkey='dbg')
                kb.barrier()

        slots = sbt(es, "slots", [128, NT, 2], I32)
        wts = sbt(es, "wts", [128, NT, 2], F32)
        kb.op('dve', lambda g: g.memset(slots[:], 0), writes=["slots"])
        kb.op('dve', lambda g: g.memset(wts[:], 0.0), writes=["wts"])
        if stage >= 5:
            with contextlib.ExitStack() as s5:
                ycT_all = sbt(s5, "ycT_all", [128, 16, S], BF16)
                Wo = sbt(s5, "Wo", [128, 16, D], BF16)
                for j in range(16):
                    kb.dma('sp', out=ycT_all[:, j, :], in_=ycat_scr[:, j, :], writes=["ycT_all"], semkey='ld_yc')
                kb.regroup(["ycT_all"], 'ld_yc')
                for cb in range(4):
                    kb.dma('pool', out=Wo[:, :, cb * 512:(cb + 1) * 512],
                           in_=w_out[:, cb * 512:(cb + 1) * 512].rearrange("(c p) n -> p c n", p=128), writes=["Wo"], semkey='ld_Wo')
                kb.regroup(["Wo"], 'ld_Wo')
                ln2_bc = sbt(s5, "ln2_bc", [128, D], F32)
                kb.dma('sp', out=ln2_bc[:], in_=ln2_w.partition_broadcast(128), writes=["ln2_bc"], semkey='ld_c5')
                wr_sb = sbt(s5, "wr_sb", [128, 16, 36], F32)
                kb.dma('sp', out=wr_sb[:], in_=w_r.rearrange("(c p) n -> p c n", p=128), writes=["wr_sb"], semkey='ld_c5')
                kb.regroup(["ln2_bc", "wr_sb"], 'ld_c5')
                xr = sbt(s5, "xr", [128, D], F32)
                hsb = sbt(s5, "hsb", [128, D], F32)
                ssh = sbt(s5, "ssh", [128, 1], F32)
                hnT = sbt(s5, "hnT", [128, 16, 128], F32)
                r8 = sbt(s5, "r8", [128, 16], F32)
                goh = sbt(s5, "goh", [128, 4], F32)
                ein4 = sbt(s5, "ein4", [128, 4, 8], F32)
                ein = sbt(s5, "ein", [128, 8], F32)
                oh1 = sbt(s5, "oh1", [128, 8], F32)
                oh2 = sbt(s5, "oh2", [128, 8], F32)
                em = sbt(s5, "em", [128, 8], F32)
                sel1 = sbt(s5, "sel1", [128, 32], F32)
                sel2 = sbt(s5, "sel2", [128, 32], F32)
                selb = sbt(s5, "selb", [128, 32], BF16)
                cnt = sbt(s5, "cnt", [128, 32], F32)
                rk = sbt(s5, "rk", [128, 32], F32)
                tmp32 = sbt(s5, "tmp32", [128, 32], F32)
                okm = sbt(s5, "okm", [128, 32], F32)
                slf = sbt(s5, "slf", [128, 2], F32)
                kb.op('dve', lambda g: g.memset(cnt[:], 0.0), writes=["cnt"])
                hns = [sbt(s5, f"hn{k}", [128, D], F32) for k in range(2)]
                hnbs = [sbt(s5, f"hnb{k}", [128, D], BF16) for k in range(3)]
                lgs = [sbt(s5, f"lg{k}", [128, 36], F32) for k in range(2)]

                def A1(i):
                        tsl = slice(i * 128, (i + 1) * 128)
                        kb.dma('sp', out=xr[:], in_=x_tm[tsl, :], writes=["xr"], semkey='ld_xr')
                        for cb in range(4):
                            for cc in range(16):
                                mm(PB[cb][:, :], ycT_all[:, cc, tsl], Wo[:, cc, cb * 512:(cb + 1) * 512], cc == 0, cc == 15,
                                   ["ycT_all", "Wo"], [P(cb)], inc=(cc == 15))
                            tt(hsb[:, cb * 512:(cb + 1) * 512], PB[cb][:, :], xr[:, cb * 512:(cb + 1) * 512], ALU.add, [P(cb), "xr"], ["hsb"])
                        kb.dma('sp', out=h_scr[tsl, :], in_=hsb[:], reads=["hsb"], writes=[("h_scr", i)], semkey='st_h')

                def A2(i):
                        kb.op('dve', lambda g: g.memset(ssh[:], 0.0), writes=["ssh"])
                        act(hnbs[i % 3][:], hsb[:], AF.Square, ["hsb", "ssh"], [f"hnb{i%3}", "ssh"], accum=ssh[:])
                        rsqrt(ssh[:], ssh[:], 1.0 / D, ["ssh"], ["ssh"])
                        stt(hns[i % 2][:], hsb[:], ssh[:, 0:1], ln2_bc[:], ALU.mult, ALU.mult, ["hsb", "ssh", "ln2_bc"], [f"hn{i%2}"])
                        cp(hnbs[i % 3][:].rearrange("t (c p) -> t c p", p=128), hns[i % 2][:].rearrange("t (p c) -> t c p", c=16), [f"hn{i%2}"], [f"hnb{i%3}"], e='pool')


                def Btr(i):
                        for half in range(2):
                            b = 4 + half
                            for d8 in range(8):
                                dc = half * 8 + d8
                                tr(PB[b][:, (d8 % 4) * 128:(d8 % 4 + 1) * 128] if d8 < 4 else PB[b][:, (d8 % 4) * 128:(d8 % 4 + 1) * 128],
                                   hns[i % 2][:, dc * 128:(dc + 1) * 128], ident_f[:], [f"hn{i%2}", "ident_f"], [P(b)], inc=(d8 % 4 == 3))
                                if d8 % 4 == 3:
                                    c0 = half * 8 + (d8 // 4) * 4
                                    cp(hnT[:, c0:c0 + 4, :].rearrange("p c t -> p (c t)"), PB[b][:, :], [P(b)], ["hnT"], e='act')

                def Brt(i):
                        for dc in range(16):
                            mm(PB[6][:, 0:36], hnT[:, dc, :], wr_sb[:, dc, :], dc == 0, dc == 15, ["hnT", "wr_sb"], [P(6)], inc=(dc == 15))

                def Blg(i):
                        tt(lgs[i % 2][:], PB[6][:, 0:36], br_bc[:], ALU.add, [P(6), "br_bc"], [f"lg{i%2}"])


                def Ca(i):
                        red(r8[:, 0:1], lgs[i % 2][:, 0:4], ALU.max, [f"lg{i%2}"], ["r8"])
                        ts(goh[:], lgs[i % 2][:, 0:4], r8[:, 0:1], None, ALU.is_equal, None, [f"lg{i%2}", "r8"], ["goh"])
                        ts(tmp32[:, 0:4], lgs[i % 2][:, 0:4], r8[:, 0:1], None, ALU.subtract, None, [f"lg{i%2}", "r8"], ["tmp32"])
                        act(tmp32[:, 0:4], tmp32[:, 0:4], AF.Exp, ["tmp32"], ["tmp32"])
                        red(r8[:, 1:2], tmp32[:, 0:4], ALU.add, ["tmp32"], ["r8"])
                        kb.op('dve', lambda g: g.reciprocal(out=r8[:, 2:3], in_=r8[:, 1:2]), reads=["r8"], writes=["r8"])
                        tt(ein4[:], lgs[i % 2][:, 4:36].rearrange("p (g e) -> p g e", e=8), goh[:].unsqueeze(2).to_broadcast([128, 4, 8]), ALU.mult,
                           [f"lg{i%2}", "goh"], ["ein4"])
                        red(ein[:], ein4[:].rearrange("p g e -> p e g"), ALU.add, ["ein4"], ["ein"])
                        red(r8[:, 3:4], ein[:], ALU.max, ["ein"], ["r8"])
                        ts(oh1[:], ein[:], r8[:, 3:4], None, ALU.is_equal, None, ["ein", "r8"], ["oh1"])
                        stt(em[:], oh1[:], -1e30, ein[:], ALU.mult, ALU.add, ["oh1", "ein"], ["em"])
                        red(r8[:, 4:5], em[:], ALU.max, ["em"], ["r8"])
                        ts(oh2[:], em[:], r8[:, 4:5], None, ALU.is_equal, None, ["em", "r8"], ["oh2"])
                        tt(r8[:, 5:6], r8[:, 4:5], r8[:, 3:4], ALU.subtract, ["r8"], ["r8"])
                        act(r8[:, 5:6], r8[:, 5:6], AF.Exp, ["r8"], ["r8"])
                        ts(r8[:, 6:7], r8[:, 5:6], 1.0, None, ALU.add, None, ["r8"], ["r8"])
                        kb.op('dve', lambda g: g.reciprocal(out=r8[:, 6:7], in_=r8[:, 6:7]), reads=["r8"], writes=["r8"])
                        tt(wts[:, i, 0:1], r8[:, 6:7], r8[:, 2:3], ALU.mult, ["r8"], ["wts"])
                        tt(wts[:, i, 1:2], wts[:, i, 0:1], r8[:, 5:6], ALU.mult, ["wts", "r8"], ["wts"])
                        tt(sel1[:].rearrange("p (g e) -> p g e", e=8), goh[:].unsqueeze(2).to_broadcast([128, 4, 8]),
                           oh1[:].unsqueeze(1).to_broadcast([128, 4, 8]), ALU.mult, ["goh", "oh1"], ["sel1"])
                        tt(sel2[:].rearrange("p (g e) -> p g e", e=8), goh[:].unsqueeze(2).to_broadcast([128, 4, 8]),
                           oh2[:].unsqueeze(1).to_broadcast([128, 4, 8]), ALU.mult, ["goh", "oh2"], ["sel2"])
                        tt(selb[:], sel1[:], sel2[:], ALU.add, ["sel1", "sel2"], ["selb"])

                def Crk(i):
                        mm(PB[7][:, 0:32], ustrict_b[:], selb[:], True, True, ["ustrict_b", "selb"], [P(7)])
                        mm(PB[7][:, 32:64], ones_b[:], selb[:], True, True, ["ones_b", "selb"], [P(7)])

                def Cb(i):
                        tt(rk[:], PB[7][:, 0:32], cnt[:], ALU.add, [P(7), "cnt"], ["rk"])
                        tt(cnt[:], cnt[:], PB[7][:, 32:64], ALU.add, ["cnt", P(7)], ["cnt"])
                        ts(okm[:], rk[:], float(CAP) - 0.5, None, ALU.is_lt, None, ["rk"], ["okm"])
                        tt(rk[:], rk[:], ebase[:], ALU.add, ["rk", "ebase"], ["rk"])
                        ts(rk[:], rk[:], -float(NE * CAP), None, ALU.add, None, ["rk"], ["rk"])
                        tt(rk[:], rk[:], okm[:], ALU.mult, ["rk", "okm"], ["rk"])
                        ts(rk[:], rk[:], float(NE * CAP), None, ALU.add, None, ["rk"], ["rk"])
                        tt(tmp32[:], okm[:], sel1[:], ALU.mult, ["okm", "sel1"], ["tmp32"])
                        red(r8[:, 8:9], tmp32[:], ALU.add, ["tmp32"], ["r8"])
                        tt(tmp32[:], okm[:], sel2[:], ALU.mult, ["okm", "sel2", "r8"], ["tmp32"])
                        red(r8[:, 9:10], tmp32[:], ALU.add, ["tmp32"], ["r8"])
                        tt(wts[:, i, :], wts[:, i, :], r8[:, 8:10], ALU.mult, ["wts", "r8"], ["wts"])
                        tt(tmp32[:], rk[:], sel1[:], ALU.mult, ["rk", "sel1"], ["tmp32"])
                        red(slf[:, 0:1], tmp32[:], ALU.add, ["tmp32"], ["slf"])
                        tt(tmp32[:], rk[:], sel2[:], ALU.mult, ["rk", "sel2", "slf"], ["tmp32"])
                        red(slf[:, 1:2], tmp32[:], ALU.add, ["tmp32"], ["slf"])
                        cp(slots[:, i, :], slf[:], ["slf"], ["slots"])
                        for k in range(2):
                            kb.dma('pool', fn=lambda g: g.indirect_dma_start(
                                out=xg_scr, out_offset=bass.IndirectOffsetOnAxis(ap=slots[:, i, k:k + 1], axis=0),
                                in_=hnbs[i % 3][:], in_offset=None), reads=[f"hnb{i%3}", "slots"], writes=["xg_scr"], semkey='sc_xg')


                A1(0)
                A2(0)
                for i in range(NT):
                    if i + 1 < NT:
                        A1(i + 1)
                    Btr(i)
                    if i >= 1:
                        Ca(i - 1)
                    Brt(i)
                    if i >= 1:
                        Crk(i - 1)
                        Cb(i - 1)
                    Blg(i)
                    if i + 1 < NT:
                        A2(i + 1)
                Ca(NT - 1)
                Crk(NT - 1)
                Cb(NT - 1)
                kb.regroup(["xg_scr"], 'sc_xg')
                kb.barrier()
            if "h" in dbg_out:
                kb.dma('sp', out=dbg_out["h"], in_=h_scr, reads=[("h_scr", i) for i in range(NT)], writes=["dbgh"], semkey='dbg')
            if "slots" in dbg_out:
                with contextlib.ExitStack() as sd:
                    sf = sbt(sd, "sf", [128, NT * 2], F32)
                    cp(sf[:], slots[:].rearrange("p i k -> p (i k)"), ["slots"], ["sf"])
                    kb.dma('sp', out=dbg_out["slots"], in_=sf[:], reads=["sf"], writes=["dbgs"], semkey='dbg')
                    kb.dma('sp', out=dbg_out["wts"], in_=wts[:].rearrange("p i k -> p (i k)"), reads=["wts"], writes=["dbgw"], semkey='dbg')
                    kb.barrier()

        if stage >= 6:
            with contextlib.ExitStack() as s6:
                NWB = 4
                wbuf = [sbt(s6, f"wbuf{k}", [128, 16 * 1024], BF16) for k in range(NWB)]
                xgs = [sbt(s6, f"xg{k}", [128, NST, D], BF16) for k in range(2)]
                xgT = sbt(s6, "xgT", [128, 16, CAP], BF16)
                gT = sbt(s6, "gT", [128, 8, CAP], BF16)
                hT = sbt(s6, "hT", [128, 8, CAP], BF16)
                ysb = [sbt(s6, f"ysb{k}", [128, D], BF16) for k in range(2)]
                wctr = [0]
                kb.op('dve', lambda g: g.memset(ysb[0][:], 0.0), writes=["ysb0"])
                kb.dma('sp', out=ys_scr[NE * CAP:NE * CAP + 128, :], in_=ysb[0][:], reads=["ysb0"], writes=["ys_scr"], semkey='st_ys0')

                def wload(src, a, b_, flat):
                    k = wctr[0] % NWB
                    wctr[0] += 1
                    v3 = wbuf[k][:, 0:a * b_].rearrange("p (a b) -> p a b", b=b_)
                    kb.dma('pool', out=(wbuf[k][:, 0:a * b_] if flat else v3), in_=src, writes=[f"wbuf{k}"], semkey=f'ld_w{k}')
                    return v3, f"wbuf{k}"

                def xgload(e):
                    kb.dma('sp', out=xgs[e % 2][:], in_=xg_scr[e * CAP:(e + 1) * CAP, :].rearrange("(s p) d -> p s d", p=128),
                           reads=["xg_scr"], writes=[f"xg{e%2}"], semkey=f'ld_xg{e%2}')
                xgload(0)
                ytile = [0]
                for e in range(NE):
                    if e + 1 < NE:
                        xgload(e + 1)
                    xg = xgs[e % 2]
                    for st_ in range(NST):
                        for dc4 in range(4):
                            b = dc4 % 2
                            for q4 in range(4):
                                dc = dc4 * 4 + q4
                                tr(pbb(b)[:, q4 * 128:(q4 + 1) * 128], xg[:, st_, dc * 128:(dc + 1) * 128], ident_b[:], [f"xg{e%2}", "ident_b"], [P(b)],
                                   inc=(q4 == 3))
                            cp(xgT[:, dc4 * 4:(dc4 + 1) * 4, st_ * 128:(st_ + 1) * 128],
                               pbb(b)[:, 0:512].rearrange("p (c t) -> p c t", t=128), [P(b)], ["xgT"], e=('act' if dc4 % 2 else 'dve'))
                    Wg, wgn = wload(w_gate[e].rearrange("(p c) n -> p (c n)", c=16), 16, 1024, True)
                    Wu, wun = wload(w_up[e].rearrange("(p c) n -> p (c n)", c=16), 16, 1024, True)
                    for hc in range(8):
                        bg = 2 + (hc % 2)
                        for dc in range(16):
                            mm(PB[bg][:, 0:CAP], Wg[:, dc, hc * 128:(hc + 1) * 128], xgT[:, dc, :], dc == 0, dc == 15, [wgn, "xgT"], [P(bg)], inc=(dc == 15))
                        act(gT[:, hc, :], PB[bg][:, 0:CAP], AF.Silu, [P(bg)], [("gT", hc)])
                    for hc in range(8):
                        bu = 4 + (hc % 2)
                        for dc in range(16):
                            mm(PB[bu][:, 0:CAP], Wu[:, dc, hc * 128:(hc + 1) * 128], xgT[:, dc, :], dc == 0, dc == 15, [wun, "xgT"], [P(bu)], inc=(dc == 15))
                        tt(hT[:, hc, :], gT[:, hc, :], PB[bu][:, 0:CAP], ALU.mult, [("gT", hc), P(bu)], ["hT"])
                    Wdv, wdn = wload(w_down[e].rearrange("(c p) n -> p c n", p=128), 8, D, False)
                    for st_ in range(NST):
                        yk = ytile[0] % 2
                        ytile[0] += 1
                        yb = ysb[yk]
                        for cb in range(4):
                            b = 6 + (cb % 2)
                            for kc in range(8):
                                mm(PB[b][:, :], hT[:, kc, st_ * 128:(st_ + 1) * 128], Wdv[:, kc, cb * 512:(cb + 1) * 512], kc == 0, kc == 7,
                                   ["hT", wdn], [P(b)], inc=(kc == 7))
                            cp(yb[:, cb * 512:(cb + 1) * 512], PB[b][:, :], [P(b)], [f"ysb{yk}"], e=('act' if cb % 2 else 'dve'))
                        kb.dma('sp', out=ys_scr[e * CAP + st_ * 128: e * CAP + (st_ + 1) * 128, :], in_=yb[:], reads=[f"ysb{yk}"],
                               writes=["ys_scr"], semkey=f'st_ys{yk}')
                kb.barrier()
                kb.regroup(["ys_scr"], 'st_ys0')

        if stage >= 7:
            with contextlib.ExitStack() as s7:
                NB7 = 4
                hb_ = [sbt(s7, f"hc{k}", [128, D], F32) for k in range(NB7)]
                y1 = [sbt(s7, f"y1{k}", [128, D], BF16) for k in range(NB7)]
                y2 = [sbt(s7, f"y2{k}", [128, D], BF16) for k in range(NB7)]
                ia = [sbt(s7, f"ia{k}", [128, 1], I32) for k in range(NB7)]
                ib = [sbt(s7, f"ib{k}", [128, 1], I32) for k in range(NB7)]
                for k in range(NB7):
                    kb.op('dve', lambda g: g.memset(ia[k][:], 0), writes=[f"ia{k}"])
                    kb.op('dve', lambda g: g.memset(ib[k][:], 0), writes=[f"ib{k}"])
                    kb.op('dve', lambda g: g.memset(y1[k][:], 0.0), writes=[f"y1{k}"])
                    kb.op('dve', lambda g: g.memset(y2[k][:], 0.0), writes=[f"y2{k}"])

                def p7_load(i):
                    k = i % NB7
                    tsl = slice(i * 128, (i + 1) * 128)
                    kb.dma('sp', out=hb_[k][:], in_=h_scr[tsl, :], reads=[("h_scr", i)], writes=[f"hc{k}"], semkey=f'ld_hc{k}')
                    cp(ia[k][:], slots[:, i, 0:1], ["slots"], [f"ia{k}"])
                    cp(ib[k][:], slots[:, i, 1:2], ["slots"], [f"ib{k}"])
                    kb.dma('pool', fn=lambda g: g.indirect_dma_start(
                        out=y1[k][:], out_offset=None, in_=ys_scr,
                        in_offset=bass.IndirectOffsetOnAxis(ap=ia[k][:, 0:1], axis=0)),
                        reads=["ys_scr", f"ia{k}"], writes=[f"y1{k}"], semkey=f'ga_y1{k}')
                    kb.dma('pool', fn=lambda g: g.indirect_dma_start(
                        out=y2[k][:], out_offset=None, in_=ys_scr,
                        in_offset=bass.IndirectOffsetOnAxis(ap=ib[k][:, 0:1], axis=0)),
                        reads=["ys_scr", f"ib{k}"], writes=[f"y2{k}"], semkey=f'ga_y2{k}')

                for i in range(min(NB7 - 1, NT)):
                    p7_load(i)
                for i in range(NT):
                    if i + NB7 - 1 < NT:
                        p7_load(i + NB7 - 1)
                    k = i % NB7
                    tsl = slice(i * 128, (i + 1) * 128)
                    stt(hb_[k][:], y1[k][:], wts[:, i, 0:1], hb_[k][:], ALU.mult, ALU.add, [f"y1{k}", "wts", f"hc{k}"], [f"hc{k}"])
                    stt(hb_[k][:], y2[k][:], wts[:, i, 1:2], hb_[k][:], ALU.mult, ALU.add, [f"y2{k}", "wts", f"hc{k}"], [f"hc{k}"])
                    kb.dma('sp', out=out[tsl, :], in_=hb_[k][:], reads=[f"hc{k}"], writes=[("out", i)], semkey=f'st_out{k}')
                kb.barrier()
        kb.barrier()
        print("instructions (incl. waits):", kb.ninst, "sems:", len(kb.sems))
    return nc


def host_consts():
    j = np.arange(128)[:, None]
    l = np.arange(128)[None, :]
    c = {}
    c["c_ident"] = np.eye(128, dtype=np.float32)
    c["c_utri"] = (j <= l).astype(np.float32)
    c["c_ustrict"] = (j < l).astype(np.float32)
    c["c_negmask4"] = np.tile(np.where(l < j, -30000.0, 0.0).astype(np.float32), (1, 4))
    eh = np.zeros((16, 16, 128), np.float32)
    for h in range(16):
        eh[h, h, :] = 1.0
    c["c_ehsel"] = eh
    invf = (500000.0 ** (-np.arange(0, 16, 2, dtype=np.float32) / 16.0)).astype(np.float32)
    c["c_invf"] = np.tile(invf[None, :], (128, 1)).astype(np.float32)
    c["c_ebase"] = np.tile((np.arange(NE, dtype=np.float32) * CAP)[None, :], (128, 1)).astype(np.float32)
    return c


def make_in_maps(inputs, cores):
    f = lambda a: np.ascontiguousarray(a)
    shared = dict(host_consts())
    shared["w_in"] = f(inputs["w_in"][0])
    shared["w_out"] = f(inputs["w_out"][0])
    shared["w_gate"] = f(inputs["w_gate"][0])
    shared["w_up"] = f(inputs["w_up"][0])
    shared["w_down"] = f(inputs["w_down"][0])
    shared["ln1_t"] = f(inputs["ln1_w"][0].reshape(16, 128).T)
    shared["conv_wt"] = f(inputs["conv_w"][0].reshape(4, 12, 128).transpose(2, 1, 0))
    shared["conv_bt"] = f(inputs["conv_b"][0].reshape(12, 128).T)
    for k in ("dt_bias", "a_log", "d_skip", "ssd_norm_w", "q_norm_w", "k_norm_w", "subln_w", "ln2_w"):
        shared[k] = f(inputs[k][0])
    shared["lq1"] = f(inputs["lambda_q1"][0]); shared["lk1"] = f(inputs["lambda_k1"][0])
    shared["lq2"] = f(inputs["lambda_q2"][0]); shared["lk2"] = f(inputs["lambda_k2"][0])
    shared["w_r"] = f(np.concatenate([inputs["w_router_group"][0], inputs["w_router_expert"][0]], axis=1))
    shared["b_r"] = f(np.concatenate([inputs["b_router_group"][0], inputs["b_router_expert"][0]], axis=0))
    maps = []
    for b in cores:
        m = dict(shared)
        xb = inputs["x"][b]
        m["x_tm"] = f(xb)
        m["xT"] = f(xb.T)
        m["pos_tm"] = f(inputs["positions"][b].reshape(NT, 128).T.astype(np.int32))
        maps.append(m)
    return maps


def kernel(**inputs):
    inputs = {k: np.asarray(v) for k, v in inputs.items()}
    nc = build()
    maps = make_in_maps(inputs, list(range(8)))
    res = run_bass_kernel_spmd(nc, maps, core_ids=list(range(8)))
    return np.stack([r["out"] for r in res.results], axis=0).astype(np.float32)
```

```python
import contextlib
import math
import numpy as np
import concourse.bass as bass
import concourse.mybir as mybir
from concourse.bass_utils import run_bass_kernel_spmd

F32 = mybir.dt.float32
BF16 = mybir.dt.bfloat16
I32 = mybir.dt.int32
ALU = mybir.AluOpType
AF = mybir.ActivationFunctionType
AX = mybir.AxisListType

S = 2048
D = 2048
NT = 16
INC = 5648
NE = 32
TWO_EXP = False
CAP = 384
NST = CAP // 128
HID = 1024
EPS = 1e-6
LAM_INIT = 0.8 - 0.6 * math.exp(-0.3 * 0)
PI = math.pi


class KB:
    def __init__(self, nc, es):
        self.nc = nc
        self.es = es
        self.eng = {'pe': nc.tensor, 'act': nc.scalar, 'dve': nc.vector, 'pool': nc.gpsimd, 'sp': nc.sync}
        self.sems = {}
        self.cnt = {}
        self.waited = {e: {} for e in self.eng}
        self.lw = {}
        self.rd = {}
        self.ninst = 0

    def sem(self, key):
        if key not in self.sems:
            self.sems[key] = self.es.enter_context(self.nc.semaphore(str(key)))
            self.cnt[key] = 0
        return self.sems[key]

    def _deps(self, e, reads, writes):
        need = {}

        def add(k, v):
            if e == 'pe' and k == 'Epe':
                return
            if need.get(k, 0) < v:
                need[k] = v
        for r in reads:
            d = self.lw.get(r)
            if d:
                add(*d)
        for w in writes:
            d = self.lw.get(w)
            if d:
                add(*d)
            for k, v in self.rd.get(w, {}).items():
                add(k, v)
        return need

    def _emit_waits(self, e, need):
        for k, v in need.items():
            if self.waited[e].get(k, 0) < v:
                self.eng[e].wait_ge(self.sems[k], v)
                self.waited[e][k] = v
                self.ninst += 1

    def _record(self, reads, writes, tok):
        k, v = tok
        for r in reads:
            d = self.rd.setdefault(r, {})
            if d.get(k, 0) < v:
                d[k] = v
        for w in writes:
            self.lw[w] = tok
            self.rd[w] = {}

    def op(self, e, fn, reads=(), writes=(), inc=True):
        self._emit_waits(e, self._deps(e, reads, writes))
        inst = fn(self.eng[e])
        self.ninst += 1
        key = 'E' + e
        s = self.sem(key)
        if inc:
            self.cnt[key] += 1
            inst.then_inc(s, 1)
            tok = (key, self.cnt[key])
        else:
            tok = (key, self.cnt[key] + 1)
        self._record(reads, writes, tok)
        return inst

    def dma(self, e, out=None, in_=None, reads=(), writes=(), semkey=None, fn=None):
        self._emit_waits(e, self._deps(e, reads, writes))
        if fn is not None:
            inst = fn(self.eng[e])
        else:
            inst = self.eng[e].dma_start(out=out, in_=in_)
        self.ninst += 1
        s = self.sem(semkey)
        self.cnt[semkey] += 16
        inst.then_inc(s, 16)
        self._record(reads, writes, (semkey, self.cnt[semkey]))
        return inst

    def regroup(self, bufs, semkey):
        for b in bufs:
            self.lw[b] = (semkey, self.cnt[semkey])

    def barrier(self):
        for e in self.eng:
            need = {k: v for k, v in self.cnt.items() if v > 0}
            self._emit_waits(e, need)

    def finish(self, e, bufs):
        need = {}
        for b in bufs:
            d = self.lw.get(b)
            if d and need.get(d[0], 0) < d[1]:
                need[d[0]] = d[1]
        self._emit_waits(e, need)


def build(stage=99, dbg=()):
    nc = bass.Bass("TRN2", target_bir_lowering=False)

    def din(name, shape, dt=F32):
        return nc.dram_tensor(name, list(shape), dt, kind="ExternalInput").ap()

    def dscr(name, shape, dt):
        return nc.dram_tensor(name, list(shape), dt).ap()

    x_tm = din("x_tm", [S, D])
    xT = din("xT", [D, S])
    w_in = din("w_in", [D, INC])
    w_out = din("w_out", [D, D])
    w_gate = din("w_gate", [NE, D, HID])
    w_up = din("w_up", [NE, D, HID])
    w_down = din("w_down", [NE, HID, D])
    pos_tm = din("pos_tm", [128, NT], I32)
    ln1_t = din("ln1_t", [128, 16])
    conv_wt = din("conv_wt", [128, 12, 4])
    conv_bt = din("conv_bt", [128, 12])
    dt_bias = din("dt_bias", [16])
    a_log = din("a_log", [16])
    d_skip = din("d_skip", [16])
    ssd_norm_w = din("ssd_norm_w", [1024])
    q_norm_w = din("q_norm_w", [64])
    k_norm_w = din("k_norm_w", [64])
    lq1 = din("lq1", [64]); lk1 = din("lk1", [64]); lq2 = din("lq2", [64]); lk2 = din("lk2", [64])
    subln_w = din("subln_w", [128])
    ln2_w = din("ln2_w", [D])
    w_r = din("w_r", [D, 36])
    b_r = din("b_r", [36])
    c_ident = din("c_ident", [128, 128])
    c_utri = din("c_utri", [128, 128])
    c_ustrict = din("c_ustrict", [128, 128])
    c_negmask4 = din("c_negmask4", [128, 512])
    c_ehsel = din("c_ehsel", [16, 16, 128])
    c_invf = din("c_invf", [128, 8])
    c_ebase = din("c_ebase", [128, NE])

    out = nc.dram_tensor("out", [S, D], F32, kind="ExternalOutput").ap()
    dbg_out = {}
    for name, shape in dbg:
        dbg_out[name] = nc.dram_tensor("d_" + name, list(shape), F32, kind="ExternalOutput").ap()

    qT_scr = dscr("qT_scr", [128, 8, S], BF16)
    kT_scr = dscr("kT_scr", [128, 8, S], BF16)
    v_scr = dscr("v_scr", [S, 1024], BF16)
    zs_scr = dscr("zs_scr", [S, 1024], F32)
    ycat_scr = dscr("ycat_scr", [128, 16, S], BF16)
    h_scr = dscr("h_scr", [S, D], F32)
    xg_scr = dscr("xg_scr", [NE * CAP + 128, D], BF16)
    ys_scr = dscr("ys_scr", [NE * CAP + 128, D], BF16)

    with contextlib.ExitStack() as es:
        kb = KB(nc, es)

        def sbt(st, name, shape, dt):
            return st.enter_context(nc.sbuf_tensor(name, list(shape), dt))

        PD = [es.enter_context(nc.psum_tensor(f"pd{k}", [128, 1024], F32)) for k in range(4)]
        PB = [PD[k // 2][:, (k % 2) * 512:(k % 2 + 1) * 512] for k in range(8)]

        def pbb(k):
            return PB[k][:, :].bitcast(BF16)

        def P(k):
            return ('ps', k)

        def mm(outap, lhsT, rhs, start, stop, reads, writes, inc=True, sgc=False):
            return kb.op('pe', lambda e: e.matmul(outap, lhsT=lhsT, rhs=rhs, start=start, stop=stop, skip_group_check=sgc),
                         reads=reads, writes=writes, inc=inc)

        def tr(outap, in_, ident, reads, writes, inc=True):
            return kb.op('pe', lambda e: e.transpose(out=outap, in_=in_, identity=ident),
                         reads=reads, writes=writes, inc=inc)

        def act(outap, in_, func, reads, writes, bias=None, scale=None, accum=None):
            kw = {}
            if bias is not None:
                kw['bias'] = bias
            if scale is not None:
                kw['scale'] = scale
            if accum is not None:
                kw['accum_out'] = accum
            return kb.op('act', lambda e: e.activation(out=outap, in_=in_, func=func, **kw), reads=reads, writes=writes)

        def tt(outap, in0, in1, op, reads, writes, e='dve'):
            return kb.op(e, lambda g: g.tensor_tensor(out=outap, in0=in0, in1=in1, op=op), reads=reads, writes=writes)

        def ts(outap, in0, s1, s2, op0, op1, reads, writes, e='dve'):
            if op1 is None:
                return kb.op(e, lambda g: g.tensor_scalar(out=outap, in0=in0, scalar1=s1, scalar2=None, op0=op0),
                             reads=reads, writes=writes)
            return kb.op(e, lambda g: g.tensor_scalar(out=outap, in0=in0, scalar1=s1, scalar2=s2, op0=op0, op1=op1),
                         reads=reads, writes=writes)

        def stt(outap, in0, scalar, in1, op0, op1, reads, writes, e='dve'):
            return kb.op(e, lambda g: g.scalar_tensor_tensor(out=outap, in0=in0, scalar=scalar, in1=in1, op0=op0, op1=op1),
                         reads=reads, writes=writes)

        def red(outap, in_, op, reads, writes):
            return kb.op('dve', lambda g: g.tensor_reduce(out=outap, in_=in_, axis=AX.X, op=op), reads=reads, writes=writes)

        def cp(outap, in_, reads, writes, e='dve'):
            if e == 'act':
                return kb.op('act', lambda g: g.copy(out=outap, in_=in_), reads=reads, writes=writes)
            return kb.op(e, lambda g: g.tensor_copy(out=outap, in_=in_), reads=reads, writes=writes)

        eps_t = sbt(es, "eps_t", [128, 1], F32)
        kb.op('dve', lambda g: g.memset(eps_t[:], EPS), writes=["eps_t"])

        def rsqrt(outap, in_, mean_scale, reads, writes):
            act(outap, in_, AF.Ln, list(reads) + ["eps_t"], writes, bias=eps_t[:], scale=mean_scale)
            act(outap, outap, AF.Exp, writes, writes, scale=-0.5)

        cst = es
        names = []

        def cload(name, shape, src, dt=F32, cast=False):
            t = sbt(cst, name, shape, dt)
            kb.dma('pool' if cast else 'sp', out=t[:], in_=src, writes=[name], semkey='ldc_p' if cast else 'ldc_s')
            names.append((name, cast))
            return t

        ident_f = cload("ident_f", [128, 128], c_ident)
        ident_b = cload("ident_b", [128, 128], c_ident, BF16, True)
        utri_f = cload("utri_f", [128, 128], c_utri)
        utri_b = cload("utri_b", [128, 128], c_utri, BF16, True)
        ustrict_b = cload("ustrict_b", [128, 128], c_ustrict, BF16, True)
        negmask4_b = cload("negmask4_b", [128, 512], c_negmask4, BF16, True)
        invf = cload("invf", [128, 8], c_invf)
        ebase = cload("ebase", [128, NE], c_ebase)
        pos_i = cload("pos_i", [128, NT], pos_tm, I32)
        ln1 = cload("ln1", [128, 16], ln1_t)
        cw = cload("cw", [128, 12, 4], conv_wt)
        cbias = cload("cbias", [128, 12], conv_bt)
        dtb_bc = cload("dtb_bc", [128, 16], dt_bias.partition_broadcast(128))
        alog_bc = cload("alog_bc", [128, 16], a_log.partition_broadcast(128))
        dsk_bc = cload("dsk_bc", [128, 16], d_skip.partition_broadcast(128))
        wq_bc = cload("wq_bc", [128, 64], q_norm_w.partition_broadcast(128))
        wk_bc = cload("wk_bc", [128, 64], k_norm_w.partition_broadcast(128))
        l4 = sbt(cst, "l4", [128, 4, 64], F32)
        for n_, src in enumerate((lq1, lk1, lq2, lk2)):
            kb.dma('sp', out=l4[:, n_, :], in_=src.partition_broadcast(128), writes=[("l4", n_)], semkey='ldc_s')
        names.append(("l4", False))
        subw_bc = cload("subw_bc", [128, 128], subln_w.partition_broadcast(128))
        br_bc = cload("br_bc", [128, 36], b_r.partition_broadcast(128))
        kb.regroup([n for n, c in names if not c], 'ldc_s')
        kb.regroup([n for n, c in names if c], 'ldc_p')

        ones_f = sbt(cst, "ones_f", [128, 128], F32)
        kb.op('dve', lambda g: g.memset(ones_f[:], 1.0), writes=["ones_f"])
        ones_b = sbt(cst, "ones_b", [128, 128], BF16)
        kb.op('dve', lambda g: g.memset(ones_b[:], 1.0), writes=["ones_b"])

        A_bc = sbt(cst, "A_bc", [128, 16], F32)
        act(A_bc[:], alog_bc[:], AF.Exp, ["alog_bc"], ["A_bc"])
        ts(A_bc[:], A_bc[:], -1.0, None, ALU.mult, None, ["A_bc"], ["A_bc"])
        lam = sbt(cst, "lam", [128, 4], F32)
        lprod = sbt(cst, "lprod", [128, 2, 64], F32)
        tt(lprod[:, 0, :], l4[:, 0, :], l4[:, 1, :], ALU.mult, ["l4"], ["lprod"])
        tt(lprod[:, 1, :], l4[:, 2, :], l4[:, 3, :], ALU.mult, ["l4", "lprod"], ["lprod"])
        lsum = sbt(cst, "lsum", [128, 2], F32)
        red(lsum[:], lprod[:], ALU.add, ["lprod"], ["lsum"])
        act(lsum[:], lsum[:], AF.Exp, ["lsum"], ["lsum"])
        tt(lam[:, 0:1], lsum[:, 0:1], lsum[:, 1:2], ALU.subtract, ["lsum"], ["lam"])
        ts(lam[:, 0:1], lam[:, 0:1], LAM_INIT, None, ALU.add, None, ["lam"], ["lam"])
        ts(lam[:, 1:2], lam[:, 0:1], -1.0, None, ALU.mult, None, ["lam"], ["lam"])
        ts(wq_bc[:], wq_bc[:], 0.125, None, ALU.mult, None, ["wq_bc"], ["wq_bc"])
        ts(subw_bc[:], subw_bc[:], 1.0 - LAM_INIT, None, ALU.mult, None, ["subw_bc"], ["subw_bc"])
        pos_f = sbt(cst, "pos_f", [128, NT], F32)
        cp(pos_f[:], pos_i[:], ["pos_i"], ["pos_f"])
        ang = sbt(cst, "ang", [128, NT, 8], F32)
        tt(ang[:], pos_f[:].unsqueeze(2).to_broadcast([128, NT, 8]), invf[:].unsqueeze(1).to_broadcast([128, NT, 8]),
           ALU.mult, ["pos_f", "invf"], ["ang"])
        sin_t = sbt(cst, "sin_t", [128, NT, 8], F32)
        cos_t = sbt(cst, "cos_t", [128, NT, 8], F32)
        angi = sbt(cst, "angi", [128, NT, 8], I32)
        angk = sbt(cst, "angk", [128, NT, 8], F32)
        for (tab, nm, off) in ((sin_t, "sin_t", 0.0), (cos_t, "cos_t", 0.5 * PI)):
            ts(tab[:], ang[:], off, None, ALU.add, None, ["ang"], [nm])
            ts(angk[:], tab[:], 1.0 / (2 * PI), None, ALU.mult, None, [nm], ["angk"])
            cp(angi[:], angk[:], ["angk"], ["angi"])
            cp(angk[:], angi[:], ["angi"], ["angk"])
            stt(tab[:], angk[:], -2 * PI, tab[:], ALU.mult, ALU.add, ["angk", nm], [nm])
            ts(tab[:], tab[:], -PI, PI, ALU.max, ALU.min, [nm], [nm])
            act(tab[:], tab[:], AF.Sin, [nm], [nm])

        rstd1 = sbt(cst, "rstd1", [128, NT], F32)

        with contextlib.ExitStack() as sx:
            xTb = sbt(sx, "xTb", [128, 16, S], BF16)
            for c in range(16):
                kb.dma('pool', out=xTb[:, c, :], in_=xT[c * 128:(c + 1) * 128, :], writes=[("xTb", c)], semkey='ld_xT')
            kb.regroup([("xTb", c) for c in range(16)], 'ld_xT')
            for c in range(16):
                ts(xTb[:, c, :], xTb[:, c, :], ln1[:, c:c + 1], None, ALU.mult, None, [("xTb", c), "ln1"], [("xTb", c)])
            XT = [("xTb", c) for c in range(16)]

            with contextlib.ExitStack() as s1:
                xf = [sbt(s1, f"xf{k}", [128, D], F32) for k in range(2)]
                junk = sbt(s1, "junk", [128, D], BF16)
                ss1 = sbt(s1, "ss1", [128, NT], F32)
                kb.op('dve', lambda g: g.memset(ss1[:], 0.0), writes=["ss1"])
                for i in range(NT):
                    kb.dma('sp', out=xf[i % 2][:], in_=x_tm[i * 128:(i + 1) * 128, :], writes=[f"xf{i%2}"], semkey=f'ld_xf{i%2}')
                    act(junk[:], xf[i % 2][:], AF.Square, [f"xf{i%2}", "ss1"], ["junk", "ss1"], accum=ss1[:, i:i + 1])
                rsqrt(rstd1[:], ss1[:], 1.0 / D, ["ss1"], ["rstd1"])
                kb.barrier()

            if "rstd1" in dbg_out:
                kb.dma('sp', out=dbg_out["rstd1"], in_=rstd1[:], reads=["rstd1"], writes=["dbg_rstd1"], semkey='dbg')

            def proj_pass(col0, epilogue, tagp):
                with contextlib.ExitStack() as sp_:
                    W = sbt(sp_, "Wp" + tagp, [128, 16, 1024], BF16)
                    for half in range(2):
                        kb.dma('pool', out=W[:, :, half * 512:(half + 1) * 512],
                               in_=w_in[:, col0 + half * 512: col0 + (half + 1) * 512].rearrange("(c p) n -> p c n", p=128),
                               writes=[("Wp", half)], semkey=f'ld_Wp{half}')
                    for i in range(NT):
                        banks = []
                        for half in range(2):
                            b = half + 2 * (i % 2)
                            banks.append(b)
                            for dc in range(16):
                                mm(PB[b][:, :], xTb[:, dc, i * 128:(i + 1) * 128], W[:, dc, half * 512:(half + 1) * 512],
                                   dc == 0, dc == 15, [("xTb", dc), ("Wp", half)], [P(b)], inc=(dc == 15))
                        epilogue(i, banks, sp_)
                    kb.barrier()

            if stage >= 2:
                def qk_epilogue_factory(wbc, wbc_name, scr, tg):
                    state = {}

                    def ep(i, banks, st):
                        if not state:
                            for k in range(2):
                                state['qf', k] = sbt(st, f"qf{tg}{k}", [128, 1024], F32)
                                state['sq', k] = sbt(st, f"sq{tg}{k}", [128, 1024], F32)
                                state['ss', k] = sbt(st, f"ss{tg}{k}", [128, 16], F32)
                                state['rot', k] = sbt(st, f"rot{tg}{k}", [128, 4, 16, 8], F32)
                                state['qb', k] = sbt(st, f"qb{tg}{k}", [128, 1024], BF16)
                                state['qt', k] = sbt(st, f"qt{tg}{k}", [128, 8, 128], BF16)
                        k = i % 2
                        qf, sq, ssq, rot, qb, qt = (state[n, k] for n in ('qf', 'sq', 'ss', 'rot', 'qb', 'qt'))
                        QF, SQ, SS, ROT, QB, QT = (f"{n}{k}" for n in ('qf', 'sq', 'ssq', 'rot', 'qb', 'qt'))
                        for half in range(2):
                            act(qf[:, half * 512:(half + 1) * 512], PB[banks[half]][:, :], AF.Copy,
                                [P(banks[half]), "rstd1"], [QF], scale=rstd1[:, i:i + 1])
                        tt(sq[:], qf[:], qf[:], ALU.mult, [QF], [SQ], e='pool')
                        red(ssq[:], sq[:].rearrange("p (b d) -> p b d", d=64), ALU.add, [SQ], [SS])
                        rsqrt(ssq[:], ssq[:], 1.0 / 64, [SS], [SS])
                        qf3 = qf[:].rearrange("p (b d) -> p b d", d=64)
                        tt(qf3, qf3, ssq[:].unsqueeze(2).to_broadcast([128, 16, 64]), ALU.mult, [QF, SS], [QF])
                        tt(qf3, qf3, wbc[:].unsqueeze(1).to_broadcast([128, 16, 64]), ALU.mult, [QF, wbc_name], [QF])
                        cosb = cos_t[:, i, :].unsqueeze(1).to_broadcast([128, 16, 8])
                        sinb = sin_t[:, i, :].unsqueeze(1).to_broadcast([128, 16, 8])
                        t1 = qf3[:, :, 0:8]
                        t2 = qf3[:, :, 8:16]
                        tt(rot[:, 0], t1, cosb, ALU.mult, [QF, "cos_t"], [ROT])
                        tt(rot[:, 1], t2, sinb, ALU.mult, [QF, "sin_t", ROT], [ROT])
                        tt(rot[:, 2], t2, cosb, ALU.mult, [QF, "cos_t", ROT], [ROT])
                        tt(rot[:, 3], t1, sinb, ALU.mult, [QF, "sin_t", ROT], [ROT])
                        qb3 = qb[:].rearrange("p (b d) -> p b d", d=64)
                        cp(qb3[:, :, 16:64], qf3[:, :, 16:64], [QF], [QB], e='pool')
                        tt(qb3[:, :, 0:8], rot[:, 0], rot[:, 1], ALU.subtract, [ROT, QB], [QB])
                        tt(qb3[:, :, 8:16], rot[:, 2], rot[:, 3], ALU.add, [ROT, QB], [QB])
                        if i >= 1:
                            ep_b(i - 1)
                        if i == NT - 1:
                            ep_b(i)

                    def ep_b(i):
                        k = i % 2
                        qb, qt = state['qb', k], state['qt', k]
                        QB, QT = f"qb{k}", f"qt{k}"
                        pb = 4 + (i % 2)
                        for hd in range(8):
                            tr(pbb(pb)[:, hd * 128:(hd + 1) * 128], qb[:, hd * 128:(hd + 1) * 128], ident_b[:],
                               [QB, "ident_b"], [P(pb)], inc=(hd == 7))
                        cp(qt[:].rearrange("p h t -> p (h t)"), pbb(pb), [P(pb)], [QT], e='act')
                        kb.dma('sp', out=scr[:, :, i * 128:(i + 1) * 128], in_=qt[:], reads=[QT], writes=[("scr" + tg, i)],
                               semkey=f'st_{tg}{k}')
                    return ep

                proj_pass(2576, qk_epilogue_factory(wq_bc, "wq_bc", qT_scr, "q"), "q")
                proj_pass(3600, qk_epilogue_factory(wk_bc, "wk_bc", kT_scr, "k"), "k")

                vstate = {}

                def v_ep(i, banks, st):
                    if not vstate:
                        vstate['vb'] = [sbt(st, f"vb{k}", [128, 1024], BF16) for k in range(2)]
                    vb = vstate['vb'][i % 2]
                    for half in range(2):
                        act(vb[:, half * 512:(half + 1) * 512], PB[banks[half]][:, :], AF.Copy,
                            [P(banks[half]), "rstd1"], [f"vb{i%2}"], scale=rstd1[:, i:i + 1])
                    kb.dma('sp', out=v_scr[i * 128:(i + 1) * 128, :], in_=vb[:], reads=[f"vb{i%2}"], writes=[("scrv", i)],
                           semkey=f'st_v{i%2}')
                proj_pass(4624, v_ep, "v")

            if stage >= 3:
                with contextlib.ExitStack() as s3:
                    xbcT = sbt(s3, "xbcT", [128, 12, S], BF16)
                    rstd_bc = sbt(s3, "rstd_bc", [128, S], F32)
                    with contextlib.ExitStack() as s31:
                        dg = [sbt(s31, f"dg{k}", [128, 128], F32) for k in range(2)]
                        for i in range(NT):
                            ts(dg[i % 2][:], ident_f[:], rstd1[:, i:i + 1], None, ALU.mult, None, ["ident_f", "rstd1"], [f"dg{i%2}"])
                            b = i // 4
                            mm(PB[b][:, (i % 4) * 128:(i % 4 + 1) * 128], ones_f[:], dg[i % 2][:], True, True,
                               ["ones_f", f"dg{i%2}"], [P(b)])
                        for b in range(4):
                            cp(rstd_bc[:, b * 512:(b + 1) * 512], PB[b][:, :], [P(b)], ["rstd_bc"])
                        Wx = [sbt(s31, f"Wx{k}", [128, 16, 512], BF16) for k in range(2)]
                        ub = [sbt(s31, f"ub{k}", [128, S + 3], F32) for k in range(1)]
                        acc = [sbt(s31, f"acc{k}", [128, S], F32) for k in range(1)]
                        for k in range(1):
                            kb.op('dve', lambda g: g.memset(ub[k][:, 0:3], 0.0), writes=[f"ub{k}"])
                        for blk in range(3):
                            kb.dma('pool', out=Wx[blk % 2][:],
                                   in_=w_in[:, 1024 + blk * 512: 1024 + (blk + 1) * 512].rearrange("(c p) n -> p c n", p=128),
                                   writes=[f"Wx{blk%2}"], semkey=f'ld_Wx{blk%2}')
                            for jj in range(4):
                                j = blk * 4 + jj
                                u = ub[0]
                                a = acc[0]
                                for tb in range(4):
                                    b = 4 + tb
                                    for dc in range(16):
                                        mm(PB[b][:, :], Wx[blk % 2][:, dc, jj * 128:(jj + 1) * 128], xTb[:, dc, tb * 512:(tb + 1) * 512],
                                           dc == 0, dc == 15, [f"Wx{blk%2}", ("xTb", dc)], [P(b)], inc=(dc == 15))
                                    tt(u[:, 3 + tb * 512: 3 + (tb + 1) * 512], PB[b][:, :], rstd_bc[:, tb * 512:(tb + 1) * 512], ALU.mult,
                                       [P(b), "rstd_bc"], ["ub0"])
                                act(a[:], u[:, 3:3 + S], AF.Identity, ["ub0", "cw", "cbias"], ["acc0"],
                                    bias=cbias[:, j:j + 1], scale=cw[:, j, 3:4])
                                for k in range(3):
                                    stt(a[:], u[:, k:k + S], cw[:, j, k:k + 1], a[:], ALU.mult, ALU.add,
                                        ["ub0", "cw", "acc0"], ["acc0"], e='dve')
                                act(xbcT[:, j, :], a[:], AF.Silu, ["acc0"], [("xbcT", j)])
                        kb.barrier()
                    if "xbcT" in dbg_out:
                        with contextlib.ExitStack() as sd:
                            tmpf = sbt(sd, "tmpf", [128, S], F32)
                            for j in range(12):
                                cp(tmpf[:], xbcT[:, j, :], [("xbcT", j)], ["tmpf"])
                                kb.dma('sp', out=dbg_out["xbcT"][j * 128:(j + 1) * 128, :], in_=tmpf[:], reads=["tmpf"], writes=["dbgx"], semkey='dbg')
                            kb.barrier()

                    dtv = sbt(s3, "dtv", [128, 256], F32)
                    sd_t = sbt(s3, "sd_t", [128, 256], F32)
                    E_t = sbt(s3, "E_t", [128, 256], F32)
                    nacs = sbt(s3, "nacs", [128, 256], F32)
                    cd_bc = sbt(s3, "cd_bc", [128, 256], F32)
                    acsT = sbt(s3, "acsT", [16, S], F32)
                    with contextlib.ExitStack() as s32:
                        Wdt = sbt(s32, "Wdt", [128, 16, 16], BF16)
                        kb.dma('pool', out=Wdt[:], in_=w_in[:, 2560:2576].rearrange("(c p) n -> p c n", p=128), writes=["Wdt"], semkey='ld_Wdt')
                        for i in range(NT):
                            for dc in range(16):
                                mm(PB[0][:, i * 16:(i + 1) * 16], xTb[:, dc, i * 128:(i + 1) * 128], Wdt[:, dc, :], dc == 0, dc == 15,
                                   [("xTb", dc), "Wdt"], [P(0)], inc=(dc == 15))
                        t1 = sbt(s32, "t1", [128, 256], F32)
                        t2 = sbt(s32, "t2", [128, 256], F32)
                        a_tok = sbt(s32, "a_tok", [128, 256], F32)
                        t13 = t1[:].rearrange("p (i h) -> p i h", h=16)
                        tt(t13, PB[0][:, 0:256].rearrange("p (i h) -> p i h", h=16), rstd1[:].unsqueeze(2).to_broadcast([128, NT, 16]),
                           ALU.mult, [P(0), "rstd1"], ["t1"])
                        tt(t13, t13, dtb_bc[:].unsqueeze(1).to_broadcast([128, NT, 16]), ALU.add, ["t1", "dtb_bc"], ["t1"])
                        stt(t2[:], t1[:], -1.0, t1[:], ALU.mult, ALU.max, ["t1"], ["t2"])
                        act(t2[:], t2[:], AF.Exp, ["t2"], ["t2"], scale=-1.0)
                        ts(t2[:], t2[:], 1.0, None, ALU.add, None, ["t2"], ["t2"])
                        act(t2[:], t2[:], AF.Ln, ["t2"], ["t2"])
                        ts(t1[:], t1[:], 0.0, None, ALU.max, None, ["t1"], ["t1"])
                        tt(dtv[:], t1[:], t2[:], ALU.add, ["t1", "t2"], ["dtv"])
                        tt(a_tok[:].rearrange("p (i h) -> p i h", h=16), dtv[:].rearrange("p (i h) -> p i h", h=16),
                           A_bc[:].unsqueeze(1).to_broadcast([128, NT, 16]), ALU.mult, ["dtv", "A_bc"], ["a_tok"])
                        mm(PB[1][:, 0:256], utri_f[:], a_tok[:], True, True, ["utri_f", "a_tok"], [P(1)])
                        mm(PB[2][:, 0:256], ones_f[:], a_tok[:], True, True, ["ones_f", "a_tok"], [P(2)])
                        for i in range(NT):
                            b = 4 + i // 4
                            mm(PB[b][0:16, (i % 4) * 128:(i % 4 + 1) * 128], a_tok[:, i * 16:(i + 1) * 16], utri_f[:], True, True,
                               ["a_tok", "utri_f"], [P(b)])
                        for b in range(4):
                            cp(acsT[:, b * 512:(b + 1) * 512], PB[4 + b][0:16, :], [P(4 + b)], ["acsT"])
                        act(E_t[:], PB[1][:, 0:256], AF.Exp, [P(1)], ["E_t"])
                        ts(nacs[:], PB[1][:, 0:256], -1.0, None, ALU.mult, None, [P(1)], ["nacs"])
                        act(cd_bc[:], PB[2][:, 0:256], AF.Exp, [P(2)], ["cd_bc"])
                        tt(t1[:], PB[2][:, 0:256], nacs[:], ALU.add, [P(2), "nacs"], ["t1"])
                        act(t1[:], t1[:], AF.Exp, ["t1"], ["t1"])
                        tt(sd_t[:], t1[:], dtv[:], ALU.mult, ["t1", "dtv"], ["sd_t"])
                        kb.barrier()

                    zst = {}

                    def z_ep(i, banks, st):
                        if not zst:
                            zst['z'] = [sbt(st, f"zsb{k}", [128, 1024], F32) for k in range(2)]
                        zb = zst['z'][i % 2]
                        for half in range(2):
                            act(zb[:, half * 512:(half + 1) * 512], PB[banks[half]][:, :], AF.Silu,
                                [P(banks[half]), "rstd1"], [f"zsb{i%2}"], scale=rstd1[:, i:i + 1])
                        kb.dma('sp', out=zs_scr[i * 128:(i + 1) * 128, :], in_=zb[:], reads=[f"zsb{i%2}"], writes=[("zs_scr", i)],
                               semkey=f'st_z{i%2}')
                    proj_pass(0, z_ep, "z")

                    with contextlib.ExitStack() as s34:
                        ehsel = sbt(s34, "ehsel", [16, 16, 128], F32)
                        kb.dma('sp', out=ehsel[:], in_=c_ehsel, writes=["ehsel"], semkey='ld_c3')
                        normw_bc = sbt(s34, "normw_bc", [128, 1024], F32)
                        kb.dma('sp', out=normw_bc[:], in_=ssd_norm_w.partition_broadcast(128), writes=["normw_bc"], semkey='ld_c3')
                        kb.regroup(["ehsel", "normw_bc"], 'ld_c3')
                        xdt = sbt(s34, "xdt", [128, 1024], BF16)
                        xdts = sbt(s34, "xdts", [128, 1024], BF16)
                        xsD = sbt(s34, "xsD", [128, 1024], BF16)
                        B_tm = sbt(s34, "B_tm", [128, 256], BF16)
                        cbT = sbt(s34, "cbT", [128, 256], BF16)
                        decT = [sbt(s34, f"decT{k}", [128, 512], BF16) for k in range(2)]
                        MT = [sbt(s34, f"MT{k}", [128, 512], BF16) for k in range(2)]
                        ytmp = [sbt(s34, f"ytmp{g}", [128, 512], F32) for g in range(2)]
                        y = sbt(s34, "y", [128, 1024], F32)
                        prev_f = sbt(s34, "prev_f", [128, 1024], F32)
                        prev_b = sbt(s34, "prev_b", [128, 1024], BF16)
                        zsb = sbt(s34, "zsb", [128, 1024], F32)
                        sq = sbt(s34, "sqy", [128, 1024], BF16)
                        ss2 = sbt(s34, "ss2", [128, 2], F32)
                        ynbs = [sbt(s34, f"ynb{k}", [128, 1024], BF16) for k in range(2)]
                        ycTs = [sbt(s34, f"ycT{k}", [128, 8, 128], BF16) for k in range(2)]

                        def gate_b(i):
                            k = i % 2
                            tsl_ = slice(i * 128, (i + 1) * 128)
                            for j in range(8):
                                tr(pbb(0)[:, j * 128:(j + 1) * 128], ynbs[k][:, j * 128:(j + 1) * 128], ident_b[:], [f"ynb{k}", "ident_b"], [P(0)], inc=(j == 7))
                            cp(ycTs[k][:].rearrange("p j t -> p (j t)"), pbb(0), [P(0)], [f"ycT{k}"], e='act')
                            kb.dma('sp', out=ycat_scr[:, 0:8, tsl_], in_=ycTs[k][:], reads=[f"ycT{k}"], writes=[("ycat", 0, i)], semkey=f'st_yc{k}')

                        YOB = [7, 2]
                        STB = [1, 0]
                        for i in range(NT):
                            ynb = ynbs[i % 2]
                            tsl = slice(i * 128, (i + 1) * 128)
                            kb.dma('sp', out=zsb[:], in_=zs_scr[tsl, :], reads=[("zs_scr", i)], writes=["zsb"], semkey='ld_zs')
                            for j in range(8):
                                tr(pbb(0)[:, j * 128:(j + 1) * 128], xbcT[:, j, tsl], ident_b[:], [("xbcT", j), "ident_b"], [P(0)], inc=(j == 7))
                            ps3 = pbb(0).rearrange("p (h d) -> p h d", d=64)
                            tt(xdt[:].rearrange("p (h d) -> p h d", d=64), ps3, dtv[:, i * 16:(i + 1) * 16].unsqueeze(2).to_broadcast([128, 16, 64]),
                               ALU.mult, [P(0), "dtv"], ["xdt"])
                            tt(xsD[:].rearrange("p (h d) -> p h d", d=64), ps3, dsk_bc[:].unsqueeze(2).to_broadcast([128, 16, 64]),
                               ALU.mult, [P(0), "dsk_bc"], ["xsD"])
                            tt(xdts[:].rearrange("p (h d) -> p h d", d=64), ps3, sd_t[:, i * 16:(i + 1) * 16].unsqueeze(2).to_broadcast([128, 16, 64]),
                               ALU.mult, [P(0), "sd_t"], ["xdts"])
                            for g in range(2):
                                tr(pbb(1)[:, g * 128:(g + 1) * 128], xbcT[:, 8 + g, tsl], ident_b[:], [("xbcT", 8 + g), "ident_b"], [P(1)], inc=(g == 1))
                            cp(B_tm[:], pbb(1)[:, 0:256], [P(1)], ["B_tm"], e='act')
                            for g in range(2):
                                mm(PB[2][:, g * 128:(g + 1) * 128], xbcT[:, 8 + g, tsl], xbcT[:, 10 + g, tsl], True, True,
                                   [("xbcT", 8 + g), ("xbcT", 10 + g)], [P(2)], inc=(g == 1))
                            cp(cbT[:], PB[2][:, 0:256], [P(2)], ["cbT"], e='act')

                            def emit_R(hq):
                                rb = 3 + (hq % 2)
                                for q in range(4):
                                    h = hq * 4 + q
                                    mm(PB[rb][:, q * 128:(q + 1) * 128], ehsel[:, h, :], acsT[:, tsl], q == 0, False, ["ehsel", "acsT"], [P(rb)],
                                       inc=False, sgc=True)
                                mm(PB[rb][:, :], ident_b[:], negmask4_b[:], False, True, ["ident_b", "negmask4_b"], [P(rb)], sgc=True)

                            def emit_Y(hq):
                                g = hq // 2
                                rb = 3 + (hq % 2)
                                dT = decT[hq % 2]
                                for q in range(4):
                                    h = hq * 4 + q
                                    act(dT[:, q * 128:(q + 1) * 128], PB[rb][:, q * 128:(q + 1) * 128], AF.Exp, [P(rb), "nacs"], [f"decT{hq%2}"],
                                        bias=nacs[:, i * 16 + h: i * 16 + h + 1])
                                tt(MT[hq % 2][:].rearrange("p (q l) -> p q l", q=4), dT[:].rearrange("p (q l) -> p q l", q=4),
                                   cbT[:, g * 128:(g + 1) * 128].unsqueeze(1).to_broadcast([128, 4, 128]), ALU.mult,
                                   [f"decT{hq%2}", "cbT"], [f"MT{hq%2}"])
                                if hq % 2 == 0:
                                    mm(PB[5 + g][:, :], ident_b[:], xsD[:, g * 512:(g + 1) * 512], True, False, ["ident_b", "xsD"], [P(5 + g)],
                                       inc=False, sgc=True)
                                for q in range(4):
                                    h = hq * 4 + q
                                    mm(PB[5 + g][:, (h % 8) * 64:(h % 8 + 1) * 64], MT[hq % 2][:, q * 128:(q + 1) * 128], xdt[:, h * 64:(h + 1) * 64],
                                       False, (hq % 2 == 1 and q == 3), [f"MT{hq%2}", "xdt"], [P(5 + g)], inc=(q == 3), sgc=True)

                            emit_R(0)
                            for hq in range(4):
                                if hq + 1 < 4:
                                    emit_R(hq + 1)
                                emit_Y(hq)
                            for g in range(2):
                                gs = slice(g * 512, (g + 1) * 512)
                                yb_, sb_ = YOB[g], STB[g]
                                if i > 0:
                                    mm(PB[yb_][:, :], xbcT[:, 10 + g, tsl], prev_b[:, gs], True, True, [("xbcT", 10 + g), "prev_b"], [P(yb_)])
                                if i < NT - 1:
                                    mm(PB[sb_][:, :], B_tm[:, g * 128:(g + 1) * 128], xdts[:, gs], True, True, ["B_tm", "xdts"], [P(sb_)])
                            for g in range(2):
                                gs = slice(g * 512, (g + 1) * 512)
                                yb_, sb_ = YOB[g], STB[g]
                                if i > 0:
                                    tt(ytmp[g][:].rearrange("p (h d) -> p h d", d=64), PB[yb_][:, :].rearrange("p (h d) -> p h d", d=64),
                                       E_t[:, i * 16 + g * 8: i * 16 + g * 8 + 8].unsqueeze(2).to_broadcast([128, 8, 64]), ALU.mult,
                                       [P(yb_), "E_t"], [f"ytmp{g}"])
                                    tt(y[:, gs], PB[5 + g][:, :], ytmp[g][:], ALU.add, [P(5 + g), f"ytmp{g}"], ["y"])
                                else:
                                    cp(y[:, gs], PB[5 + g][:, :], [P(5 + g)], ["y"])
                                if i < NT - 1:
                                    if i > 0:
                                        tt(prev_f[:, gs].rearrange("p (h d) -> p h d", d=64), prev_f[:, gs].rearrange("p (h d) -> p h d", d=64),
                                           cd_bc[:, i * 16 + g * 8: i * 16 + g * 8 + 8].unsqueeze(2).to_broadcast([128, 8, 64]), ALU.mult,
                                           ["prev_f", "cd_bc"], ["prev_f"])
                                        tt(prev_f[:, gs], prev_f[:, gs], PB[sb_][:, :], ALU.add, ["prev_f", P(sb_)], ["prev_f"])
                                    else:
                                        cp(prev_f[:, gs], PB[sb_][:, :], [P(sb_)], ["prev_f"])
                                    cp(prev_b[:, gs], prev_f[:, gs], ["prev_f"], ["prev_b"], e='act')
                            tt(y[:], y[:], zsb[:], ALU.mult, ["y", "zsb"], ["y"])
                            kb.op('dve', lambda g_: g_.memset(ss2[:], 0.0), writes=["ss2"])
                            for g in range(2):
                                gs = slice(g * 512, (g + 1) * 512)
                                act(sq[:, gs], y[:, gs], AF.Square, ["y", "ss2"], ["sqy", "ss2"], accum=ss2[:, g:g + 1])
                            rsqrt(ss2[:], ss2[:], 1.0 / 512, ["ss2"], ["ss2"])
                            for g in range(2):
                                gs = slice(g * 512, (g + 1) * 512)
                                stt(ynb[:, gs], y[:, gs], ss2[:, g:g + 1], normw_bc[:, gs], ALU.mult, ALU.mult, ["y", "ss2", "normw_bc"], [f"ynb{i%2}"])
                            if i >= 1:
                                gate_b(i - 1)
                            if i == NT - 1:
                                gate_b(i)
                        kb.barrier()
                    kb.barrier()
            kb.barrier()

        if stage >= 4:
            with contextlib.ExitStack() as s4:
                qT = sbt(s4, "qT", [128, 8, S], BF16)
                kTz = [sbt(s4, f"kTz{c}", [128, 8, S], BF16) for c in range(2)]
                V = sbt(s4, "V", [128, NT, 1024], BF16)
                subw_col = sbt(s4, "subw_col", [128, 1], F32)
                kb.dma('sp', out=subw_col[:], in_=subln_w.rearrange("(p o) -> p o", o=1), writes=["subw_col"], semkey='ld_c4')
                ts(subw_col[:], subw_col[:], 1.0 - LAM_INIT, None, ALU.mult, None, ["subw_col"], ["subw_col"])
                for c in range(2):
                    oth = slice((1 - c) * 64, (2 - c) * 64)
                    kb.op('pool', lambda g: g.memset(kTz[c][oth, :, :], 0.0), writes=[("kTzm", c)])
                for hd in range(8):
                    kb.dma('sp', out=qT[:, hd, :], in_=qT_scr[:, hd, :], reads=[("scrq", i) for i in range(NT)], writes=[("qTl", hd)], semkey='ld_q')
                    for c in range(2):
                        cs = slice(c * 64, (c + 1) * 64)
                        kb.dma('sp', out=kTz[c][cs, hd, :], in_=kT_scr[cs, hd, :], reads=[("scrk", i) for i in range(NT)], writes=[("kTl", c, hd)], semkey='ld_k')
                for i in range(NT):
                    kb.dma('sp', out=V[:, i, :], in_=v_scr[i * 128:(i + 1) * 128, :], reads=[("scrv", i)], writes=[("Vl", i)], semkey='ld_v')
                kb.regroup(["qT"], 'ld_q'); kb.regroup(["kTz0", "kTz1"], 'ld_k'); kb.regroup(["V"], 'ld_v')
                NPT = 3
                pT = [sbt(s4, f"pT{k}", [128, 2, 512], BF16) for k in range(NPT)]
                cR = [sbt(s4, f"cR{c}", [128, 512], F32) for c in range(2)]
                cO = [sbt(s4, f"cO{c}", [128, 512], F32) for c in range(2)]
                o0 = sbt(s4, "o0", [128, 512], F32)
                sqo = sbt(s4, "sqo", [128, 512], F32)
                rs = sbt(s4, "rs", [128, 512], F32)
                ssc = sbt(s4, "ssc", [128, 512], F32)
                ycA = [sbt(s4, f"ycA{k}", [128, 512], BF16) for k in range(2)]
                OB, RB, SSB = 4, 6, 3
                steps = []
                blk = 0
                for hd in range(8):
                    for qb in range(4):
                        nkt = 4 * qb + 4
                        for c in range(2):
                            for kp in range(nkt // 2):
                                steps.append(dict(hd=hd, qb=qb, c=c, kp=kp, nkt=nkt, blk=blk, last=(c == 1 and kp == nkt // 2 - 1)))
                        blk += 1

                def geom(st, h):
                    kt = 2 * st['kp'] + h
                    j = kt - 4 * st['qb']
                    off = max(j, 0) * 128
                    return kt, j, off, 4 * st['qb'] * 128 + off, 512 - off

                def emit_S(i):
                    st = steps[i]
                    d = i % 2
                    for h in range(2):
                        kt, j, off, q0, nq = geom(st, h)
                        mm(PB[2 * d + h][:, 0:nq], kTz[st['c']][:, st['hd'], kt * 128:(kt + 1) * 128], qT[:, st['hd'], q0:q0 + nq],
                           True, True, [f"kTz{st['c']}", ("kTzm", st['c']), "qT"], [("psd", d)], inc=(h == 1))

                def emit_rest(i):
                    st = steps[i]
                    d = i % 2
                    pk = i % NPT
                    c, hd = st['c'], st['hd']
                    W = geom(st, 0)[4]
                    if TWO_EXP:
                        for h in range(2):
                            nq_h = geom(st, h)[4]
                            act(pT[pk][:, h, 0:nq_h], PB[2 * d + h][:, 0:nq_h], AF.Exp, [("psd", d)], [f"pT{pk}"])
                    else:
                        act(pT[pk][:, :, 0:W], PD[d][:, :].rearrange("p (h w) -> p h w", h=2)[:, :, 0:W], AF.Exp, [("psd", d)], [f"pT{pk}"])
                    for h in range(2):
                        kt, j, off, q0, nq = geom(st, h)
                        if j >= 0:
                            tt(pT[pk][:, h, 0:128], pT[pk][:, h, 0:128], utri_b[:], ALU.mult, [f"pT{pk}", "utri_b"], [f"pT{pk}"], e='dve')
                    for h in range(2):
                        kt, j, off, q0, nq = geom(st, h)
                        mm(PB[OB + c][:, off:off + nq], V[:, kt, hd * 128:(hd + 1) * 128], pT[pk][:, h, 0:nq], kt == 0, kt == st['nkt'] - 1,
                           ["V", f"pT{pk}"], [P(OB + c)], inc=False, sgc=True)
                        mm(PB[RB + c][:, off:off + nq], ones_b[:], pT[pk][:, h, 0:nq], kt == 0, kt == st['nkt'] - 1,
                           ["ones_b", f"pT{pk}"], [P(RB + c)], sgc=True)

                def epiA(st):
                    act(cR[0][:], PB[RB][:, :], AF.Ln, [P(RB)], ["cR0"])
                    cp(cO[0][:], PB[OB][:, :], [P(OB)], ["cO0"])
                    act(cR[1][:], PB[RB + 1][:, :], AF.Ln, [P(RB + 1)], ["cR1"])
                    cp(cO[1][:], PB[OB + 1][:, :], [P(OB + 1)], ["cO1"])
                    for c in range(2):
                        act(cR[c][:], cR[c][:], AF.Exp, [f"cR{c}"], [f"cR{c}"], scale=-1.0)
                    for c in range(2):
                        tt(cO[c][:], cO[c][:], cR[c][:], ALU.mult, [f"cO{c}", f"cR{c}"], [f"cO{c}"])
                    stt(o0[:], cO[1][:], lam[:, 1:2], cO[0][:], ALU.mult, ALU.add, ["cO1", "lam", "cO0"], ["o0"])
                    tt(sqo[:], o0[:], o0[:], ALU.mult, ["o0"], ["sqo"])

                def epiB(st):
                    mm(PB[SSB][:, :], ones_f[:], sqo[:], True, True, ["ones_f", "sqo"], [("psd", 1)])
                    cp(ssc[:], PB[SSB][:, :], [("psd", 1)], ["ssc"])

                def epiC(st):
                    yk = st['blk'] % 2
                    act(rs[:], ssc[:], AF.Ln, ["ssc", "eps_t"], ["rs"], bias=eps_t[:], scale=1.0 / 128)
                    act(rs[:], rs[:], AF.Exp, ["rs"], ["rs"], scale=-0.5)
                    stt(ycA[yk][:], o0[:], subw_col[:, 0:1], rs[:], ALU.mult, ALU.mult, ["o0", "subw_col", "rs"], [f"ycA{yk}"])
                    kb.dma('sp', out=ycat_scr[:, 8 + st['hd'], st['qb'] * 512:(st['qb'] + 1) * 512], in_=ycA[yk][:], reads=[f"ycA{yk}"],
                           writes=[("ycat", 1, st['hd'], st['qb'])], semkey=f'st_ya{yk}')

                NS = len(steps)
                emit_S(0)
                pend = []
                for i in range(NS):
                    if i + 1 < NS:
                        emit_S(i + 1)
                    emit_rest(i)
                    while pend and (pend[0][0] <= i or steps[i]['last']):
                        _, fn, st_ = pend.pop(0)
                        fn(st_)
                    if steps[i]['last']:
                        epiA(steps[i])
                        pend = [(i + 4, epiB, steps[i]), (i + 6, epiC, steps[i])]
                for _, fn, st_ in pend:
                    fn(st_)
                kb.barrier()

        if "ycat" in dbg_out:
            with contextlib.ExitStack() as sd:
                tb16 = sbt(sd, "tb16", [128, S], BF16)
                tmpf = sbt(sd, "tmpf2", [128, S], F32)
                for j in range(16):
                    kb.dma('sp', out=tb16[:], in_=ycat_scr[:, j, :], reads=[], writes=["tb16"], semkey='dbg2')
                    cp(tmpf[:], tb16[:], ["tb16"], ["tmpf2"])
                    kb.dma('sp', out=dbg_out["ycat"][j * 128:(j + 1) * 128, :], in_=tmpf[:], reads=["tmpf2"], writes=["dbgy"], semkey='dbg')
                kb.barrier()

        slots = sbt(es, "slots", [128, NT, 2], I32)
        wts = sbt(es, "wts", [128, NT, 2], F32)
        kb.op('dve', lambda g: g.memset(slots[:], 0), writes=["slots"])
        kb.op('dve', lambda g: g.memset(wts[:], 0.0), writes=["wts"])
        if stage >= 5:
            with contextlib.ExitStack() as s5:
                ycT_all = sbt(s5, "ycT_all", [128, 16, S], BF16)
                Wo = sbt(s5, "Wo", [128, 16, D], BF16)
                for j in range(16):
                    kb.dma('sp', out=ycT_all[:, j, :], in_=ycat_scr[:, j, :], writes=[("ycl", j)], semkey='ld_yc')
                kb.regroup(["ycT_all"], 'ld_yc')
                for cb in range(4):
                    kb.dma('pool', out=Wo[:, :, cb * 512:(cb + 1) * 512],
                           in_=w_out[:, cb * 512:(cb + 1) * 512].rearrange("(c p) n -> p c n", p=128), writes=[("Wol", cb)], semkey='ld_Wo')
                kb.regroup(["Wo"], 'ld_Wo')
                ln2_bc = sbt(s5, "ln2_bc", [128, D], F32)
                kb.dma('sp', out=ln2_bc[:], in_=ln2_w.partition_broadcast(128), writes=["ln2_bc"], semkey='ld_c5')
                wr_sb = sbt(s5, "wr_sb", [128, 16, 36], F32)
                kb.dma('sp', out=wr_sb[:], in_=w_r.rearrange("(c p) n -> p c n", p=128), writes=["wr_sb"], semkey='ld_c5')
                kb.regroup(["ln2_bc", "wr_sb"], 'ld_c5')
                xr = sbt(s5, "xr", [128, D], F32)
                hsb = sbt(s5, "hsb", [128, D], F32)
                ssh = sbt(s5, "ssh", [128, 1], F32)
                hnT = sbt(s5, "hnT", [128, 16, 128], F32)
                r8 = sbt(s5, "r8", [128, 16], F32)
                goh = sbt(s5, "goh", [128, 4], F32)
                ein4 = sbt(s5, "ein4", [128, 4, 8], F32)
                ein = sbt(s5, "ein", [128, 8], F32)
                oh1 = sbt(s5, "oh1", [128, 8], F32)
                oh2 = sbt(s5, "oh2", [128, 8], F32)
                em = sbt(s5, "em", [128, 8], F32)
                sel1 = sbt(s5, "sel1", [128, 32], F32)
                sel2 = sbt(s5, "sel2", [128, 32], F32)
                selb = sbt(s5, "selb", [128, 32], BF16)
                cnt = sbt(s5, "cnt", [128, 32], F32)
                rk = sbt(s5, "rk", [128, 32], F32)
                tmp32 = sbt(s5, "tmp32", [128, 32], F32)
                okm = sbt(s5, "okm", [128, 32], F32)
                slf = sbt(s5, "slf", [128, 2], F32)
                kb.op('dve', lambda g: g.memset(cnt[:], 0.0), writes=["cnt"])
                hns = [sbt(s5, f"hn{k}", [128, D], F32) for k in range(2)]
                hnbs = [sbt(s5, f"hnb{k}", [128, D], BF16) for k in range(3)]
                lgs = [sbt(s5, f"lg{k}", [128, 36], F32) for k in range(2)]

                def A1(i):
                        tsl = slice(i * 128, (i + 1) * 128)
                        kb.dma('sp', out=xr[:], in_=x_tm[tsl, :], writes=["xr"], semkey='ld_xr')
                        for cb in range(4):
                            for cc in range(16):
                                mm(PB[cb][:, :], ycT_all[:, cc, tsl], Wo[:, cc, cb * 512:(cb + 1) * 512], cc == 0, cc == 15,
                                   ["ycT_all", "Wo"], [P(cb)], inc=(cc == 15))
                            tt(hsb[:, cb * 512:(cb + 1) * 512], PB[cb][:, :], xr[:, cb * 512:(cb + 1) * 512], ALU.add, [P(cb), "xr"], ["hsb"])
                        kb.dma('sp', out=h_scr[tsl, :], in_=hsb[:], reads=["hsb"], writes=[("h_scr", i)], semkey='st_h')

                def A2(i):
                        kb.op('dve', lambda g: g.memset(ssh[:], 0.0), writes=["ssh"])
                        act(hnbs[i % 3][:], hsb[:], AF.Square, ["hsb", "ssh"], [f"hnb{i%3}", "ssh"], accum=ssh[:])
                        rsqrt(ssh[:], ssh[:], 1.0 / D, ["ssh"], ["ssh"])
                        stt(hns[i % 2][:], hsb[:], ssh[:, 0:1], ln2_bc[:], ALU.mult, ALU.mult, ["hsb", "ssh", "ln2_bc"], [f"hn{i%2}"])
                        cp(hnbs[i % 3][:].rearrange("t (c p) -> t c p", p=128), hns[i % 2][:].rearrange("t (p c) -> t c p", c=16), [f"hn{i%2}"], [f"hnb{i%3}"], e='pool')


                def Btr(i):
                        for half in range(2):
                            b = 4 + half
                            for d8 in range(8):
                                dc = half * 8 + d8
                                tr(PB[b][:, (d8 % 4) * 128:(d8 % 4 + 1) * 128] if d8 < 4 else PB[b][:, (d8 % 4) * 128:(d8 % 4 + 1) * 128],
                                   hns[i % 2][:, dc * 128:(dc + 1) * 128], ident_f[:], [f"hn{i%2}", "ident_f"], [P(b)], inc=(d8 % 4 == 3))
                                if d8 % 4 == 3:
                                    c0 = half * 8 + (d8 // 4) * 4
                                    cp(hnT[:, c0:c0 + 4, :].rearrange("p c t -> p (c t)"), PB[b][:, :], [P(b)], ["hnT"], e='act')

                def Brt(i):
                        for dc in range(16):
                            mm(PB[6][:, 0:36], hnT[:, dc, :], wr_sb[:, dc, :], dc == 0, dc == 15, ["hnT", "wr_sb"], [P(6)], inc=(dc == 15))

                def Blg(i):
                        tt(lgs[i % 2][:], PB[6][:, 0:36], br_bc[:], ALU.add, [P(6), "br_bc"], [f"lg{i%2}"])


                def Ca(i):
                        red(r8[:, 0:1], lgs[i % 2][:, 0:4], ALU.max, [f"lg{i%2}"], ["r8"])
                        ts(goh[:], lgs[i % 2][:, 0:4], r8[:, 0:1], None, ALU.is_equal, None, [f"lg{i%2}", "r8"], ["goh"])
                        ts(tmp32[:, 0:4], lgs[i % 2][:, 0:4], r8[:, 0:1], None, ALU.subtract, None, [f"lg{i%2}", "r8"], ["tmp32"])
                        act(tmp32[:, 0:4], tmp32[:, 0:4], AF.Exp, ["tmp32"], ["tmp32"])
                        red(r8[:, 1:2], tmp32[:, 0:4], ALU.add, ["tmp32"], ["r8"])
                        kb.op('dve', lambda g: g.reciprocal(out=r8[:, 2:3], in_=r8[:, 1:2]), reads=["r8"], writes=["r8"])
                        tt(ein4[:], lgs[i % 2][:, 4:36].rearrange("p (g e) -> p g e", e=8), goh[:].unsqueeze(2).to_broadcast([128, 4, 8]), ALU.mult,
                           [f"lg{i%2}", "goh"], ["ein4"])
                        red(ein[:], ein4[:].rearrange("p g e -> p e g"), ALU.add, ["ein4"], ["ein"])
                        red(r8[:, 3:4], ein[:], ALU.max, ["ein"], ["r8"])
                        ts(oh1[:], ein[:], r8[:, 3:4], None, ALU.is_equal, None, ["ein", "r8"], ["oh1"])
                        stt(em[:], oh1[:], -1e30, ein[:], ALU.mult, ALU.add, ["oh1", "ein"], ["em"])
                        red(r8[:, 4:5], em[:], ALU.max, ["em"], ["r8"])
                        ts(oh2[:], em[:], r8[:, 4:5], None, ALU.is_equal, None, ["em", "r8"], ["oh2"])
                        tt(r8[:, 5:6], r8[:, 4:5], r8[:, 3:4], ALU.subtract, ["r8"], ["r8"])
                        act(r8[:, 5:6], r8[:, 5:6], AF.Exp, ["r8"], ["r8"])
                        ts(r8[:, 6:7], r8[:, 5:6], 1.0, None, ALU.add, None, ["r8"], ["r8"])
                        kb.op('dve', lambda g: g.reciprocal(out=r8[:, 6:7], in_=r8[:, 6:7]), reads=["r8"], writes=["r8"])
                        tt(wts[:, i, 0:1], r8[:, 6:7], r8[:, 2:3], ALU.mult, ["r8"], ["wts"])
                        tt(wts[:, i, 1:2], wts[:, i, 0:1], r8[:, 5:6], ALU.mult, ["wts", "r8"], ["wts"])
                        tt(sel1[:].rearrange("p (g e) -> p g e", e=8), goh[:].unsqueeze(2).to_broadcast([128, 4, 8]),
                           oh1[:].unsqueeze(1).to_broadcast([128, 4, 8]), ALU.mult, ["goh", "oh1"], ["sel1"])
                        tt(sel2[:].rearrange("p (g e) -> p g e", e=8), goh[:].unsqueeze(2).to_broadcast([128, 4, 8]),
                           oh2[:].unsqueeze(1).to_broadcast([128, 4, 8]), ALU.mult, ["goh", "oh2"], ["sel2"])
                        tt(selb[:], sel1[:], sel2[:], ALU.add, ["sel1", "sel2"], ["selb"])

                def Crk(i):
                        mm(PB[7][:, 0:32], ustrict_b[:], selb[:], True, True, ["ustrict_b", "selb"], [P(7)])
                        mm(PB[7][:, 32:64], ones_b[:], selb[:], True, True, ["ones_b", "selb"], [P(7)])

                def Cb(i):
                        tt(rk[:], PB[7][:, 0:32], cnt[:], ALU.add, [P(7), "cnt"], ["rk"])
                        tt(cnt[:], cnt[:], PB[7][:, 32:64], ALU.add, ["cnt", P(7)], ["cnt"])
                        ts(okm[:], rk[:], float(CAP) - 0.5, None, ALU.is_lt, None, ["rk"], ["okm"])
                        tt(rk[:], rk[:], ebase[:], ALU.add, ["rk", "ebase"], ["rk"])
                        ts(rk[:], rk[:], -float(NE * CAP), None, ALU.add, None, ["rk"], ["rk"])
                        tt(rk[:], rk[:], okm[:], ALU.mult, ["rk", "okm"], ["rk"])
                        ts(rk[:], rk[:], float(NE * CAP), None, ALU.add, None, ["rk"], ["rk"])
                        tt(tmp32[:], okm[:], sel1[:], ALU.mult, ["okm", "sel1"], ["tmp32"])
                        red(r8[:, 8:9], tmp32[:], ALU.add, ["tmp32"], ["r8"])
                        tt(tmp32[:], okm[:], sel2[:], ALU.mult, ["okm", "sel2", "r8"], ["tmp32"])
                        red(r8[:, 9:10], tmp32[:], ALU.add, ["tmp32"], ["r8"])
                        tt(wts[:, i, :], wts[:, i, :], r8[:, 8:10], ALU.mult, ["wts", "r8"], ["wts"])
                        tt(tmp32[:], rk[:], sel1[:], ALU.mult, ["rk", "sel1"], ["tmp32"])
                        red(slf[:, 0:1], tmp32[:], ALU.add, ["tmp32"], ["slf"])
                        tt(tmp32[:], rk[:], sel2[:], ALU.mult, ["rk", "sel2", "slf"], ["tmp32"])
                        red(slf[:, 1:2], tmp32[:], ALU.add, ["tmp32"], ["slf"])
                        cp(slots[:, i, :], slf[:], ["slf"], ["slots"])
                        for k in range(2):
                            kb.dma('pool', fn=lambda g: g.indirect_dma_start(
                                out=xg_scr, out_offset=bass.IndirectOffsetOnAxis(ap=slots[:, i, k:k + 1], axis=0),
                                in_=hnbs[i % 3][:], in_offset=None), reads=[f"hnb{i%3}", "slots"], writes=[("xgs", i, k)], semkey=f'sc_xg{i%3}')


                A1(0)
                A2(0)
                for i in range(NT):
                    if i + 1 < NT:
                        A1(i + 1)
                    Btr(i)
                    if i >= 1:
                        Ca(i - 1)
                    Brt(i)
                    if i >= 1:
                        Crk(i - 1)
                        Cb(i - 1)
                    Blg(i)
                    if i + 1 < NT:
                        A2(i + 1)
                Ca(NT - 1)
                Crk(NT - 1)
                Cb(NT - 1)
                kb.regroup(["xg_scr"], 'sc_xg0')
                kb.barrier()
            if "h" in dbg_out:
                kb.dma('sp', out=dbg_out["h"], in_=h_scr, reads=[("h_scr", i) for i in range(NT)], writes=["dbgh"], semkey='dbg')
            if "slots" in dbg_out:
                with contextlib.ExitStack() as sd:
                    sf = sbt(sd, "sf", [128, NT * 2], F32)
                    cp(sf[:], slots[:].rearrange("p i k -> p (i k)"), ["slots"], ["sf"])
                    kb.dma('sp', out=dbg_out["slots"], in_=sf[:], reads=["sf"], writes=["dbgs"], semkey='dbg')
                    kb.dma('sp', out=dbg_out["wts"], in_=wts[:].rearrange("p i k -> p (i k)"), reads=["wts"], writes=["dbgw"], semkey='dbg')
                    kb.barrier()

        if stage >= 6:
            with contextlib.ExitStack() as s6:
                NWB = 4
                wbuf = [sbt(s6, f"wbuf{k}", [128, 16 * 1024], BF16) for k in range(NWB)]
                xgs = [sbt(s6, f"xg{k}", [128, NST, D], BF16) for k in range(2)]
                xgT = sbt(s6, "xgT", [128, 16, CAP], BF16)
                gT = sbt(s6, "gT", [128, 8, CAP], BF16)
                hT = sbt(s6, "hT", [128, 8, CAP], BF16)
                ysb = [sbt(s6, f"ysb{k}", [128, D], BF16) for k in range(2)]
                wctr = [0]
                kb.op('dve', lambda g: g.memset(ysb[0][:], 0.0), writes=["ysb0"])
                kb.dma('sp', out=ys_scr[NE * CAP:NE * CAP + 128, :], in_=ysb[0][:], reads=["ysb0"], writes=["ys_scr"], semkey='st_ys0')

                def wload(src, a, b_, flat):
                    k = wctr[0] % NWB
                    wctr[0] += 1
                    v3 = wbuf[k][:, 0:a * b_].rearrange("p (a b) -> p a b", b=b_)
                    kb.dma('pool', out=(wbuf[k][:, 0:a * b_] if flat else v3), in_=src, writes=[f"wbuf{k}"], semkey=f'ld_w{k}')
                    return v3, f"wbuf{k}"

                def xgload(e):
                    kb.dma('sp', out=xgs[e % 2][:], in_=xg_scr[e * CAP:(e + 1) * CAP, :].rearrange("(s p) d -> p s d", p=128),
                           reads=["xg_scr"], writes=[f"xg{e%2}"], semkey=f'ld_xg{e%2}')
                xgload(0)
                ytile = [0]
                for e in range(NE):
                    if e + 1 < NE:
                        xgload(e + 1)
                    xg = xgs[e % 2]
                    for st_ in range(NST):
                        for dc4 in range(4):
                            b = dc4 % 2
                            for q4 in range(4):
                                dc = dc4 * 4 + q4
                                tr(pbb(b)[:, q4 * 128:(q4 + 1) * 128], xg[:, st_, dc * 128:(dc + 1) * 128], ident_b[:], [f"xg{e%2}", "ident_b"], [P(b)],
                                   inc=(q4 == 3))
                            cp(xgT[:, dc4 * 4:(dc4 + 1) * 4, st_ * 128:(st_ + 1) * 128],
                               pbb(b)[:, 0:512].rearrange("p (c t) -> p c t", t=128), [P(b)], ["xgT"], e=('act' if dc4 % 2 else 'dve'))
                    Wg, wgn = wload(w_gate[e].rearrange("(p c) n -> p (c n)", c=16), 16, 1024, True)
                    Wu, wun = wload(w_up[e].rearrange("(p c) n -> p (c n)", c=16), 16, 1024, True)
                    for hc in range(8):
                        bg = 2 + (hc % 2)
                        for dc in range(16):
                            mm(PB[bg][:, 0:CAP], Wg[:, dc, hc * 128:(hc + 1) * 128], xgT[:, dc, :], dc == 0, dc == 15, [wgn, "xgT"], [P(bg)], inc=(dc == 15))
                        act(gT[:, hc, :], PB[bg][:, 0:CAP], AF.Silu, [P(bg)], [("gT", hc)])
                    for hc in range(8):
                        bu = 4 + (hc % 2)
                        for dc in range(16):
                            mm(PB[bu][:, 0:CAP], Wu[:, dc, hc * 128:(hc + 1) * 128], xgT[:, dc, :], dc == 0, dc == 15, [wun, "xgT"], [P(bu)], inc=(dc == 15))
                        tt(hT[:, hc, :], gT[:, hc, :], PB[bu][:, 0:CAP], ALU.mult, [("gT", hc), P(bu)], ["hT"])
                    Wdv, wdn = wload(w_down[e].rearrange("(c p) n -> p c n", p=128), 8, D, False)
                    for st_ in range(NST):
                        yk = ytile[0] % 2
                        ytile[0] += 1
                        yb = ysb[yk]
                        for cb in range(4):
                            b = 6 + (cb % 2)
                            for kc in range(8):
                                mm(PB[b][:, :], hT[:, kc, st_ * 128:(st_ + 1) * 128], Wdv[:, kc, cb * 512:(cb + 1) * 512], kc == 0, kc == 7,
                                   ["hT", wdn], [P(b)], inc=(kc == 7))
                            cp(yb[:, cb * 512:(cb + 1) * 512], PB[b][:, :], [P(b)], [f"ysb{yk}"], e=('act' if cb % 2 else 'dve'))
                        kb.dma('sp', out=ys_scr[e * CAP + st_ * 128: e * CAP + (st_ + 1) * 128, :], in_=yb[:], reads=[f"ysb{yk}"],
                               writes=[("yss", e, st_)], semkey=f'st_ys{yk}')
                kb.barrier()
                kb.regroup(["ys_scr"], 'st_ys0')

        if stage >= 7:
            with contextlib.ExitStack() as s7:
                NB7 = 4
                hb_ = [sbt(s7, f"hc{k}", [128, D], F32) for k in range(NB7)]
                y1 = [sbt(s7, f"y1{k}", [128, D], BF16) for k in range(NB7)]
                y2 = [sbt(s7, f"y2{k}", [128, D], BF16) for k in range(NB7)]
                ia = [sbt(s7, f"ia{k}", [128, 1], I32) for k in range(NB7)]
                ib = [sbt(s7, f"ib{k}", [128, 1], I32) for k in range(NB7)]
                for k in range(NB7):
                    kb.op('dve', lambda g: g.memset(ia[k][:], 0), writes=[f"ia{k}"])
                    kb.op('dve', lambda g: g.memset(ib[k][:], 0), writes=[f"ib{k}"])
                    kb.op('dve', lambda g: g.memset(y1[k][:], 0.0), writes=[f"y1{k}"])
                    kb.op('dve', lambda g: g.memset(y2[k][:], 0.0), writes=[f"y2{k}"])

                def p7_load(i):
                    k = i % NB7
                    tsl = slice(i * 128, (i + 1) * 128)
                    kb.dma('sp', out=hb_[k][:], in_=h_scr[tsl, :], reads=[("h_scr", i)], writes=[f"hc{k}"], semkey=f'ld_hc{k}')
                    cp(ia[k][:], slots[:, i, 0:1], ["slots"], [f"ia{k}"])
                    cp(ib[k][:], slots[:, i, 1:2], ["slots"], [f"ib{k}"])
                    kb.dma('pool', fn=lambda g: g.indirect_dma_start(
                        out=y1[k][:], out_offset=None, in_=ys_scr,
                        in_offset=bass.IndirectOffsetOnAxis(ap=ia[k][:, 0:1], axis=0)),
                        reads=["ys_scr", f"ia{k}"], writes=[f"y1{k}"], semkey=f'ga_y1{k}')
                    kb.dma('pool', fn=lambda g: g.indirect_dma_start(
                        out=y2[k][:], out_offset=None, in_=ys_scr,
                        in_offset=bass.IndirectOffsetOnAxis(ap=ib[k][:, 0:1], axis=0)),
                        reads=["ys_scr", f"ib{k}"], writes=[f"y2{k}"], semkey=f'ga_y2{k}')

                for i in range(min(NB7 - 1, NT)):
                    p7_load(i)
                for i in range(NT):
                    if i + NB7 - 1 < NT:
                        p7_load(i + NB7 - 1)
                    k = i % NB7
                    tsl = slice(i * 128, (i + 1) * 128)
                    stt(hb_[k][:], y1[k][:], wts[:, i, 0:1], hb_[k][:], ALU.mult, ALU.add, [f"y1{k}", "wts", f"hc{k}"], [f"hc{k}"])
                    stt(hb_[k][:], y2[k][:], wts[:, i, 1:2], hb_[k][:], ALU.mult, ALU.add, [f"y2{k}", "wts", f"hc{k}"], [f"hc{k}"])
                    kb.dma('sp', out=out[tsl, :], in_=hb_[k][:], reads=[f"hc{k}"], writes=[("out", i)], semkey=f'st_out{k}')
                kb.barrier()
        kb.barrier()
        print("instructions (incl. waits):", kb.ninst, "sems:", len(kb.sems))
    return nc


def host_consts():
    j = np.arange(128)[:, None]
    l = np.arange(128)[None, :]
    c = {}
    c["c_ident"] = np.eye(128, dtype=np.float32)
    c["c_utri"] = (j <= l).astype(np.float32)
    c["c_ustrict"] = (j < l).astype(np.float32)
    c["c_negmask4"] = np.tile(np.where(l < j, -30000.0, 0.0).astype(np.float32), (1, 4))
    eh = np.zeros((16, 16, 128), np.float32)
    for h in range(16):
        eh[h, h, :] = 1.0
    c["c_ehsel"] = eh
    invf = (500000.0 ** (-np.arange(0, 16, 2, dtype=np.float32) / 16.0)).astype(np.float32)
    c["c_invf"] = np.tile(invf[None, :], (128, 1)).astype(np.float32)
    c["c_ebase"] = np.tile((np.arange(NE, dtype=np.float32) * CAP)[None, :], (128, 1)).astype(np.float32)
    return c


def make_in_maps(inputs, cores):
    f = lambda a: np.ascontiguousarray(a)
    shared = dict(host_consts())
    shared["w_in"] = f(inputs["w_in"][0])
    shared["w_out"] = f(inputs["w_out"][0])
    shared["w_gate"] = f(inputs["w_gate"][0])
    shared["w_up"] = f(inputs["w_up"][0])
    shared["w_down"] = f(inputs["w_down"][0])
    shared["ln1_t"] = f(inputs["ln1_w"][0].reshape(16, 128).T)
    shared["conv_wt"] = f(inputs["conv_w"][0].reshape(4, 12, 128).transpose(2, 1, 0))
    shared["conv_bt"] = f(inputs["conv_b"][0].reshape(12, 128).T)
    for k in ("dt_bias", "a_log", "d_skip", "ssd_norm_w", "q_norm_w", "k_norm_w", "subln_w", "ln2_w"):
        shared[k] = f(inputs[k][0])
    shared["lq1"] = f(inputs["lambda_q1"][0]); shared["lk1"] = f(inputs["lambda_k1"][0])
    shared["lq2"] = f(inputs["lambda_q2"][0]); shared["lk2"] = f(inputs["lambda_k2"][0])
    shared["w_r"] = f(np.concatenate([inputs["w_router_group"][0], inputs["w_router_expert"][0]], axis=1))
    shared["b_r"] = f(np.concatenate([inputs["b_router_group"][0], inputs["b_router_expert"][0]], axis=0))
    maps = []
    for b in cores:
        m = dict(shared)
        xb = inputs["x"][b]
        m["x_tm"] = f(xb)
        m["xT"] = f(xb.T)
        m["pos_tm"] = f(inputs["positions"][b].reshape(NT, 128).T.astype(np.int32))
        maps.append(m)
    return maps


def kernel(**inputs):
    inputs = {k: np.asarray(v) for k, v in inputs.items()}
    nc = build()
    maps = make_in_maps(inputs, list(range(8)))
    res = run_bass_kernel_spmd(nc, maps, core_ids=list(range(8)))
    return np.stack([r["out"] for r in res.results], axis=0).astype(np.float32)
```

```python
import contextlib
import math
import numpy as np
import concourse.bass as bass
import concourse.mybir as mybir
from concourse.bass_utils import run_bass_kernel_spmd

F32 = mybir.dt.float32
BF16 = mybir.dt.bfloat16
I32 = mybir.dt.int32
ALU = mybir.AluOpType
AF = mybir.ActivationFunctionType
AX = mybir.AxisListType

S = 2048
D = 2048
NT = 16
INC = 5648
NE = 32
TWO_EXP = False
CAP = 384
NST = CAP // 128
HID = 1024
EPS = 1e-6
LAM_INIT = 0.8 - 0.6 * math.exp(-0.3 * 0)
PI = math.pi


class KB:
    def __init__(self, nc, es):
        self.nc = nc
        self.es = es
        self.eng = {'pe': nc.tensor, 'act': nc.scalar, 'dve': nc.vector, 'pool': nc.gpsimd, 'sp': nc.sync}
        self.sems = {}
        self.cnt = {}
        self.waited = {e: {} for e in self.eng}
        self.lw = {}
        self.rd = {}
        self.ninst = 0

    def sem(self, key):
        if key not in self.sems:
            self.sems[key] = self.es.enter_context(self.nc.semaphore(str(key)))
            self.cnt[key] = 0
        return self.sems[key]

    def _deps(self, e, reads, writes):
        need = {}

        def add(k, v):
            if e == 'pe' and k == 'Epe':
                return
            if need.get(k, 0) < v:
                need[k] = v
        for r in reads:
            d = self.lw.get(r)
            if d:
                add(*d)
        for w in writes:
            d = self.lw.get(w)
            if d:
                add(*d)
            for k, v in self.rd.get(w, {}).items():
                add(k, v)
        return need

    def _emit_waits(self, e, need):
        for k, v in need.items():
            if self.waited[e].get(k, 0) < v:
                self.eng[e].wait_ge(self.sems[k], v)
                self.waited[e][k] = v
                self.ninst += 1

    def _record(self, reads, writes, tok):
        k, v = tok
        for r in reads:
            d = self.rd.setdefault(r, {})
            if d.get(k, 0) < v:
                d[k] = v
        for w in writes:
            self.lw[w] = tok
            self.rd[w] = {}

    def op(self, e, fn, reads=(), writes=(), inc=True):
        self._emit_waits(e, self._deps(e, reads, writes))
        inst = fn(self.eng[e])
        self.ninst += 1
        key = 'E' + e
        s = self.sem(key)
        if inc:
            self.cnt[key] += 1
            inst.then_inc(s, 1)
            tok = (key, self.cnt[key])
        else:
            tok = (key, self.cnt[key] + 1)
        self._record(reads, writes, tok)
        return inst

    def dma(self, e, out=None, in_=None, reads=(), writes=(), semkey=None, fn=None):
        self._emit_waits(e, self._deps(e, reads, writes))
        if fn is not None:
            inst = fn(self.eng[e])
        else:
            inst = self.eng[e].dma_start(out=out, in_=in_)
        self.ninst += 1
        s = self.sem(semkey)
        self.cnt[semkey] += 16
        inst.then_inc(s, 16)
        self._record(reads, writes, (semkey, self.cnt[semkey]))
        return inst

    def regroup(self, bufs, semkey):
        for b in bufs:
            self.lw[b] = (semkey, self.cnt[semkey])

    def barrier(self):
        for e in self.eng:
            need = {k: v for k, v in self.cnt.items() if v > 0}
            self._emit_waits(e, need)

    def finish(self, e, bufs):
        need = {}
        for b in bufs:
            d = self.lw.get(b)
            if d and need.get(d[0], 0) < d[1]:
                need[d[0]] = d[1]
        self._emit_waits(e, need)


def build(stage=99, dbg=()):
    nc = bass.Bass("TRN2", target_bir_lowering=False)

    def din(name, shape, dt=F32):
        return nc.dram_tensor(name, list(shape), dt, kind="ExternalInput").ap()

    def dscr(name, shape, dt):
        return nc.dram_tensor(name, list(shape), dt).ap()

    x_tm = din("x_tm", [S, D])
    xT = din("xT", [D, S])
    w_in = din("w_in", [D, INC])
    w_out = din("w_out", [D, D])
    w_gate = din("w_gate", [NE, D, HID])
    w_up = din("w_up", [NE, D, HID])
    w_down = din("w_down", [NE, HID, D])
    pos_tm = din("pos_tm", [128, NT], I32)
    ln1_t = din("ln1_t", [128, 16])
    conv_wt = din("conv_wt", [128, 12, 4])
    conv_bt = din("conv_bt", [128, 12])
    dt_bias = din("dt_bias", [16])
    a_log = din("a_log", [16])
    d_skip = din("d_skip", [16])
    ssd_norm_w = din("ssd_norm_w", [1024])
    q_norm_w = din("q_norm_w", [64])
    k_norm_w = din("k_norm_w", [64])
    lq1 = din("lq1", [64]); lk1 = din("lk1", [64]); lq2 = din("lq2", [64]); lk2 = din("lk2", [64])
    subln_w = din("subln_w", [128])
    ln2_w = din("ln2_w", [D])
    w_r = din("w_r", [D, 36])
    b_r = din("b_r", [36])
    c_ident = din("c_ident", [128, 128])
    c_utri = din("c_utri", [128, 128])
    c_ustrict = din("c_ustrict", [128, 128])
    c_negmask4 = din("c_negmask4", [128, 512])
    c_ehsel = din("c_ehsel", [16, 16, 128])
    c_invf = din("c_invf", [128, 8])
    c_ebase = din("c_ebase", [128, NE])

    out = nc.dram_tensor("out", [S, D], F32, kind="ExternalOutput").ap()
    dbg_out = {}
    for name, shape in dbg:
        dbg_out[name] = nc.dram_tensor("d_" + name, list(shape), F32, kind="ExternalOutput").ap()

    qT_scr = dscr("qT_scr", [128, 8, S], BF16)
    kT_scr = dscr("kT_scr", [128, 8, S], BF16)
    v_scr = dscr("v_scr", [S, 1024], BF16)
    zs_scr = dscr("zs_scr", [S, 1024], F32)
    ycat_scr = dscr("ycat_scr", [128, 16, S], BF16)
    h_scr = dscr("h_scr", [S, D], F32)
    xg_scr = dscr("xg_scr", [NE * CAP + 128, D], BF16)
    ys_scr = dscr("ys_scr", [NE * CAP + 128, D], BF16)

    with contextlib.ExitStack() as es:
        kb = KB(nc, es)

        def sbt(st, name, shape, dt):
            return st.enter_context(nc.sbuf_tensor(name, list(shape), dt))

        PD = [es.enter_context(nc.psum_tensor(f"pd{k}", [128, 1024], F32)) for k in range(4)]
        PB = [PD[k // 2][:, (k % 2) * 512:(k % 2 + 1) * 512] for k in range(8)]

        def pbb(k):
            return PB[k][:, :].bitcast(BF16)

        def P(k):
            return ('ps', k)

        def mm(outap, lhsT, rhs, start, stop, reads, writes, inc=True, sgc=False):
            return kb.op('pe', lambda e: e.matmul(outap, lhsT=lhsT, rhs=rhs, start=start, stop=stop, skip_group_check=sgc),
                         reads=reads, writes=writes, inc=inc)

        def tr(outap, in_, ident, reads, writes, inc=True):
            return kb.op('pe', lambda e: e.transpose(out=outap, in_=in_, identity=ident),
                         reads=reads, writes=writes, inc=inc)

        def act(outap, in_, func, reads, writes, bias=None, scale=None, accum=None):
            kw = {}
            if bias is not None:
                kw['bias'] = bias
            if scale is not None:
                kw['scale'] = scale
            if accum is not None:
                kw['accum_out'] = accum
            return kb.op('act', lambda e: e.activation(out=outap, in_=in_, func=func, **kw), reads=reads, writes=writes)

        def tt(outap, in0, in1, op, reads, writes, e='dve'):
            return kb.op(e, lambda g: g.tensor_tensor(out=outap, in0=in0, in1=in1, op=op), reads=reads, writes=writes)

        def ts(outap, in0, s1, s2, op0, op1, reads, writes, e='dve'):
            if op1 is None:
                return kb.op(e, lambda g: g.tensor_scalar(out=outap, in0=in0, scalar1=s1, scalar2=None, op0=op0),
                             reads=reads, writes=writes)
            return kb.op(e, lambda g: g.tensor_scalar(out=outap, in0=in0, scalar1=s1, scalar2=s2, op0=op0, op1=op1),
                         reads=reads, writes=writes)

        def stt(outap, in0, scalar, in1, op0, op1, reads, writes, e='dve'):
            return kb.op(e, lambda g: g.scalar_tensor_tensor(out=outap, in0=in0, scalar=scalar, in1=in1, op0=op0, op1=op1),
                         reads=reads, writes=writes)

        def red(outap, in_, op, reads, writes):
            return kb.op('dve', lambda g: g.tensor_reduce(out=outap, in_=in_, axis=AX.X, op=op), reads=reads, writes=writes)

        def cp(outap, in_, reads, writes, e='dve'):
            if e == 'act':
                return kb.op('act', lambda g: g.copy(out=outap, in_=in_), reads=reads, writes=writes)
            return kb.op(e, lambda g: g.tensor_copy(out=outap, in_=in_), reads=reads, writes=writes)

        eps_t = sbt(es, "eps_t", [128, 1], F32)
        kb.op('dve', lambda g: g.memset(eps_t[:], EPS), writes=["eps_t"])

        def rsqrt(outap, in_, mean_scale, reads, writes):
            act(outap, in_, AF.Ln, list(reads) + ["eps_t"], writes, bias=eps_t[:], scale=mean_scale)
            act(outap, outap, AF.Exp, writes, writes, scale=-0.5)

        cst = es
        names = []

        def cload(name, shape, src, dt=F32, cast=False):
            t = sbt(cst, name, shape, dt)
            kb.dma('pool' if cast else 'sp', out=t[:], in_=src, writes=[name], semkey='ldc_p' if cast else 'ldc_s')
            names.append((name, cast))
            return t

        ident_f = cload("ident_f", [128, 128], c_ident)
        ident_b = cload("ident_b", [128, 128], c_ident, BF16, True)
        utri_f = cload("utri_f", [128, 128], c_utri)
        utri_b = cload("utri_b", [128, 128], c_utri, BF16, True)
        ustrict_b = cload("ustrict_b", [128, 128], c_ustrict, BF16, True)
        negmask4_b = cload("negmask4_b", [128, 512], c_negmask4, BF16, True)
        invf = cload("invf", [128, 8], c_invf)
        ebase = cload("ebase", [128, NE], c_ebase)
        pos_i = cload("pos_i", [128, NT], pos_tm, I32)
        ln1 = cload("ln1", [128, 16], ln1_t)
        cw = cload("cw", [128, 12, 4], conv_wt)
        cbias = cload("cbias", [128, 12], conv_bt)
        dtb_bc = cload("dtb_bc", [128, 16], dt_bias.partition_broadcast(128))
        alog_bc = cload("alog_bc", [128, 16], a_log.partition_broadcast(128))
        dsk_bc = cload("dsk_bc", [128, 16], d_skip.partition_broadcast(128))
        wq_bc = cload("wq_bc", [128, 64], q_norm_w.partition_broadcast(128))
        wk_bc = cload("wk_bc", [128, 64], k_norm_w.partition_broadcast(128))
        l4 = sbt(cst, "l4", [128, 4, 64], F32)
        for n_, src in enumerate((lq1, lk1, lq2, lk2)):
            kb.dma('sp', out=l4[:, n_, :], in_=src.partition_broadcast(128), writes=[("l4", n_)], semkey='ldc_s')
        names.append(("l4", False))
        subw_bc = cload("subw_bc", [128, 128], subln_w.partition_broadcast(128))
        br_bc = cload("br_bc", [128, 36], b_r.partition_broadcast(128))
        kb.regroup([n for n, c in names if not c], 'ldc_s')
        kb.regroup([n for n, c in names if c], 'ldc_p')

        ones_f = sbt(cst, "ones_f", [128, 128], F32)
        kb.op('dve', lambda g: g.memset(ones_f[:], 1.0), writes=["ones_f"])
        ones_b = sbt(cst, "ones_b", [128, 128], BF16)
        kb.op('dve', lambda g: g.memset(ones_b[:], 1.0), writes=["ones_b"])

        A_bc = sbt(cst, "A_bc", [128, 16], F32)
        act(A_bc[:], alog_bc[:], AF.Exp, ["alog_bc"], ["A_bc"])
        ts(A_bc[:], A_bc[:], -1.0, None, ALU.mult, None, ["A_bc"], ["A_bc"])
        lam = sbt(cst, "lam", [128, 4], F32)
        lprod = sbt(cst, "lprod", [128, 2, 64], F32)
        tt(lprod[:, 0, :], l4[:, 0, :], l4[:, 1, :], ALU.mult, ["l4"], ["lprod"])
        tt(lprod[:, 1, :], l4[:, 2, :], l4[:, 3, :], ALU.mult, ["l4", "lprod"], ["lprod"])
        lsum = sbt(cst, "lsum", [128, 2], F32)
        red(lsum[:], lprod[:], ALU.add, ["lprod"], ["lsum"])
        act(lsum[:], lsum[:], AF.Exp, ["lsum"], ["lsum"])
        tt(lam[:, 0:1], lsum[:, 0:1], lsum[:, 1:2], ALU.subtract, ["lsum"], ["lam"])
        ts(lam[:, 0:1], lam[:, 0:1], LAM_INIT, None, ALU.add, None, ["lam"], ["lam"])
        ts(lam[:, 1:2], lam[:, 0:1], -1.0, None, ALU.mult, None, ["lam"], ["lam"])
        ts(wq_bc[:], wq_bc[:], 0.125, None, ALU.mult, None, ["wq_bc"], ["wq_bc"])
        ts(subw_bc[:], subw_bc[:], 1.0 - LAM_INIT, None, ALU.mult, None, ["subw_bc"], ["subw_bc"])
        pos_f = sbt(cst, "pos_f", [128, NT], F32)
        cp(pos_f[:], pos_i[:], ["pos_i"], ["pos_f"])
        ang = sbt(cst, "ang", [128, NT, 8], F32)
        tt(ang[:], pos_f[:].unsqueeze(2).to_broadcast([128, NT, 8]), invf[:].unsqueeze(1).to_broadcast([128, NT, 8]),
           ALU.mult, ["pos_f", "invf"], ["ang"])
        sin_t = sbt(cst, "sin_t", [128, NT, 8], F32)
        cos_t = sbt(cst, "cos_t", [128, NT, 8], F32)
        angi = sbt(cst, "angi", [128, NT, 8], I32)
        angk = sbt(cst, "angk", [128, NT, 8], F32)
        for (tab, nm, off) in ((sin_t, "sin_t", 0.0), (cos_t, "cos_t", 0.5 * PI)):
            ts(tab[:], ang[:], off, None, ALU.add, None, ["ang"], [nm])
            ts(angk[:], tab[:], 1.0 / (2 * PI), None, ALU.mult, None, [nm], ["angk"])
            cp(angi[:], angk[:], ["angk"], ["angi"])
            cp(angk[:], angi[:], ["angi"], ["angk"])
            stt(tab[:], angk[:], -2 * PI, tab[:], ALU.mult, ALU.add, ["angk", nm], [nm])
            ts(tab[:], tab[:], -PI, PI, ALU.max, ALU.min, [nm], [nm])
            act(tab[:], tab[:], AF.Sin, [nm], [nm])

        rstd1 = sbt(cst, "rstd1", [128, NT], F32)

        with contextlib.ExitStack() as sx:
            xTb = sbt(sx, "xTb", [128, 16, S], BF16)
            for c in range(16):
                kb.dma('pool', out=xTb[:, c, :], in_=xT[c * 128:(c + 1) * 128, :], writes=[("xTb", c)], semkey=f'ld_xT{c}')
            for c in range(16):
                ts(xTb[:, c, :], xTb[:, c, :], ln1[:, c:c + 1], None, ALU.mult, None, [("xTb", c), "ln1"], [("xTb", c)])
            XT = [("xTb", c) for c in range(16)]

            with contextlib.ExitStack() as s1:
                xf = [sbt(s1, f"xf{k}", [128, D], F32) for k in range(2)]
                junk = sbt(s1, "junk", [128, D], BF16)
                ss1 = sbt(s1, "ss1", [128, NT], F32)
                kb.op('dve', lambda g: g.memset(ss1[:], 0.0), writes=["ss1"])
                for i in range(NT):
                    kb.dma('sp', out=xf[i % 2][:], in_=x_tm[i * 128:(i + 1) * 128, :], writes=[f"xf{i%2}"], semkey=f'ld_xf{i%2}')
                    act(junk[:], xf[i % 2][:], AF.Square, [f"xf{i%2}", "ss1"], ["junk", "ss1"], accum=ss1[:, i:i + 1])
                rsqrt(rstd1[:], ss1[:], 1.0 / D, ["ss1"], ["rstd1"])
                kb.barrier()

            if "rstd1" in dbg_out:
                kb.dma('sp', out=dbg_out["rstd1"], in_=rstd1[:], reads=["rstd1"], writes=["dbg_rstd1"], semkey='dbg')

            def proj_pass(col0, epilogue, tagp):
                with contextlib.ExitStack() as sp_:
                    W = sbt(sp_, "Wp" + tagp, [128, 16, 1024], BF16)
                    for half in range(2):
                        kb.dma('pool', out=W[:, :, half * 512:(half + 1) * 512],
                               in_=w_in[:, col0 + half * 512: col0 + (half + 1) * 512].rearrange("(c p) n -> p c n", p=128),
                               writes=[("Wp", half)], semkey=f'ld_Wp{half}')
                    for i in range(NT):
                        banks = []
                        for half in range(2):
                            b = half + 2 * (i % 2)
                            banks.append(b)
                            for dc in range(16):
                                mm(PB[b][:, :], xTb[:, dc, i * 128:(i + 1) * 128], W[:, dc, half * 512:(half + 1) * 512],
                                   dc == 0, dc == 15, [("xTb", dc), ("Wp", half)], [P(b)], inc=(dc == 15))
                        epilogue(i, banks, sp_)
                    kb.barrier()

            if stage >= 2:
                def qk_epilogue_factory(wbc, wbc_name, scr, tg):
                    state = {}

                    def ep(i, banks, st):
                        if not state:
                            for k in range(2):
                                state['qf', k] = sbt(st, f"qf{tg}{k}", [128, 1024], F32)
                                state['sq', k] = sbt(st, f"sq{tg}{k}", [128, 1024], F32)
                                state['ss', k] = sbt(st, f"ss{tg}{k}", [128, 16], F32)
                                state['rot', k] = sbt(st, f"rot{tg}{k}", [128, 4, 16, 8], F32)
                                state['qb', k] = sbt(st, f"qb{tg}{k}", [128, 1024], BF16)
                                state['qt', k] = sbt(st, f"qt{tg}{k}", [128, 8, 128], BF16)
                        k = i % 2
                        qf, sq, ssq, rot, qb, qt = (state[n, k] for n in ('qf', 'sq', 'ss', 'rot', 'qb', 'qt'))
                        QF, SQ, SS, ROT, QB, QT = (f"{n}{k}" for n in ('qf', 'sq', 'ssq', 'rot', 'qb', 'qt'))
                        for half in range(2):
                            act(qf[:, half * 512:(half + 1) * 512], PB[banks[half]][:, :], AF.Copy,
                                [P(banks[half]), "rstd1"], [QF], scale=rstd1[:, i:i + 1])
                        tt(sq[:], qf[:], qf[:], ALU.mult, [QF], [SQ], e='pool')
                        red(ssq[:], sq[:].rearrange("p (b d) -> p b d", d=64), ALU.add, [SQ], [SS])
                        rsqrt(ssq[:], ssq[:], 1.0 / 64, [SS], [SS])
                        qf3 = qf[:].rearrange("p (b d) -> p b d", d=64)
                        tt(qf3, qf3, ssq[:].unsqueeze(2).to_broadcast([128, 16, 64]), ALU.mult, [QF, SS], [QF])
                        tt(qf3, qf3, wbc[:].unsqueeze(1).to_broadcast([128, 16, 64]), ALU.mult, [QF, wbc_name], [QF])
                        cosb = cos_t[:, i, :].unsqueeze(1).to_broadcast([128, 16, 8])
                        sinb = sin_t[:, i, :].unsqueeze(1).to_broadcast([128, 16, 8])
                        t1 = qf3[:, :, 0:8]
                        t2 = qf3[:, :, 8:16]
                        tt(rot[:, 0], t1, cosb, ALU.mult, [QF, "cos_t"], [ROT])
                        tt(rot[:, 1], t2, sinb, ALU.mult, [QF, "sin_t", ROT], [ROT])
                        tt(rot[:, 2], t2, cosb, ALU.mult, [QF, "cos_t", ROT], [ROT])
                        tt(rot[:, 3], t1, sinb, ALU.mult, [QF, "sin_t", ROT], [ROT])
                        qb3 = qb[:].rearrange("p (b d) -> p b d", d=64)
                        cp(qb3[:, :, 16:64], qf3[:, :, 16:64], [QF], [QB], e='pool')
                        tt(qb3[:, :, 0:8], rot[:, 0], rot[:, 1], ALU.subtract, [ROT, QB], [QB])
                        tt(qb3[:, :, 8:16], rot[:, 2], rot[:, 3], ALU.add, [ROT, QB], [QB])
                        if i >= 1:
                            ep_b(i - 1)
                        if i == NT - 1:
                            ep_b(i)

                    def ep_b(i):
                        k = i % 2
                        qb, qt = state['qb', k], state['qt', k]
                        QB, QT = f"qb{k}", f"qt{k}"
                        pb = 4 + (i % 2)
                        for hd in range(8):
                            tr(pbb(pb)[:, hd * 128:(hd + 1) * 128], qb[:, hd * 128:(hd + 1) * 128], ident_b[:],
                               [QB, "ident_b"], [P(pb)], inc=(hd == 7))
                        cp(qt[:].rearrange("p h t -> p (h t)"), pbb(pb), [P(pb)], [QT], e='act')
                        kb.dma('sp', out=scr[:, :, i * 128:(i + 1) * 128], in_=qt[:], reads=[QT], writes=[("scr" + tg, i)],
                               semkey=f'st_{tg}{k}')
                    return ep

                proj_pass(2576, qk_epilogue_factory(wq_bc, "wq_bc", qT_scr, "q"), "q")
                proj_pass(3600, qk_epilogue_factory(wk_bc, "wk_bc", kT_scr, "k"), "k")

                vstate = {}

                def v_ep(i, banks, st):
                    if not vstate:
                        vstate['vb'] = [sbt(st, f"vb{k}", [128, 1024], BF16) for k in range(2)]
                    vb = vstate['vb'][i % 2]
                    for half in range(2):
                        act(vb[:, half * 512:(half + 1) * 512], PB[banks[half]][:, :], AF.Copy,
                            [P(banks[half]), "rstd1"], [f"vb{i%2}"], scale=rstd1[:, i:i + 1])
                    kb.dma('sp', out=v_scr[i * 128:(i + 1) * 128, :], in_=vb[:], reads=[f"vb{i%2}"], writes=[("scrv", i)],
                           semkey=f'st_v{i%2}')
                proj_pass(4624, v_ep, "v")

            if stage >= 3:
                with contextlib.ExitStack() as s3:
                    xbcT = sbt(s3, "xbcT", [128, 12, S], BF16)
                    rstd_bc = sbt(s3, "rstd_bc", [128, S], F32)
                    with contextlib.ExitStack() as s31:
                        dg = [sbt(s31, f"dg{k}", [128, 128], F32) for k in range(2)]
                        for i in range(NT):
                            ts(dg[i % 2][:], ident_f[:], rstd1[:, i:i + 1], None, ALU.mult, None, ["ident_f", "rstd1"], [f"dg{i%2}"])
                            b = i // 4
                            mm(PB[b][:, (i % 4) * 128:(i % 4 + 1) * 128], ones_f[:], dg[i % 2][:], True, True,
                               ["ones_f", f"dg{i%2}"], [P(b)])
                        for b in range(4):
                            cp(rstd_bc[:, b * 512:(b + 1) * 512], PB[b][:, :], [P(b)], ["rstd_bc"])
                        Wx = [sbt(s31, f"Wx{k}", [128, 16, 512], BF16) for k in range(2)]
                        ub = [sbt(s31, f"ub{k}", [128, S + 3], F32) for k in range(1)]
                        acc = [sbt(s31, f"acc{k}", [128, S], F32) for k in range(1)]
                        for k in range(1):
                            kb.op('dve', lambda g: g.memset(ub[k][:, 0:3], 0.0), writes=[f"ub{k}"])
                        for blk in range(3):
                            kb.dma('pool', out=Wx[blk % 2][:],
                                   in_=w_in[:, 1024 + blk * 512: 1024 + (blk + 1) * 512].rearrange("(c p) n -> p c n", p=128),
                                   writes=[f"Wx{blk%2}"], semkey=f'ld_Wx{blk%2}')
                            for jj in range(4):
                                j = blk * 4 + jj
                                u = ub[0]
                                a = acc[0]
                                for tb in range(4):
                                    b = 4 + tb
                                    for dc in range(16):
                                        mm(PB[b][:, :], Wx[blk % 2][:, dc, jj * 128:(jj + 1) * 128], xTb[:, dc, tb * 512:(tb + 1) * 512],
                                           dc == 0, dc == 15, [f"Wx{blk%2}", ("xTb", dc)], [P(b)], inc=(dc == 15))
                                    tt(u[:, 3 + tb * 512: 3 + (tb + 1) * 512], PB[b][:, :], rstd_bc[:, tb * 512:(tb + 1) * 512], ALU.mult,
                                       [P(b), "rstd_bc"], ["ub0"])
                                act(a[:], u[:, 3:3 + S], AF.Identity, ["ub0", "cw", "cbias"], ["acc0"],
                                    bias=cbias[:, j:j + 1], scale=cw[:, j, 3:4])
                                for k in range(3):
                                    stt(a[:], u[:, k:k + S], cw[:, j, k:k + 1], a[:], ALU.mult, ALU.add,
                                        ["ub0", "cw", "acc0"], ["acc0"], e='dve')
                                act(xbcT[:, j, :], a[:], AF.Silu, ["acc0"], [("xbcT", j)])
                        kb.barrier()
                    if "xbcT" in dbg_out:
                        with contextlib.ExitStack() as sd:
                            tmpf = sbt(sd, "tmpf", [128, S], F32)
                            for j in range(12):
                                cp(tmpf[:], xbcT[:, j, :], [("xbcT", j)], ["tmpf"])
                                kb.dma('sp', out=dbg_out["xbcT"][j * 128:(j + 1) * 128, :], in_=tmpf[:], reads=["tmpf"], writes=["dbgx"], semkey='dbg')
                            kb.barrier()

                    dtv = sbt(s3, "dtv", [128, 256], F32)
                    sd_t = sbt(s3, "sd_t", [128, 256], F32)
                    E_t = sbt(s3, "E_t", [128, 256], F32)
                    nacs = sbt(s3, "nacs", [128, 256], F32)
                    cd_bc = sbt(s3, "cd_bc", [128, 256], F32)
                    acsT = sbt(s3, "acsT", [16, S], F32)
                    with contextlib.ExitStack() as s32:
                        Wdt = sbt(s32, "Wdt", [128, 16, 16], BF16)
                        kb.dma('pool', out=Wdt[:], in_=w_in[:, 2560:2576].rearrange("(c p) n -> p c n", p=128), writes=["Wdt"], semkey='ld_Wdt')
                        for i in range(NT):
                            for dc in range(16):
                                mm(PB[0][:, i * 16:(i + 1) * 16], xTb[:, dc, i * 128:(i + 1) * 128], Wdt[:, dc, :], dc == 0, dc == 15,
                                   [("xTb", dc), "Wdt"], [P(0)], inc=(dc == 15))
                        t1 = sbt(s32, "t1", [128, 256], F32)
                        t2 = sbt(s32, "t2", [128, 256], F32)
                        a_tok = sbt(s32, "a_tok", [128, 256], F32)
                        t13 = t1[:].rearrange("p (i h) -> p i h", h=16)
                        tt(t13, PB[0][:, 0:256].rearrange("p (i h) -> p i h", h=16), rstd1[:].unsqueeze(2).to_broadcast([128, NT, 16]),
                           ALU.mult, [P(0), "rstd1"], ["t1"])
                        tt(t13, t13, dtb_bc[:].unsqueeze(1).to_broadcast([128, NT, 16]), ALU.add, ["t1", "dtb_bc"], ["t1"])
                        stt(t2[:], t1[:], -1.0, t1[:], ALU.mult, ALU.max, ["t1"], ["t2"])
                        act(t2[:], t2[:], AF.Exp, ["t2"], ["t2"], scale=-1.0)
                        ts(t2[:], t2[:], 1.0, None, ALU.add, None, ["t2"], ["t2"])
                        act(t2[:], t2[:], AF.Ln, ["t2"], ["t2"])
                        ts(t1[:], t1[:], 0.0, None, ALU.max, None, ["t1"], ["t1"])
                        tt(dtv[:], t1[:], t2[:], ALU.add, ["t1", "t2"], ["dtv"])
                        tt(a_tok[:].rearrange("p (i h) -> p i h", h=16), dtv[:].rearrange("p (i h) -> p i h", h=16),
                           A_bc[:].unsqueeze(1).to_broadcast([128, NT, 16]), ALU.mult, ["dtv", "A_bc"], ["a_tok"])
                        mm(PB[1][:, 0:256], utri_f[:], a_tok[:], True, True, ["utri_f", "a_tok"], [P(1)])
                        mm(PB[2][:, 0:256], ones_f[:], a_tok[:], True, True, ["ones_f", "a_tok"], [P(2)])
                        for i in range(NT):
                            b = 4 + i // 4
                            mm(PB[b][0:16, (i % 4) * 128:(i % 4 + 1) * 128], a_tok[:, i * 16:(i + 1) * 16], utri_f[:], True, True,
                               ["a_tok", "utri_f"], [P(b)])
                        for b in range(4):
                            cp(acsT[:, b * 512:(b + 1) * 512], PB[4 + b][0:16, :], [P(4 + b)], ["acsT"])
                        act(E_t[:], PB[1][:, 0:256], AF.Exp, [P(1)], ["E_t"])
                        ts(nacs[:], PB[1][:, 0:256], -1.0, None, ALU.mult, None, [P(1)], ["nacs"])
                        act(cd_bc[:], PB[2][:, 0:256], AF.Exp, [P(2)], ["cd_bc"])
                        tt(t1[:], PB[2][:, 0:256], nacs[:], ALU.add, [P(2), "nacs"], ["t1"])
                        act(t1[:], t1[:], AF.Exp, ["t1"], ["t1"])
                        tt(sd_t[:], t1[:], dtv[:], ALU.mult, ["t1", "dtv"], ["sd_t"])
                        kb.barrier()

                    zst = {}

                    def z_ep(i, banks, st):
                        if not zst:
                            zst['z'] = [sbt(st, f"zsb{k}", [128, 1024], F32) for k in range(2)]
                        zb = zst['z'][i % 2]
                        for half in range(2):
                            act(zb[:, half * 512:(half + 1) * 512], PB[banks[half]][:, :], AF.Silu,
                                [P(banks[half]), "rstd1"], [f"zsb{i%2}"], scale=rstd1[:, i:i + 1])
                        kb.dma('sp', out=zs_scr[i * 128:(i + 1) * 128, :], in_=zb[:], reads=[f"zsb{i%2}"], writes=[("zs_scr", i)],
                               semkey=f'st_z{i%2}')
                    proj_pass(0, z_ep, "z")

                    with contextlib.ExitStack() as s34:
                        ehsel = sbt(s34, "ehsel", [16, 16, 128], F32)
                        kb.dma('sp', out=ehsel[:], in_=c_ehsel, writes=["ehsel"], semkey='ld_c3')
                        normw_bc = sbt(s34, "normw_bc", [128, 1024], F32)
                        kb.dma('sp', out=normw_bc[:], in_=ssd_norm_w.partition_broadcast(128), writes=["normw_bc"], semkey='ld_c3')
                        kb.regroup(["ehsel", "normw_bc"], 'ld_c3')
                        xdt = sbt(s34, "xdt", [128, 1024], BF16)
                        xdts = sbt(s34, "xdts", [128, 1024], BF16)
                        xsD = sbt(s34, "xsD", [128, 1024], BF16)
                        B_tm = sbt(s34, "B_tm", [128, 256], BF16)
                        cbT = sbt(s34, "cbT", [128, 256], BF16)
                        decT = [sbt(s34, f"decT{k}", [128, 512], BF16) for k in range(2)]
                        MT = [sbt(s34, f"MT{k}", [128, 512], BF16) for k in range(2)]
                        ytmp = [sbt(s34, f"ytmp{g}", [128, 512], F32) for g in range(2)]
                        y = sbt(s34, "y", [128, 1024], F32)
                        prev_f = sbt(s34, "prev_f", [128, 1024], F32)
                        prev_b = sbt(s34, "prev_b", [128, 1024], BF16)
                        zsb = sbt(s34, "zsb", [128, 1024], F32)
                        sq = sbt(s34, "sqy", [128, 1024], BF16)
                        ss2 = sbt(s34, "ss2", [128, 2], F32)
                        ynbs = [sbt(s34, f"ynb{k}", [128, 1024], BF16) for k in range(2)]
                        ycTs = [sbt(s34, f"ycT{k}", [128, 8, 128], BF16) for k in range(2)]

                        def gate_b(i):
                            k = i % 2
                            tsl_ = slice(i * 128, (i + 1) * 128)
                            for j in range(8):
                                tr(pbb(0)[:, j * 128:(j + 1) * 128], ynbs[k][:, j * 128:(j + 1) * 128], ident_b[:], [f"ynb{k}", "ident_b"], [P(0)], inc=(j == 7))
                            cp(ycTs[k][:].rearrange("p j t -> p (j t)"), pbb(0), [P(0)], [f"ycT{k}"], e='act')
                            kb.dma('sp', out=ycat_scr[:, 0:8, tsl_], in_=ycTs[k][:], reads=[f"ycT{k}"], writes=[("ycat", 0, i)], semkey=f'st_yc{k}')

                        YOB = [7, 2]
                        STB = [1, 0]
                        for i in range(NT):
                            ynb = ynbs[i % 2]
                            tsl = slice(i * 128, (i + 1) * 128)
                            kb.dma('sp', out=zsb[:], in_=zs_scr[tsl, :], reads=[("zs_scr", i)], writes=["zsb"], semkey='ld_zs')
                            for j in range(8):
                                tr(pbb(0)[:, j * 128:(j + 1) * 128], xbcT[:, j, tsl], ident_b[:], [("xbcT", j), "ident_b"], [P(0)], inc=(j == 7))
                            ps3 = pbb(0).rearrange("p (h d) -> p h d", d=64)
                            tt(xdt[:].rearrange("p (h d) -> p h d", d=64), ps3, dtv[:, i * 16:(i + 1) * 16].unsqueeze(2).to_broadcast([128, 16, 64]),
                               ALU.mult, [P(0), "dtv"], ["xdt"])
                            tt(xsD[:].rearrange("p (h d) -> p h d", d=64), ps3, dsk_bc[:].unsqueeze(2).to_broadcast([128, 16, 64]),
                               ALU.mult, [P(0), "dsk_bc"], ["xsD"])
                            tt(xdts[:].rearrange("p (h d) -> p h d", d=64), ps3, sd_t[:, i * 16:(i + 1) * 16].unsqueeze(2).to_broadcast([128, 16, 64]),
                               ALU.mult, [P(0), "sd_t"], ["xdts"])
                            for g in range(2):
                                tr(pbb(1)[:, g * 128:(g + 1) * 128], xbcT[:, 8 + g, tsl], ident_b[:], [("xbcT", 8 + g), "ident_b"], [P(1)], inc=(g == 1))
                            cp(B_tm[:], pbb(1)[:, 0:256], [P(1)], ["B_tm"], e='act')
                            for g in range(2):
                                mm(PB[2][:, g * 128:(g + 1) * 128], xbcT[:, 8 + g, tsl], xbcT[:, 10 + g, tsl], True, True,
                                   [("xbcT", 8 + g), ("xbcT", 10 + g)], [P(2)], inc=(g == 1))
                            cp(cbT[:], PB[2][:, 0:256], [P(2)], ["cbT"], e='act')

                            def emit_R(hq):
                                rb = 3 + (hq % 2)
                                for q in range(4):
                                    h = hq * 4 + q
                                    mm(PB[rb][:, q * 128:(q + 1) * 128], ehsel[:, h, :], acsT[:, tsl], q == 0, False, ["ehsel", "acsT"], [P(rb)],
                                       inc=False, sgc=True)
                                mm(PB[rb][:, :], ident_b[:], negmask4_b[:], False, True, ["ident_b", "negmask4_b"], [P(rb)], sgc=True)

                            def emit_Y(hq):
                                g = hq // 2
                                rb = 3 + (hq % 2)
                                dT = decT[hq % 2]
                                for q in range(4):
                                    h = hq * 4 + q
                                    act(dT[:, q * 128:(q + 1) * 128], PB[rb][:, q * 128:(q + 1) * 128], AF.Exp, [P(rb), "nacs"], [f"decT{hq%2}"],
                                        bias=nacs[:, i * 16 + h: i * 16 + h + 1])
                                tt(MT[hq % 2][:].rearrange("p (q l) -> p q l", q=4), dT[:].rearrange("p (q l) -> p q l", q=4),
                                   cbT[:, g * 128:(g + 1) * 128].unsqueeze(1).to_broadcast([128, 4, 128]), ALU.mult,
                                   [f"decT{hq%2}", "cbT"], [f"MT{hq%2}"])
                                if hq % 2 == 0:
                                    mm(PB[5 + g][:, :], ident_b[:], xsD[:, g * 512:(g + 1) * 512], True, False, ["ident_b", "xsD"], [P(5 + g)],
                                       inc=False, sgc=True)
                                for q in range(4):
                                    h = hq * 4 + q
                                    mm(PB[5 + g][:, (h % 8) * 64:(h % 8 + 1) * 64], MT[hq % 2][:, q * 128:(q + 1) * 128], xdt[:, h * 64:(h + 1) * 64],
                                       False, (hq % 2 == 1 and q == 3), [f"MT{hq%2}", "xdt"], [P(5 + g)], inc=(q == 3), sgc=True)

                            emit_R(0)
                            for hq in range(4):
                                if hq + 1 < 4:
                                    emit_R(hq + 1)
                                emit_Y(hq)
                            for g in range(2):
                                gs = slice(g * 512, (g + 1) * 512)
                                yb_, sb_ = YOB[g], STB[g]
                                if i > 0:
                                    mm(PB[yb_][:, :], xbcT[:, 10 + g, tsl], prev_b[:, gs], True, True, [("xbcT", 10 + g), "prev_b"], [P(yb_)])
                                if i < NT - 1:
                                    mm(PB[sb_][:, :], B_tm[:, g * 128:(g + 1) * 128], xdts[:, gs], True, True, ["B_tm", "xdts"], [P(sb_)])
                            for g in range(2):
                                gs = slice(g * 512, (g + 1) * 512)
                                yb_, sb_ = YOB[g], STB[g]
                                if i > 0:
                                    tt(ytmp[g][:].rearrange("p (h d) -> p h d", d=64), PB[yb_][:, :].rearrange("p (h d) -> p h d", d=64),
                                       E_t[:, i * 16 + g * 8: i * 16 + g * 8 + 8].unsqueeze(2).to_broadcast([128, 8, 64]), ALU.mult,
                                       [P(yb_), "E_t"], [f"ytmp{g}"])
                                    tt(y[:, gs], PB[5 + g][:, :], ytmp[g][:], ALU.add, [P(5 + g), f"ytmp{g}"], ["y"])
                                else:
                                    cp(y[:, gs], PB[5 + g][:, :], [P(5 + g)], ["y"])
                                if i < NT - 1:
                                    if i > 0:
                                        tt(prev_f[:, gs].rearrange("p (h d) -> p h d", d=64), prev_f[:, gs].rearrange("p (h d) -> p h d", d=64),
                                           cd_bc[:, i * 16 + g * 8: i * 16 + g * 8 + 8].unsqueeze(2).to_broadcast([128, 8, 64]), ALU.mult,
                                           ["prev_f", "cd_bc"], ["prev_f"])
                                        tt(prev_f[:, gs], prev_f[:, gs], PB[sb_][:, :], ALU.add, ["prev_f", P(sb_)], ["prev_f"])
                                    else:
                                        cp(prev_f[:, gs], PB[sb_][:, :], [P(sb_)], ["prev_f"])
                                    cp(prev_b[:, gs], prev_f[:, gs], ["prev_f"], ["prev_b"], e='act')
                            tt(y[:], y[:], zsb[:], ALU.mult, ["y", "zsb"], ["y"])
                            kb.op('dve', lambda g_: g_.memset(ss2[:], 0.0), writes=["ss2"])
                            for g in range(2):
                                gs = slice(g * 512, (g + 1) * 512)
                                act(sq[:, gs], y[:, gs], AF.Square, ["y", "ss2"], ["sqy", "ss2"], accum=ss2[:, g:g + 1])
                            rsqrt(ss2[:], ss2[:], 1.0 / 512, ["ss2"], ["ss2"])
                            for g in range(2):
                                gs = slice(g * 512, (g + 1) * 512)
                                stt(ynb[:, gs], y[:, gs], ss2[:, g:g + 1], normw_bc[:, gs], ALU.mult, ALU.mult, ["y", "ss2", "normw_bc"], [f"ynb{i%2}"])
                            if i >= 1:
                                gate_b(i - 1)
                            if i == NT - 1:
                                gate_b(i)
                        kb.barrier()
                    kb.barrier()
            kb.barrier()

        if stage >= 4:
            with contextlib.ExitStack() as s4:
                qT = sbt(s4, "qT", [128, 8, S], BF16)
                kTz = [sbt(s4, f"kTz{c}", [128, 8, S], BF16) for c in range(2)]
                V = sbt(s4, "V", [128, NT, 1024], BF16)
                subw_col = sbt(s4, "subw_col", [128, 1], F32)
                kb.dma('sp', out=subw_col[:], in_=subln_w.rearrange("(p o) -> p o", o=1), writes=["subw_col"], semkey='ld_c4')
                ts(subw_col[:], subw_col[:], 1.0 - LAM_INIT, None, ALU.mult, None, ["subw_col"], ["subw_col"])
                for c in range(2):
                    oth = slice((1 - c) * 64, (2 - c) * 64)
                    kb.op('pool', lambda g: g.memset(kTz[c][oth, :, :], 0.0), writes=[("kTzm", c)])
                for hd in range(8):
                    kb.dma('sp', out=qT[:, hd, :], in_=qT_scr[:, hd, :], reads=[("scrq", i) for i in range(NT)], writes=[("qTl", hd)], semkey='ld_q')
                    for c in range(2):
                        cs = slice(c * 64, (c + 1) * 64)
                        kb.dma('sp', out=kTz[c][cs, hd, :], in_=kT_scr[cs, hd, :], reads=[("scrk", i) for i in range(NT)], writes=[("kTl", c, hd)], semkey='ld_k')
                for i in range(NT):
                    kb.dma('sp', out=V[:, i, :], in_=v_scr[i * 128:(i + 1) * 128, :], reads=[("scrv", i)], writes=[("Vl", i)], semkey='ld_v')
                kb.regroup(["qT"], 'ld_q'); kb.regroup(["kTz0", "kTz1"], 'ld_k'); kb.regroup(["V"], 'ld_v')
                NPT = 3
                pT = [sbt(s4, f"pT{k}", [128, 2, 512], BF16) for k in range(NPT)]
                cR = [sbt(s4, f"cR{c}", [128, 512], F32) for c in range(2)]
                cO = [sbt(s4, f"cO{c}", [128, 512], F32) for c in range(2)]
                o0 = sbt(s4, "o0", [128, 512], F32)
                sqo = sbt(s4, "sqo", [128, 512], F32)
                rs = sbt(s4, "rs", [128, 512], F32)
                ssc = sbt(s4, "ssc", [128, 512], F32)
                ycA = [sbt(s4, f"ycA{k}", [128, 512], BF16) for k in range(2)]
                OB, RB, SSB = 4, 6, 3
                steps = []
                blk = 0
                for hd in range(8):
                    for qb in range(4):
                        nkt = 4 * qb + 4
                        for c in range(2):
                            for kp in range(nkt // 2):
                                steps.append(dict(hd=hd, qb=qb, c=c, kp=kp, nkt=nkt, blk=blk, last=(c == 1 and kp == nkt // 2 - 1)))
                        blk += 1

                def geom(st, h):
                    kt = 2 * st['kp'] + h
                    j = kt - 4 * st['qb']
                    off = max(j, 0) * 128
                    return kt, j, off, 4 * st['qb'] * 128 + off, 512 - off

                def emit_S(i):
                    st = steps[i]
                    d = i % 2
                    for h in range(2):
                        kt, j, off, q0, nq = geom(st, h)
                        mm(PB[2 * d + h][:, 0:nq], kTz[st['c']][:, st['hd'], kt * 128:(kt + 1) * 128], qT[:, st['hd'], q0:q0 + nq],
                           True, True, [f"kTz{st['c']}", ("kTzm", st['c']), "qT"], [("psd", d)], inc=(h == 1))

                def emit_rest(i):
                    st = steps[i]
                    d = i % 2
                    pk = i % NPT
                    c, hd = st['c'], st['hd']
                    W = geom(st, 0)[4]
                    if TWO_EXP:
                        for h in range(2):
                            nq_h = geom(st, h)[4]
                            act(pT[pk][:, h, 0:nq_h], PB[2 * d + h][:, 0:nq_h], AF.Exp, [("psd", d)], [f"pT{pk}"])
                    else:
                        act(pT[pk][:, :, 0:W], PD[d][:, :].rearrange("p (h w) -> p h w", h=2)[:, :, 0:W], AF.Exp, [("psd", d)], [f"pT{pk}"])
                    for h in range(2):
                        kt, j, off, q0, nq = geom(st, h)
                        if j >= 0:
                            tt(pT[pk][:, h, 0:128], pT[pk][:, h, 0:128], utri_b[:], ALU.mult, [f"pT{pk}", "utri_b"], [f"pT{pk}"], e='dve')
                    for h in range(2):
                        kt, j, off, q0, nq = geom(st, h)
                        mm(PB[OB + c][:, off:off + nq], V[:, kt, hd * 128:(hd + 1) * 128], pT[pk][:, h, 0:nq], kt == 0, kt == st['nkt'] - 1,
                           ["V", f"pT{pk}"], [P(OB + c)], inc=False, sgc=True)
                        mm(PB[RB + c][:, off:off + nq], ones_b[:], pT[pk][:, h, 0:nq], kt == 0, kt == st['nkt'] - 1,
                           ["ones_b", f"pT{pk}"], [P(RB + c)], sgc=True)

                def epiA(st):
                    act(cR[0][:], PB[RB][:, :], AF.Ln, [P(RB)], ["cR0"])
                    cp(cO[0][:], PB[OB][:, :], [P(OB)], ["cO0"])
                    act(cR[1][:], PB[RB + 1][:, :], AF.Ln, [P(RB + 1)], ["cR1"])
                    cp(cO[1][:], PB[OB + 1][:, :], [P(OB + 1)], ["cO1"])
                    for c in range(2):
                        act(cR[c][:], cR[c][:], AF.Exp, [f"cR{c}"], [f"cR{c}"], scale=-1.0)
                    for c in range(2):
                        tt(cO[c][:], cO[c][:], cR[c][:], ALU.mult, [f"cO{c}", f"cR{c}"], [f"cO{c}"])
                    stt(o0[:], cO[1][:], lam[:, 1:2], cO[0][:], ALU.mult, ALU.add, ["cO1", "lam", "cO0"], ["o0"])
                    tt(sqo[:], o0[:], o0[:], ALU.mult, ["o0"], ["sqo"])

                def epiB(st):
                    mm(PB[SSB][:, :], ones_f[:], sqo[:], True, True, ["ones_f", "sqo"], [("psd", 1)])
                    cp(ssc[:], PB[SSB][:, :], [("psd", 1)], ["ssc"])

                def epiC(st):
                    yk = st['blk'] % 2
                    act(rs[:], ssc[:], AF.Ln, ["ssc", "eps_t"], ["rs"], bias=eps_t[:], scale=1.0 / 128)
                    act(rs[:], rs[:], AF.Exp, ["rs"], ["rs"], scale=-0.5)
                    stt(ycA[yk][:], o0[:], subw_col[:, 0:1], rs[:], ALU.mult, ALU.mult, ["o0", "subw_col", "rs"], [f"ycA{yk}"])
                    kb.dma('sp', out=ycat_scr[:, 8 + st['hd'], st['qb'] * 512:(st['qb'] + 1) * 512], in_=ycA[yk][:], reads=[f"ycA{yk}"],
                           writes=[("ycat", 1, st['hd'], st['qb'])], semkey=f'st_ya{yk}')

                NS = len(steps)
                emit_S(0)
                pend = []
                for i in range(NS):
                    if i + 1 < NS:
                        emit_S(i + 1)
                    emit_rest(i)
                    while pend and (pend[0][0] <= i or steps[i]['last']):
                        _, fn, st_ = pend.pop(0)
                        fn(st_)
                    if steps[i]['last']:
                        epiA(steps[i])
                        pend = [(i + 4, epiB, steps[i]), (i + 6, epiC, steps[i])]
                for _, fn, st_ in pend:
                    fn(st_)
                kb.barrier()

        if "ycat" in dbg_out:
            with contextlib.ExitStack() as sd:
                tb16 = sbt(sd, "tb16", [128, S], BF16)
                tmpf = sbt(sd, "tmpf2", [128, S], F32)
                for j in range(16):
                    kb.dma('sp', out=tb16[:], in_=ycat_scr[:, j, :], reads=[], writes=["tb16"], semkey='dbg2')
                    cp(tmpf[:], tb16[:], ["tb16"], ["tmpf2"])
                    kb.dma('sp', out=dbg_out["ycat"][j * 128:(j + 1) * 128, :], in_=tmpf[:], reads=["tmpf2"], writes=["dbgy"], semkey='dbg')
                kb.barrier()

        slots = sbt(es, "slots", [128, NT, 2], I32)
        wts = sbt(es, "wts", [128, NT, 2], F32)
        kb.op('dve', lambda g: g.memset(slots[:], 0), writes=["slots"])
        kb.op('dve', lambda g: g.memset(wts[:], 0.0), writes=["wts"])
        if stage >= 5:
            with contextlib.ExitStack() as s5:
                ycT_all = sbt(s5, "ycT_all", [128, 16, S], BF16)
                Wo = sbt(s5, "Wo", [128, 16, D], BF16)
                for j in range(16):
                    kb.dma('sp', out=ycT_all[:, j, :], in_=ycat_scr[:, j, :], writes=[("ycl", j)], semkey='ld_yc')
                kb.regroup(["ycT_all"], 'ld_yc')
                for cb in range(4):
                    kb.dma('pool', out=Wo[:, :, cb * 512:(cb + 1) * 512],
                           in_=w_out[:, cb * 512:(cb + 1) * 512].rearrange("(c p) n -> p c n", p=128), writes=[("Wol", cb)], semkey='ld_Wo')
                kb.regroup(["Wo"], 'ld_Wo')
                ln2_bc = sbt(s5, "ln2_bc", [128, D], F32)
                kb.dma('sp', out=ln2_bc[:], in_=ln2_w.partition_broadcast(128), writes=["ln2_bc"], semkey='ld_c5')
                wr_sb = sbt(s5, "wr_sb", [128, 16, 36], F32)
                kb.dma('sp', out=wr_sb[:], in_=w_r.rearrange("(c p) n -> p c n", p=128), writes=["wr_sb"], semkey='ld_c5')
                kb.regroup(["ln2_bc", "wr_sb"], 'ld_c5')
                xr = sbt(s5, "xr", [128, D], F32)
                hsb = sbt(s5, "hsb", [128, D], F32)
                ssh = sbt(s5, "ssh", [128, 1], F32)
                hnT = sbt(s5, "hnT", [128, 16, 128], F32)
                r8 = sbt(s5, "r8", [128, 16], F32)
                goh = sbt(s5, "goh", [128, 4], F32)
                ein4 = sbt(s5, "ein4", [128, 4, 8], F32)
                ein = sbt(s5, "ein", [128, 8], F32)
                oh1 = sbt(s5, "oh1", [128, 8], F32)
                oh2 = sbt(s5, "oh2", [128, 8], F32)
                em = sbt(s5, "em", [128, 8], F32)
                sel1 = sbt(s5, "sel1", [128, 32], F32)
                sel2 = sbt(s5, "sel2", [128, 32], F32)
                selb = sbt(s5, "selb", [128, 32], BF16)
                cnt = sbt(s5, "cnt", [128, 32], F32)
                rk = sbt(s5, "rk", [128, 32], F32)
                tmp32 = sbt(s5, "tmp32", [128, 32], F32)
                okm = sbt(s5, "okm", [128, 32], F32)
                slf = sbt(s5, "slf", [128, 2], F32)
                kb.op('dve', lambda g: g.memset(cnt[:], 0.0), writes=["cnt"])
                hns = [sbt(s5, f"hn{k}", [128, D], F32) for k in range(2)]
                hnbs = [sbt(s5, f"hnb{k}", [128, D], BF16) for k in range(3)]
                lgs = [sbt(s5, f"lg{k}", [128, 36], F32) for k in range(2)]

                def A1(i):
                        tsl = slice(i * 128, (i + 1) * 128)
                        kb.dma('sp', out=xr[:], in_=x_tm[tsl, :], writes=["xr"], semkey='ld_xr')
                        for cb in range(4):
                            for cc in range(16):
                                mm(PB[cb][:, :], ycT_all[:, cc, tsl], Wo[:, cc, cb * 512:(cb + 1) * 512], cc == 0, cc == 15,
                                   ["ycT_all", "Wo"], [P(cb)], inc=(cc == 15))
                            tt(hsb[:, cb * 512:(cb + 1) * 512], PB[cb][:, :], xr[:, cb * 512:(cb + 1) * 512], ALU.add, [P(cb), "xr"], ["hsb"])
                        kb.dma('sp', out=h_scr[tsl, :], in_=hsb[:], reads=["hsb"], writes=[("h_scr", i)], semkey='st_h')

                def A2(i):
                        kb.op('dve', lambda g: g.memset(ssh[:], 0.0), writes=["ssh"])
                        act(hnbs[i % 3][:], hsb[:], AF.Square, ["hsb", "ssh"], [f"hnb{i%3}", "ssh"], accum=ssh[:])
                        rsqrt(ssh[:], ssh[:], 1.0 / D, ["ssh"], ["ssh"])
                        stt(hns[i % 2][:], hsb[:], ssh[:, 0:1], ln2_bc[:], ALU.mult, ALU.mult, ["hsb", "ssh", "ln2_bc"], [f"hn{i%2}"])
                        cp(hnbs[i % 3][:].rearrange("t (c p) -> t c p", p=128), hns[i % 2][:].rearrange("t (p c) -> t c p", c=16), [f"hn{i%2}"], [f"hnb{i%3}"], e='pool')


                def Btr(i):
                        for half in range(2):
                            b = 4 + half
                            for d8 in range(8):
                                dc = half * 8 + d8
                                tr(PB[b][:, (d8 % 4) * 128:(d8 % 4 + 1) * 128] if d8 < 4 else PB[b][:, (d8 % 4) * 128:(d8 % 4 + 1) * 128],
                                   hns[i % 2][:, dc * 128:(dc + 1) * 128], ident_f[:], [f"hn{i%2}", "ident_f"], [P(b)], inc=(d8 % 4 == 3))
                                if d8 % 4 == 3:
                                    c0 = half * 8 + (d8 // 4) * 4
                                    cp(hnT[:, c0:c0 + 4, :].rearrange("p c t -> p (c t)"), PB[b][:, :], [P(b)], ["hnT"], e='act')

                def Brt(i):
                        for dc in range(16):
                            mm(PB[6][:, 0:36], hnT[:, dc, :], wr_sb[:, dc, :], dc == 0, dc == 15, ["hnT", "wr_sb"], [P(6)], inc=(dc == 15))

                def Blg(i):
                        tt(lgs[i % 2][:], PB[6][:, 0:36], br_bc[:], ALU.add, [P(6), "br_bc"], [f"lg{i%2}"])


                def Ca(i):
                        red(r8[:, 0:1], lgs[i % 2][:, 0:4], ALU.max, [f"lg{i%2}"], ["r8"])
                        ts(goh[:], lgs[i % 2][:, 0:4], r8[:, 0:1], None, ALU.is_equal, None, [f"lg{i%2}", "r8"], ["goh"])
                        ts(tmp32[:, 0:4], lgs[i % 2][:, 0:4], r8[:, 0:1], None, ALU.subtract, None, [f"lg{i%2}", "r8"], ["tmp32"])
                        act(tmp32[:, 0:4], tmp32[:, 0:4], AF.Exp, ["tmp32"], ["tmp32"])
                        red(r8[:, 1:2], tmp32[:, 0:4], ALU.add, ["tmp32"], ["r8"])
                        kb.op('dve', lambda g: g.reciprocal(out=r8[:, 2:3], in_=r8[:, 1:2]), reads=["r8"], writes=["r8"])
                        tt(ein4[:], lgs[i % 2][:, 4:36].rearrange("p (g e) -> p g e", e=8), goh[:].unsqueeze(2).to_broadcast([128, 4, 8]), ALU.mult,
                           [f"lg{i%2}", "goh"], ["ein4"])
                        red(ein[:], ein4[:].rearrange("p g e -> p e g"), ALU.add, ["ein4"], ["ein"])
                        red(r8[:, 3:4], ein[:], ALU.max, ["ein"], ["r8"])
                        ts(oh1[:], ein[:], r8[:, 3:4], None, ALU.is_equal, None, ["ein", "r8"], ["oh1"])
                        stt(em[:], oh1[:], -1e30, ein[:], ALU.mult, ALU.add, ["oh1", "ein"], ["em"])
                        red(r8[:, 4:5], em[:], ALU.max, ["em"], ["r8"])
                        ts(oh2[:], em[:], r8[:, 4:5], None, ALU.is_equal, None, ["em", "r8"], ["oh2"])
                        tt(r8[:, 5:6], r8[:, 4:5], r8[:, 3:4], ALU.subtract, ["r8"], ["r8"])
                        act(r8[:, 5:6], r8[:, 5:6], AF.Exp, ["r8"], ["r8"])
                        ts(r8[:, 6:7], r8[:, 5:6], 1.0, None, ALU.add, None, ["r8"], ["r8"])
                        kb.op('dve', lambda g: g.reciprocal(out=r8[:, 6:7], in_=r8[:, 6:7]), reads=["r8"], writes=["r8"])
                        tt(wts[:, i, 0:1], r8[:, 6:7], r8[:, 2:3], ALU.mult, ["r8"], ["wts"])
                        tt(wts[:, i, 1:2], wts[:, i, 0:1], r8[:, 5:6], ALU.mult, ["wts", "r8"], ["wts"])
                        tt(sel1[:].rearrange("p (g e) -> p g e", e=8), goh[:].unsqueeze(2).to_broadcast([128, 4, 8]),
                           oh1[:].unsqueeze(1).to_broadcast([128, 4, 8]), ALU.mult, ["goh", "oh1"], ["sel1"])
                        tt(sel2[:].rearrange("p (g e) -> p g e", e=8), goh[:].unsqueeze(2).to_broadcast([128, 4, 8]),
                           oh2[:].unsqueeze(1).to_broadcast([128, 4, 8]), ALU.mult, ["goh", "oh2"], ["sel2"])
                        tt(selb[:], sel1[:], sel2[:], ALU.add, ["sel1", "sel2"], ["selb"])

                def Crk(i):
                        mm(PB[7][:, 0:32], ustrict_b[:], selb[:], True, True, ["ustrict_b", "selb"], [P(7)])
                        mm(PB[7][:, 32:64], ones_b[:], selb[:], True, True, ["ones_b", "selb"], [P(7)])

                def Cb(i):
                        tt(rk[:], PB[7][:, 0:32], cnt[:], ALU.add, [P(7), "cnt"], ["rk"])
                        tt(cnt[:], cnt[:], PB[7][:, 32:64], ALU.add, ["cnt", P(7)], ["cnt"])
                        ts(okm[:], rk[:], float(CAP) - 0.5, None, ALU.is_lt, None, ["rk"], ["okm"])
                        tt(rk[:], rk[:], ebase[:], ALU.add, ["rk", "ebase"], ["rk"])
                        ts(rk[:], rk[:], -float(NE * CAP), None, ALU.add, None, ["rk"], ["rk"])
                        tt(rk[:], rk[:], okm[:], ALU.mult, ["rk", "okm"], ["rk"])
                        ts(rk[:], rk[:], float(NE * CAP), None, ALU.add, None, ["rk"], ["rk"])
                        tt(tmp32[:], okm[:], sel1[:], ALU.mult, ["okm", "sel1"], ["tmp32"])
                        red(r8[:, 8:9], tmp32[:], ALU.add, ["tmp32"], ["r8"])
                        tt(tmp32[:], okm[:], sel2[:], ALU.mult, ["okm", "sel2", "r8"], ["tmp32"])
                        red(r8[:, 9:10], tmp32[:], ALU.add, ["tmp32"], ["r8"])
                        tt(wts[:, i, :], wts[:, i, :], r8[:, 8:10], ALU.mult, ["wts", "r8"], ["wts"])
                        tt(tmp32[:], rk[:], sel1[:], ALU.mult, ["rk", "sel1"], ["tmp32"])
                        red(slf[:, 0:1], tmp32[:], ALU.add, ["tmp32"], ["slf"])
                        tt(tmp32[:], rk[:], sel2[:], ALU.mult, ["rk", "sel2", "slf"], ["tmp32"])
                        red(slf[:, 1:2], tmp32[:], ALU.add, ["tmp32"], ["slf"])
                        cp(slots[:, i, :], slf[:], ["slf"], ["slots"])
                        for k in range(2):
                            kb.dma('pool', fn=lambda g: g.indirect_dma_start(
                                out=xg_scr, out_offset=bass.IndirectOffsetOnAxis(ap=slots[:, i, k:k + 1], axis=0),
                                in_=hnbs[i % 3][:], in_offset=None), reads=[f"hnb{i%3}", "slots"], writes=[("xgs", i, k)], semkey=f'sc_xg{i%3}')


                A1(0)
                A2(0)
                for i in range(NT):
                    if i + 1 < NT:
                        A1(i + 1)
                    Btr(i)
                    if i >= 1:
                        Ca(i - 1)
                    Brt(i)
                    if i >= 1:
                        Crk(i - 1)
                        Cb(i - 1)
                    Blg(i)
                    if i + 1 < NT:
                        A2(i + 1)
                Ca(NT - 1)
                Crk(NT - 1)
                Cb(NT - 1)
                kb.regroup(["xg_scr"], 'sc_xg0')
                kb.barrier()
            if "h" in dbg_out:
                kb.dma('sp', out=dbg_out["h"], in_=h_scr, reads=[("h_scr", i) for i in range(NT)], writes=["dbgh"], semkey='dbg')
            if "slots" in dbg_out:
                with contextlib.ExitStack() as sd:
                    sf = sbt(sd, "sf", [128, NT * 2], F32)
                    cp(sf[:], slots[:].rearrange("p i k -> p (i k)"), ["slots"], ["sf"])
                    kb.dma('sp', out=dbg_out["slots"], in_=sf[:], reads=["sf"], writes=["dbgs"], semkey='dbg')
                    kb.dma('sp', out=dbg_out["wts"], in_=wts[:].rearrange("p i k -> p (i k)"), reads=["wts"], writes=["dbgw"], semkey='dbg')
                    kb.barrier()

        if stage >= 6:
            with contextlib.ExitStack() as s6:
                NWB = 4
                wbuf = [sbt(s6, f"wbuf{k}", [128, 16 * 1024], BF16) for k in range(NWB)]
                xgs = [sbt(s6, f"xg{k}", [128, NST, D], BF16) for k in range(2)]
                xgT = sbt(s6, "xgT", [128, 16, CAP], BF16)
                gT = sbt(s6, "gT", [128, 8, CAP], BF16)
                hT = sbt(s6, "hT", [128, 8, CAP], BF16)
                ysb = [sbt(s6, f"ysb{k}", [128, D], BF16) for k in range(2)]
                wctr = [0]
                kb.op('dve', lambda g: g.memset(ysb[0][:], 0.0), writes=["ysb0"])
                kb.dma('sp', out=ys_scr[NE * CAP:NE * CAP + 128, :], in_=ysb[0][:], reads=["ysb0"], writes=["ys_scr"], semkey='st_ys0')

                def wload(src, a, b_, flat):
                    k = wctr[0] % NWB
                    wctr[0] += 1
                    v3 = wbuf[k][:, 0:a * b_].rearrange("p (a b) -> p a b", b=b_)
                    kb.dma('pool', out=(wbuf[k][:, 0:a * b_] if flat else v3), in_=src, writes=[f"wbuf{k}"], semkey=f'ld_w{k}')
                    return v3, f"wbuf{k}"

                def xgload(e):
                    kb.dma('sp', out=xgs[e % 2][:], in_=xg_scr[e * CAP:(e + 1) * CAP, :].rearrange("(s p) d -> p s d", p=128),
                           reads=["xg_scr"], writes=[f"xg{e%2}"], semkey=f'ld_xg{e%2}')
                xgload(0)
                ytile = [0]
                for e in range(NE):
                    if e + 1 < NE:
                        xgload(e + 1)
                    xg = xgs[e % 2]
                    for st_ in range(NST):
                        for dc4 in range(4):
                            b = dc4 % 2
                            for q4 in range(4):
                                dc = dc4 * 4 + q4
                                tr(pbb(b)[:, q4 * 128:(q4 + 1) * 128], xg[:, st_, dc * 128:(dc + 1) * 128], ident_b[:], [f"xg{e%2}", "ident_b"], [P(b)],
                                   inc=(q4 == 3))
                            cp(xgT[:, dc4 * 4:(dc4 + 1) * 4, st_ * 128:(st_ + 1) * 128],
                               pbb(b)[:, 0:512].rearrange("p (c t) -> p c t", t=128), [P(b)], ["xgT"], e=('act' if dc4 % 2 else 'dve'))
                    Wg, wgn = wload(w_gate[e].rearrange("(p c) n -> p (c n)", c=16), 16, 1024, True)
                    Wu, wun = wload(w_up[e].rearrange("(p c) n -> p (c n)", c=16), 16, 1024, True)
                    for hc in range(8):
                        bg = 2 + (hc % 2)
                        for dc in range(16):
                            mm(PB[bg][:, 0:CAP], Wg[:, dc, hc * 128:(hc + 1) * 128], xgT[:, dc, :], dc == 0, dc == 15, [wgn, "xgT"], [P(bg)], inc=(dc == 15))
                        act(gT[:, hc, :], PB[bg][:, 0:CAP], AF.Silu, [P(bg)], [("gT", hc)])
                    for hc in range(8):
                        bu = 4 + (hc % 2)
                        for dc in range(16):
                            mm(PB[bu][:, 0:CAP], Wu[:, dc, hc * 128:(hc + 1) * 128], xgT[:, dc, :], dc == 0, dc == 15, [wun, "xgT"], [P(bu)], inc=(dc == 15))
                        tt(hT[:, hc, :], gT[:, hc, :], PB[bu][:, 0:CAP], ALU.mult, [("gT", hc), P(bu)], ["hT"])
                    Wdv, wdn = wload(w_down[e].rearrange("(c p) n -> p c n", p=128), 8, D, False)
                    for st_ in range(NST):
                        yk = ytile[0] % 2
                        ytile[0] += 1
                        yb = ysb[yk]
                        for cb in range(4):
                            b = 6 + (cb % 2)
                            for kc in range(8):
                                mm(PB[b][:, :], hT[:, kc, st_ * 128:(st_ + 1) * 128], Wdv[:, kc, cb * 512:(cb + 1) * 512], kc == 0, kc == 7,
                                   ["hT", wdn], [P(b)], inc=(kc == 7))
                            cp(yb[:, cb * 512:(cb + 1) * 512], PB[b][:, :], [P(b)], [f"ysb{yk}"], e=('act' if cb % 2 else 'dve'))
                        kb.dma('sp', out=ys_scr[e * CAP + st_ * 128: e * CAP + (st_ + 1) * 128, :], in_=yb[:], reads=[f"ysb{yk}"],
                               writes=[("yss", e, st_)], semkey=f'st_ys{yk}')
                kb.barrier()
                kb.regroup(["ys_scr"], 'st_ys0')

        if stage >= 7:
            with contextlib.ExitStack() as s7:
                NB7 = 4
                hb_ = [sbt(s7, f"hc{k}", [128, D], F32) for k in range(NB7)]
                y1 = [sbt(s7, f"y1{k}", [128, D], BF16) for k in range(NB7)]
                y2 = [sbt(s7, f"y2{k}", [128, D], BF16) for k in range(NB7)]
                ia = [sbt(s7, f"ia{k}", [128, 1], I32) for k in range(NB7)]
                ib = [sbt(s7, f"ib{k}", [128, 1], I32) for k in range(NB7)]
                for k in range(NB7):
                    kb.op('dve', lambda g: g.memset(ia[k][:], 0), writes=[f"ia{k}"])
                    kb.op('dve', lambda g: g.memset(ib[k][:], 0), writes=[f"ib{k}"])
                    kb.op('dve', lambda g: g.memset(y1[k][:], 0.0), writes=[f"y1{k}"])
                    kb.op('dve', lambda g: g.memset(y2[k][:], 0.0), writes=[f"y2{k}"])

                def p7_load(i):
                    k = i % NB7
                    tsl = slice(i * 128, (i + 1) * 128)
                    kb.dma('sp', out=hb_[k][:], in_=h_scr[tsl, :], reads=[("h_scr", i)], writes=[f"hc{k}"], semkey=f'ld_hc{k}')
                    cp(ia[k][:], slots[:, i, 0:1], ["slots"], [f"ia{k}"])
                    cp(ib[k][:], slots[:, i, 1:2], ["slots"], [f"ib{k}"])
                    kb.dma('pool', fn=lambda g: g.indirect_dma_start(
                        out=y1[k][:], out_offset=None, in_=ys_scr,
                        in_offset=bass.IndirectOffsetOnAxis(ap=ia[k][:, 0:1], axis=0)),
                        reads=["ys_scr", f"ia{k}"], writes=[f"y1{k}"], semkey=f'ga_y1{k}')
                    kb.dma('pool', fn=lambda g: g.indirect_dma_start(
                        out=y2[k][:], out_offset=None, in_=ys_scr,
                        in_offset=bass.IndirectOffsetOnAxis(ap=ib[k][:, 0:1], axis=0)),
                        reads=["ys_scr", f"ib{k}"], writes=[f"y2{k}"], semkey=f'ga_y2{k}')

                for i in range(min(NB7 - 1, NT)):
                    p7_load(i)
                for i in range(NT):
                    if i + NB7 - 1 < NT:
                        p7_load(i + NB7 - 1)
                    k = i % NB7
                    tsl = slice(i * 128, (i + 1) * 128)
                    stt(hb_[k][:], y1[k][:], wts[:, i, 0:1], hb_[k][:], ALU.mult, ALU.add, [f"y1{k}", "wts", f"hc{k}"], [f"hc{k}"])
                    stt(hb_[k][:], y2[k][:], wts[:, i, 1:2], hb_[k][:], ALU.mult, ALU.add, [f"y2{k}", "wts", f"hc{k}"], [f"hc{k}"])
                    kb.dma('sp', out=out[tsl, :], in_=hb_[k][:], reads=[f"hc{k}"], writes=[("out", i)], semkey=f'st_out{k}')
                kb.barrier()
        kb.barrier()
        print("instructions (incl. waits):", kb.ninst, "sems:", len(kb.sems))
    return nc


def host_consts():
    j = np.arange(128)[:, None]
    l = np.arange(128)[None, :]
    c = {}
    c["c_ident"] = np.eye(128, dtype=np.float32)
    c["c_utri"] = (j <= l).astype(np.float32)
    c["c_ustrict"] = (j < l).astype(np.float32)
    c["c_negmask4"] = np.tile(np.where(l < j, -30000.0, 0.0).astype(np.float32), (1, 4))
    eh = np.zeros((16, 16, 128), np.float32)
    for h in range(16):
        eh[h, h, :] = 1.0
    c["c_ehsel"] = eh
    invf = (500000.0 ** (-np.arange(0, 16, 2, dtype=np.float32) / 16.0)).astype(np.float32)
    c["c_invf"] = np.tile(invf[None, :], (128, 1)).astype(np.float32)
    c["c_ebase"] = np.tile((np.arange(NE, dtype=np.float32) * CAP)[None, :], (128, 1)).astype(np.float32)
    return c


def make_in_maps(inputs, cores):
    f = lambda a: np.ascontiguousarray(a)
    shared = dict(host_consts())
    shared["w_in"] = f(inputs["w_in"][0])
    shared["w_out"] = f(inputs["w_out"][0])
    shared["w_gate"] = f(inputs["w_gate"][0])
    shared["w_up"] = f(inputs["w_up"][0])
    shared["w_down"] = f(inputs["w_down"][0])
    shared["ln1_t"] = f(inputs["ln1_w"][0].reshape(16, 128).T)
    shared["conv_wt"] = f(inputs["conv_w"][0].reshape(4, 12, 128).transpose(2, 1, 0))
    shared["conv_bt"] = f(inputs["conv_b"][0].reshape(12, 128).T)
    for k in ("dt_bias", "a_log", "d_skip", "ssd_norm_w", "q_norm_w", "k_norm_w", "subln_w", "ln2_w"):
        shared[k] = f(inputs[k][0])
    shared["lq1"] = f(inputs["lambda_q1"][0]); shared["lk1"] = f(inputs["lambda_k1"][0])
    shared["lq2"] = f(inputs["lambda_q2"][0]); shared["lk2"] = f(inputs["lambda_k2"][0])
    shared["w_r"] = f(np.concatenate([inputs["w_router_group"][0], inputs["w_router_expert"][0]], axis=1))
    shared["b_r"] = f(np.concatenate([inputs["b_router_group"][0], inputs["b_router_expert"][0]], axis=0))
    maps = []
    for b in cores:
        m = dict(shared)
        xb = inputs["x"][b]
        m["x_tm"] = f(xb)
        m["xT"] = f(xb.T)
        m["pos_tm"] = f(inputs["positions"][b].reshape(NT, 128).T.astype(np.int32))
        maps.append(m)
    return maps


def kernel(**inputs):
    inputs = {k: np.asarray(v) for k, v in inputs.items()}
    nc = build()
    maps = make_in_maps(inputs, list(range(8)))
    res = run_bass_kernel_spmd(nc, maps, core_ids=list(range(8)))
    return np.stack([r["out"] for r in res.results], axis=0).astype(np.float32)
```
